# Optimizing a Trainium2 kernel written in Bass

```python
import jax
import jax.numpy as jnp
from jax import lax
import numpy as np

D_MODEL = 1024
BATCH = 16
SEQ = 2048
DEPTH = 1

GRID_W = 64
CTX_LEN = 256
HEAD_DIM = 64
N_Q_HEADS = 8
N_KV_HEADS = 2
Q_PER_KV = N_Q_HEADS // N_KV_HEADS
ATTN_WIDTH = N_Q_HEADS * HEAD_DIM
KV_WIDTH = N_KV_HEADS * HEAD_DIM
WINDOW = 128
BLOCK = 128
ROPE_BASE = 10000.0
ROPE_PAIRS = HEAD_DIM // 4
CONV_WIDTH = D_MODEL // 2
CONV_K = 3
PEER_HEADS = 8
PEER_KEY_DIM = 256
PEER_N_KEYS = 128
PEER_N_EXPERTS = PEER_N_KEYS * PEER_N_KEYS
PEER_TOPK = 16
PEER_CHUNK = 128
N_MOD = 6
EPS = 1e-6
NEG_INF = -1e30

PROJ_SIZES = (ATTN_WIDTH, KV_WIDTH, KV_WIDTH, CONV_WIDTH, CONV_WIDTH, CONV_WIDTH, D_MODEL, D_MODEL)
PROJ_WIDTH = sum(PROJ_SIZES)
PROJ_SPLITS = tuple(int(s) for s in np.cumsum(PROJ_SIZES)[:-1])
KV_START = ATTN_WIDTH
KV_END = ATTN_WIDTH + 2 * KV_WIDTH

kernel_name = "hybrid_swa_shortconv_peer_dit_layer"


def rmsnorm(x, g):
    xf = x.astype(jnp.float32)
    y = xf * lax.rsqrt(jnp.mean(xf * xf, axis=-1, keepdims=True) + EPS)
    return (y * g.astype(jnp.float32)).astype(x.dtype)


def adaln_params(cond, w_mod, b_mod):
    m = jax.nn.silu(cond) @ w_mod + b_mod
    return jnp.split(m, N_MOD, axis=-1)


def axial_rope_tables(length):
    rows = length // GRID_W
    row = jnp.repeat(jnp.arange(rows, dtype=jnp.float32), GRID_W)
    col = jnp.tile(jnp.arange(GRID_W, dtype=jnp.float32), rows)
    inv_freq = ROPE_BASE ** (-jnp.arange(ROPE_PAIRS, dtype=jnp.float32) / ROPE_PAIRS)
    ang_r = row[:, None] * inv_freq
    ang_c = col[:, None] * inv_freq
    return (jnp.cos(ang_r), jnp.sin(ang_r), jnp.cos(ang_c), jnp.sin(ang_c))


def _rotate(xp, cos, sin):
    x1, x2 = jnp.split(xp, 2, axis=-1)
    c = cos[:, None, :]
    s = sin[:, None, :]
    return jnp.concatenate([x1 * c - x2 * s, x1 * s + x2 * c], axis=-1)


def apply_axial_rope(x, tables):
    cos_r, sin_r, cos_c, sin_c = tables
    xr, xc = jnp.split(x.astype(jnp.float32), 2, axis=-1)
    out = jnp.concatenate([_rotate(xr, cos_r, sin_r), _rotate(xc, cos_c, sin_c)], axis=-1)
    return out.astype(x.dtype)


def split_proj(z):
    return jnp.split(z, PROJ_SPLITS, axis=-1)


def windowed_attention(q, k, v, k_ctx, v_ctx, sink):
    B, L = q.shape[0], q.shape[1]
    nb = L // BLOCK
    scale = HEAD_DIM ** -0.5
    qb = q.reshape(B, nb, BLOCK, N_KV_HEADS, Q_PER_KV, HEAD_DIM)
    pad = ((0, 0), (BLOCK, BLOCK), (0, 0), (0, 0))
    kp = jnp.pad(k, pad).reshape(B, nb + 2, BLOCK, N_KV_HEADS, HEAD_DIM)
    vp = jnp.pad(v, pad).reshape(B, nb + 2, BLOCK, N_KV_HEADS, HEAD_DIM)
    kb = jnp.concatenate([kp[:, :-2], kp[:, 1:-1], kp[:, 2:]], axis=2)
    vb = jnp.concatenate([vp[:, :-2], vp[:, 1:-1], vp[:, 2:]], axis=2)
    s_loc = jnp.einsum('bnqhgd,bnkhd->bhgnqk', qb, kb, preferred_element_type=jnp.float32) * scale
    s_ctx = jnp.einsum('bnqhgd,bchd->bhgnqc', qb, k_ctx, preferred_element_type=jnp.float32) * scale
    blk = jnp.arange(nb)[:, None]
    qpos = blk * BLOCK + jnp.arange(BLOCK)[None, :]
    kpos = (blk - 1) * BLOCK + jnp.arange(3 * BLOCK)[None, :]
    valid = ((kpos[:, None, :] >= 0) & (kpos[:, None, :] < L)
             & (jnp.abs(qpos[:, :, None] - kpos[:, None, :]) <= WINDOW))
    s_loc = jnp.where(valid, s_loc, NEG_INF)
    sink_l = jnp.broadcast_to(
        sink.astype(jnp.float32).reshape(N_KV_HEADS, Q_PER_KV)[None, :, :, None, None, None],
        s_loc.shape[:-1] + (1,))
    p = jax.nn.softmax(jnp.concatenate([s_loc, s_ctx, sink_l], axis=-1), axis=-1)
    n_loc = 3 * BLOCK
    n_ctx = k_ctx.shape[1]
    p_loc = p[..., :n_loc].astype(v.dtype)
    p_ctx = p[..., n_loc:n_loc + n_ctx].astype(v.dtype)
    out = (jnp.einsum('bhgnqk,bnkhd->bnqhgd', p_loc, vb)
           + jnp.einsum('bhgnqc,bchd->bnqhgd', p_ctx, v_ctx))
    return out.reshape(B, L, ATTN_WIDTH)


def context_attention(q, k, v, sink):
    B, C = q.shape[0], q.shape[1]
    qg = q.reshape(B, C, N_KV_HEADS, Q_PER_KV, HEAD_DIM)
    s = jnp.einsum('bqhgd,bkhd->bhgqk', qg, k, preferred_element_type=jnp.float32) * HEAD_DIM ** -0.5
    sink_l = jnp.broadcast_to(
        sink.astype(jnp.float32).reshape(N_KV_HEADS, Q_PER_KV)[None, :, :, None, None],
        s.shape[:-1] + (1,))
    p = jax.nn.softmax(jnp.concatenate([s, sink_l], axis=-1), axis=-1)[..., :C].astype(v.dtype)
    out = jnp.einsum('bhgqk,bkhd->bqhgd', p, v)
    return out.reshape(B, C, ATTN_WIDTH)


def short_conv(z, w):
    L = z.shape[1]
    zp = jnp.pad(z, ((0, 0), (1, 1), (0, 0)))
    return zp[:, :L] * w[0] + zp[:, 1:L + 1] * w[1] + zp[:, 2:] * w[2]


def conv_branch(gate_b, gate_c, u, conv_w, w_conv_out):
    return (gate_b * short_conv(gate_c * u, conv_w)) @ w_conv_out


def merge_branches(y_attn, y_conv, ga, gc, w_mix_out):
    return (jax.nn.sigmoid(ga) * y_attn + jax.nn.sigmoid(gc) * y_conv) @ w_mix_out


def peer(h, w_q, sub_keys, u_tab, v_tab):
    B, L, D = h.shape
    tok = h.reshape(-1, PEER_CHUNK, D)

    def chunk(t):
        T = t.shape[0]
        qh = (t @ w_q).reshape(T, PEER_HEADS, 2, PEER_KEY_DIM // 2)
        s = jnp.einsum('thpd,hpnd->thpn', qh, sub_keys, preferred_element_type=jnp.float32)
        s_top, i_top = lax.top_k(s, PEER_TOPK)
        cand = s_top[:, :, 0, :, None] + s_top[:, :, 1, None, :]
        cand_idx = i_top[:, :, 0, :, None] * PEER_N_KEYS + i_top[:, :, 1, None, :]
        best, pos = lax.top_k(cand.reshape(T, PEER_HEADS, PEER_TOPK * PEER_TOPK), PEER_TOPK)
        idx = jnp.take_along_axis(cand_idx.reshape(T, PEER_HEADS, PEER_TOPK * PEER_TOPK), pos, axis=-1)
        g = jax.nn.softmax(best, axis=-1)
        ue = jnp.take(u_tab, idx, axis=0)
        ve = jnp.take(v_tab, idx, axis=0)
        act = jax.nn.gelu(jnp.einsum('thkd,td->thk', ue, t, preferred_element_type=jnp.float32),
                          approximate=False)
        return jnp.einsum('thk,thkd->td', (g * act).astype(t.dtype), ve)

    return lax.map(chunk, tok).reshape(B, L, D)


def trunk_layer(x, xc, c, c_ctx, w_mod, b_mod, n1, n2, w_in, sink, conv_w,
                w_attn_out, w_conv_out, w_mix_out, pw_q, p_keys, p_u, p_v, rope, update_ctx):
    B, L, _ = x.shape
    C = xc.shape[1]
    sh1, sc1, g1, sh2, sc2, g2 = [m[:, None, :] for m in adaln_params(c, w_mod, b_mod)]
    csh1, csc1, cg1, csh2, csc2, cg2 = adaln_params(c_ctx, w_mod, b_mod)

    hc = rmsnorm(xc, n1) * (1 + csc1) + csh1
    if update_ctx:
        qc, kc, vc, gbc, gcc, uc, gac, gvc = split_proj(hc @ w_in)
    else:
        kc, vc = jnp.split(hc @ w_in[:, KV_START:KV_END], 2, axis=-1)
    kc = kc.reshape(B, C, N_KV_HEADS, HEAD_DIM)
    vc = vc.reshape(B, C, N_KV_HEADS, HEAD_DIM)

    h = rmsnorm(x, n1) * (1 + sc1) + sh1
    q, k, v, gb, gcv, u, ga, gv = split_proj(h @ w_in)
    q = apply_axial_rope(q.reshape(B, L, N_Q_HEADS, HEAD_DIM), rope)
    k = apply_axial_rope(k.reshape(B, L, N_KV_HEADS, HEAD_DIM), rope)
    v = v.reshape(B, L, N_KV_HEADS, HEAD_DIM)
    y_attn = windowed_attention(q, k, v, kc, vc, sink) @ w_attn_out
    y_conv = conv_branch(gb, gcv, u, conv_w, w_conv_out)
    x = x + g1 * merge_branches(y_attn, y_conv, ga, gv, w_mix_out)
    h2 = rmsnorm(x, n2) * (1 + sc2) + sh2
    x = x + g2 * peer(h2, pw_q, p_keys, p_u, p_v)

    if update_ctx:
        yc_attn = context_attention(qc.reshape(B, C, N_Q_HEADS, HEAD_DIM), kc, vc, sink) @ w_attn_out
        yc_conv = conv_branch(gbc, gcc, uc, conv_w, w_conv_out)
        xc = xc + cg1 * merge_branches(yc_attn, yc_conv, gac, gvc, w_mix_out)
        hc2 = rmsnorm(xc, n2) * (1 + csc2) + csh2
        xc = xc + cg2 * peer(hc2, pw_q, p_keys, p_u, p_v)
    return x, xc


def setup_inputs(seed: int = 0) -> dict:
    key = jax.random.key(seed)
    ks = jax.random.split(key, 19)

    def nrm(k, shape, s):
        return jax.random.normal(k, shape, jnp.float32) * s

    return {
        "x": nrm(ks[0], (BATCH, SEQ, D_MODEL), 1.0),
        "c": nrm(ks[1], (BATCH, D_MODEL), 1.0),
        "ctx": nrm(ks[2], (BATCH, CTX_LEN, D_MODEL), 1.0),
        "c_ctx": nrm(ks[3], (D_MODEL,), 1.0),
        "w_mod": nrm(ks[4], (DEPTH, D_MODEL, N_MOD * D_MODEL), D_MODEL ** -0.5),
        "b_mod": nrm(ks[5], (DEPTH, N_MOD * D_MODEL), 0.02),
        "norm1_g": 1.0 + nrm(ks[6], (DEPTH, D_MODEL), 0.02),
        "norm2_g": 1.0 + nrm(ks[7], (DEPTH, D_MODEL), 0.02),
        "w_in": nrm(ks[8], (DEPTH, D_MODEL, PROJ_WIDTH), D_MODEL ** -0.5),
        "attn_sink": nrm(ks[9], (DEPTH, N_Q_HEADS), 1.0),
        "conv_w": nrm(ks[10], (DEPTH, CONV_K, CONV_WIDTH), CONV_K ** -0.5),
        "w_attn_out": nrm(ks[11], (DEPTH, ATTN_WIDTH, D_MODEL), ATTN_WIDTH ** -0.5),
        "w_conv_out": nrm(ks[12], (DEPTH, CONV_WIDTH, D_MODEL), CONV_WIDTH ** -0.5),
        "w_mix_out": nrm(ks[13], (DEPTH, D_MODEL, D_MODEL), D_MODEL ** -0.5),
        "peer_w_q": nrm(ks[14], (DEPTH, D_MODEL, PEER_HEADS * PEER_KEY_DIM), D_MODEL ** -0.5),
        "peer_sub_keys": nrm(ks[15], (DEPTH, PEER_HEADS, 2, PEER_N_KEYS, PEER_KEY_DIM // 2),
                             (PEER_KEY_DIM // 2) ** -0.5),
        "peer_u": nrm(ks[16], (DEPTH, PEER_N_EXPERTS, D_MODEL), D_MODEL ** -0.5),
        "peer_v": nrm(ks[17], (DEPTH, PEER_N_EXPERTS, D_MODEL), 0.5),
        "final_g": 1.0 + nrm(ks[18], (D_MODEL,), 0.02),
    }


def reference(x, c, ctx, c_ctx, w_mod, b_mod, norm1_g, norm2_g, w_in, attn_sink, conv_w,
              w_attn_out, w_conv_out, w_mix_out, peer_w_q, peer_sub_keys, peer_u, peer_v, final_g):
    rope = axial_rope_tables(x.shape[1])
    xc = ctx
    for l in range(DEPTH):
        x, xc = trunk_layer(x, xc, c, c_ctx, w_mod[l], b_mod[l], norm1_g[l], norm2_g[l], w_in[l],
                            attn_sink[l], conv_w[l], w_attn_out[l], w_conv_out[l], w_mix_out[l],
                            peer_w_q[l], peer_sub_keys[l], peer_u[l], peer_v[l], rope,
                            l < DEPTH - 1)
    return rmsnorm(x, final_g)
```

```python
import numpy as np
from contextlib import ExitStack
import concourse.bass as bass
import concourse.mybir as mybir
from concourse.bass_utils import run_bass_kernel_spmd

F32 = mybir.dt.float32
BF16 = mybir.dt.bfloat16
U32 = mybir.dt.uint32
AF = mybir.ActivationFunctionType
ALU = mybir.AluOpType
AX = mybir.AxisListType

NCORES = 8
L = 2048
TPC = 2 * L
EPS = 1e-6
NEG = -1e30
DEBUG = {"x1": False, "sel": False, "stop_after_mixer": False, "stop": None, "no_prologue": False}


class _Stop(Exception):
    pass


class Res:
    __slots__ = ("name", "w", "r")

    def __init__(self, name):
        self.name = name
        self.w = None
        self.r = {}


class Sched:
    ENG = ("pe", "act", "dve", "pool", "sp")

    def __init__(self, nc, stack, ndma=12):
        self.nc = nc
        self.e = {"pe": nc.tensor, "act": nc.scalar, "dve": nc.vector, "pool": nc.gpsimd, "sp": nc.sync}
        self.sem = {}
        self.cnt = {}
        for k in self.ENG:
            self.sem[k] = stack.enter_context(nc.semaphore("sem_" + k))
            self.cnt[k] = 0
        self.ndma = ndma
        self.dq = {}
        for q in ("sp", "pool"):
            for i in range(ndma):
                self.sem[(q, i)] = stack.enter_context(nc.semaphore(f"dsem_{q}_{i}"))
            self.dq[q] = 0
        self.waited = {k: {} for k in self.ENG}
        self.dead = False

    def _wait(self, eng, tok):
        key, val = tok
        if self.waited[eng].get(key, 0) >= val:
            return
        self.e[eng].wait_ge(self.sem[key], val)
        self.waited[eng][key] = val

    def _deps(self, eng, reads, writes):
        toks = {}

        def add(t):
            if t is None:
                return
            k, v = t
            if toks.get(k, 0) < v:
                toks[k] = v
        for r in reads:
            add(r.w)
        for w in writes:
            add(w.w)
            for k, v in w.r.items():
                add((k, v))
        for k, v in toks.items():
            self._wait(eng, (k, v))

    def _commit(self, tok, reads, writes):
        k, v = tok
        for r in reads:
            if r.r.get(k, 0) < v:
                r.r[k] = v
        for w in writes:
            w.w = tok
            w.r = {}

    def op(self, eng, fn, reads=(), writes=()):
        if self.dead:
            return None
        self._deps(eng, reads, writes)
        ins = fn(self.e[eng])
        self.cnt[eng] += 1
        ins.then_inc(self.sem[eng], 1)
        tok = (eng, self.cnt[eng])
        self._commit(tok, reads, writes)
        return tok

    def dma(self, q, out, in_, reads=(), writes=()):
        if self.dead:
            return None
        n = self.dq[q]
        self.dq[q] += 1
        slot = n % self.ndma
        rnd = n // self.ndma
        key = (q, slot)
        if rnd > 0:
            self._wait(q, (key, 16 * rnd))
        self._deps(q, reads, writes)
        self.e[q].dma_start(out=out, in_=in_).then_inc(self.sem[key], 16)
        tok = (key, 16 * (rnd + 1))
        self._commit(tok, reads, writes)
        return tok

    def barrier(self):
        if self.dead:
            return
        toks = [(k, self.cnt[k]) for k in self.ENG if self.cnt[k] > 0]
        for q, n in self.dq.items():
            for slot in range(min(n, self.ndma)):
                rnd = (n - 1 - slot) // self.ndma
                toks.append(((q, slot), 16 * (rnd + 1)))
        for e in self.ENG:
            for t in toks:
                self._wait(e, t)

    def finish(self, ress):
        for r in ress:
            if r.w is not None:
                self._wait("sp", r.w)


def build():
    nc = bass.Bass("TRN2", target_bir_lowering=False)

    def din(name, shape, dt=F32):
        return nc.dram_tensor(name, list(shape), dt, kind="ExternalInput").ap()

    xT = din("xT", [128, 8, TPC])
    ctxT = din("ctxT", [128, 8, 512])
    cT = din("cT", [128, 8, 3])
    w_mod = din("w_mod", [128, 8, 6144])
    b_mod = din("b_mod", [128, 48])
    n1g = din("n1g", [128, 8])
    n2g = din("n2g", [128, 8])
    fing = din("fing", [128, 8])
    w_in = din("w_in", [39, 128, 8, 128])
    w_ao = din("w_ao", [64, 8, 1024])
    w_co = din("w_co", [128, 4, 1024])
    w_mo = din("w_mo", [128, 8, 1024])
    conv_w = din("conv_w", [128, 4, 3])
    sinkv = din("sinkv", [128, 8])
    pw_q = din("pw_q", [128, 8, 2048])
    keysT = din("keysT", [128, 16, 128])
    uT = din("uT", [128, 128, 1024])
    vt = din("vt", [128, 128, 1024])
    rope_cos = din("rope_cos", [128, L])
    rope_sin = din("rope_sin", [128, L])
    amask = din("amask", [128, 384])
    ident = din("ident", [128, 128])
    iota128 = din("iota128", [128, 128])
    cu32 = din("cu32", [128, 2], U32)
    outT = nc.dram_tensor("outT", [128, 8, TPC], F32, kind="ExternalOutput").ap()
    x1T = nc.dram_tensor("x1T_scr", [128, 8, TPC], F32, kind="Internal").ap()
    uTb = nc.dram_tensor("uTb_scr", [128, 128, 1024], BF16, kind="Internal").ap()
    vtb = nc.dram_tensor("vtb_scr", [128, 128, 1024], BF16, kind="Internal").ap()
    dbg = {}
    if DEBUG["x1"]:
        dbg["x1"] = nc.dram_tensor("dbg_x1", [128, 8, TPC], F32, kind="ExternalOutput").ap()
    if DEBUG["sel"]:
        dbg["sel"] = nc.dram_tensor("dbg_sel", [128, 3, 128], F32, kind="ExternalOutput").ap()

    with ExitStack() as st:
        S = Sched(nc, st)
        NG = 0
        RES = {}

        def R(*key):
            r = RES.get(key)
            if r is None:
                r = RES[key] = Res(str(key))
            return r

        _uid = [0]

        def sb(stack, name, shape, dt):
            _uid[0] += 1
            return stack.enter_context(nc.sbuf_tensor(f"{name}_{_uid[0]}", list(shape), dt))

        def V(fn, reads, writes):
            return S.op("dve", fn, reads, writes)

        def A(fn, reads, writes):
            return S.op("act", fn, reads, writes)

        def G(fn, reads, writes):
            return S.op("pool", fn, reads, writes)

        def PE(fn, reads, writes):
            return S.op("pe", fn, reads, writes)

        psum = st.enter_context(nc.psum_tensor("psum", [128, 8, 512], F32))

        def RB(b):
            return R("psb", b)

        ident_f = sb(st, "ident_f", [128, 128], F32)
        ident_b = sb(st, "ident_b", [128, 128], BF16)
        ones_b = sb(st, "ones_b", [128, 128], BF16)
        zeros_b = sb(st, "zeros_b", [128, 512], BF16)
        iota_t = sb(st, "iota_t", [128, 128], F32)
        cu32_t = sb(st, "cu32_t", [128, 2], U32)
        eps_t = sb(st, "eps_t", [128, 1], F32)
        modp = sb(st, "modp", [128, 48, 3], F32)
        A1 = sb(st, "A1", [128, 8, 3], F32)
        A2 = sb(st, "A2", [128, 8, 3], F32)
        bmod_t = sb(st, "bmod_t", [128, 48], F32)
        n1g_t = sb(st, "n1g_t", [128, 8], F32)
        n2g_t = sb(st, "n2g_t", [128, 8], F32)
        fing_t = sb(st, "fing_t", [128, 8], F32)
        sink_t = sb(st, "sink_t", [128, 8], F32)
        nsink_t = sb(st, "nsink_t", [128, 8], F32)
        convw_t = sb(st, "convw_t", [128, 4, 3], F32)

        S.dma("sp", ident_f[:], ident[:, :], writes=[R("ident_f")])
        S.dma("sp", iota_t[:], iota128[:, :], writes=[R("iota")])
        S.dma("sp", cu32_t[:], cu32[:, :], writes=[R("cu32")])
        S.dma("sp", bmod_t[:], b_mod[:, :], writes=[R("bmod")])
        S.dma("sp", n1g_t[:], n1g[:, :], writes=[R("n1g")])
        S.dma("sp", n2g_t[:], n2g[:, :], writes=[R("n2g")])
        S.dma("sp", fing_t[:], fing[:, :], writes=[R("fing")])
        S.dma("sp", sink_t[:], sinkv[:, :], writes=[R("sink")])
        S.dma("sp", convw_t[:], conv_w[:, :, :], writes=[R("convw")])
        V(lambda e: e.tensor_copy(out=ident_b[:], in_=ident_f[:]), [R("ident_f")], [R("ident_b")])
        V(lambda e: e.memset(ones_b[:], 1.0), [], [R("ones_b")])
        V(lambda e: e.memset(zeros_b[:], 0.0), [], [R("zeros_b")])
        V(lambda e: e.memset(eps_t[:], EPS), [], [R("eps")])
        V(lambda e: e.tensor_scalar(out=nsink_t[:], in0=sink_t[:], scalar1=-1.0, scalar2=None, op0=ALU.mult),
          [R("sink")], [R("nsink")])

        NPB = 2
        pro_stack = st.enter_context(ExitStack())
        pst = [sb(pro_stack, f"pst{i}", [128, 1024], F32) for i in range(NPB)]
        pbf = [sb(pro_stack, f"pbf{i}", [128, 1024], BF16) for i in range(NPB)]
        pro_state = {"k": 0}
        NPRO = 0 if DEBUG["no_prologue"] else 256

        def pro_src_dst(k):
            tab, j = divmod(k, 128)
            if tab == 0:
                return uT[j], uTb[j], R("scr_u", j)
            return vt[j], vtb[j], R("scr_v", j)

        def prologue_step():
            k = pro_state["k"]
            if k >= NPRO + 1:
                return False
            if k < NPRO:
                src, _, _ = pro_src_dst(k)
                S.dma("sp", pst[k % NPB][:], src, writes=[R("pst", k % NPB)])
            k2 = k - 1
            if 0 <= k2 < NPRO:
                _, dst, rr = pro_src_dst(k2)
                sl = k2 % NPB
                G(lambda e: e.tensor_copy(out=pbf[sl][:], in_=pst[sl][:]), [R("pst", sl)], [R("pbf", sl)])
                S.dma("sp", dst, pbf[sl][:], reads=[R("pbf", sl)], writes=[rr])
            pro_state["k"] = k + 1
            return True

        def prologue_steps(n):
            for _ in range(n):
                if not prologue_step():
                    break

        def norm_group(xs, rxs, N, sq, rstd, tmp2, bank, Acol, Bcol, outc, rout, in_place):
            A(lambda e: e.activation(out=sq[:, :, 0:N], in_=xs, func=AF.Square), [rxs], [R("sq")])
            for c in range(8):
                PE(lambda e: e.matmul(psum[:, bank, 0:N], lhsT=ones_b[:], rhs=sq[:, c, 0:N],
                                      start=(c == 0), stop=(c == 7)), [R("sq"), R("ones_b")], [RB(bank)])
            A(lambda e: e.activation(out=rstd[:, 0:N], in_=psum[:, bank, 0:N], func=AF.Sqrt,
                                     scale=1.0 / 1024.0, bias=eps_t[:, 0:1]), [RB(bank), R("eps")], [R("rstd")])
            V(lambda e: e.reciprocal(out=rstd[:, 0:N], in_=rstd[:, 0:N]), [R("rstd")], [R("rstd")])
            for c in range(8):
                if in_place:
                    t = xs[:, c, :]
                    rt = rxs
                else:
                    t = tmp2[c % 2][:, 0:N]
                    rt = R("ntmp", c % 2)
                V(lambda e: e.tensor_tensor(out=t, in0=xs[:, c, :], in1=rstd[:, 0:N], op=ALU.mult),
                  [rxs, R("rstd")], [rt])
                b = Bcol(c) if Bcol is not None else None
                if b is not None:
                    V(lambda e: e.tensor_scalar(out=outc(c), in0=t, scalar1=Acol(c), scalar2=b,
                                                op0=ALU.mult, op1=ALU.add), [rt], [rout])
                else:
                    V(lambda e: e.tensor_scalar(out=outc(c), in0=t, scalar1=Acol(c), scalar2=None,
                                                op0=ALU.mult), [rt], [rout])

        with ExitStack() as p0:
            cTt = sb(p0, "cTt", [128, 8, 3], F32)
            scT = sb(p0, "scT", [128, 8, 3], F32)
            wm = [sb(p0, f"wm{i}", [128, 8, 512], F32) for i in range(2)]
            S.dma("sp", cTt[:], cT[:, :, :], writes=[R("cTt")])
            A(lambda e: e.activation(out=scT[:], in_=cTt[:], func=AF.Silu), [R("cTt")], [R("scT")])
            for pc in range(12):
                b = pc % 2
                S.dma("sp", wm[b][:], w_mod[:, :, pc * 512:(pc + 1) * 512], writes=[R("wm", b)])
                for cc in range(4):
                    j = pc * 4 + cc
                    for kc in range(8):
                        PE(lambda e: e.matmul(psum[:, 0, j * 3:(j + 1) * 3], lhsT=wm[b][:, kc, cc * 128:(cc + 1) * 128],
                                              rhs=scT[:, kc, :], start=(kc == 0), stop=(kc == 7)),
                           [R("wm", b), R("scT")], [RB(0)])
            V(lambda e: e.tensor_tensor(out=modp[:], in0=psum[:, 0, 0:144].rearrange("p (a b) -> p a b", b=3),
                                        in1=bmod_t[:, :].unsqueeze(2).to_broadcast([128, 48, 3]), op=ALU.add),
              [RB(0), R("bmod")], [R("modp")])
            for (Ax, off, gt, rg) in ((A1, 8, n1g_t, "n1g"), (A2, 32, n2g_t, "n2g")):
                V(lambda e: e.tensor_scalar(out=Ax[:], in0=modp[:, off:off + 8, :], scalar1=1.0, scalar2=None,
                                            op0=ALU.add), [R("modp")], [R("A12")])
                V(lambda e: e.tensor_tensor(out=Ax[:], in0=Ax[:], in1=gt[:, :].unsqueeze(2).to_broadcast([128, 8, 3]),
                                            op=ALU.mult), [R("A12"), R(rg)], [R("A12")])
        S.barrier()

        try:
          with ExitStack() as p1:
              NW = 5
              wst = [sb(p1, f"wst{i}", [128, 8, 128], F32) for i in range(NW)]
              wbf = [sb(p1, f"wbf{i}", [128, 8, 128], BF16) for i in range(NW)]
              wcount = [0]

              def load_w(idx):
                  i = wcount[0] % NW
                  wcount[0] += 1
                  S.dma("sp", wst[i][:], w_in[idx], writes=[R("wst", i)])
                  G(lambda e: e.tensor_copy(out=wbf[i][:], in_=wst[i][:]), [R("wst", i)], [R("wbf", i)])
                  return wbf[i], R("wbf", i)

              class WStream:
                  def __init__(self, order):
                      self.order = order
                      self.pos = 0
                      self.q = []

                  def prefetch(self, n):
                      while len(self.q) < n and self.pos < len(self.order):
                          self.q.append(load_w(self.order[self.pos]))
                          self.pos += 1

                  def get(self):
                      self.prefetch(1)
                      return self.q.pop(0)

              hT = sb(p1, "hT", [128, 8, L], BF16)
              hcT = sb(p1, "hcT", [128, 8, 256], BF16)
              attnT = sb(p1, "attnT", [64, 8, L], BF16)
              convo = sb(p1, "convo", [128, 4, L], BF16)
              rstd = sb(p1, "rstd", [128, 512], F32)

              def load_big(dst_fn, src_fn, npieces, parts, rname):
                  for pc in range(npieces):
                      i = wcount[0] % NW
                      wcount[0] += 1
                      stg = wst[i][:].rearrange("p a b -> p (a b)")
                      S.dma("sp", stg[0:parts, :], src_fn(pc), writes=[R("wst", i)])
                      G(lambda e: e.tensor_copy(out=dst_fn(pc), in_=stg[0:parts, :]), [R("wst", i)], [R(rname)])


              for s in range(2):
                  order = [0, 4, 1, 5, 2, 6, 3, 7, 8, 9, 10] + list(range(11, 23))
                  for tg in range(4):
                      for oc in range(8):
                          order += [23 + oc, 31 + oc]
                  ws = WStream(order)
                  ws.prefetch(2)

                  with ExitStack() as psa:
                      xs = sb(psa, "xs", [128, 8, 512], F32)
                      sq = sb(psa, "sq", [128, 8, 512], BF16)
                      for tg in range(4):
                          S.dma("sp", xs[:], xT[:, :, s * L + tg * 512: s * L + (tg + 1) * 512], writes=[R("xs")])
                          norm_group(xs[:], R("xs"), 512, sq, rstd, None, tg % 2,
                                     lambda c: A1[:, c, s:s + 1], lambda c: modp[:, c, s:s + 1],
                                     lambda c: hT[:, c, tg * 512:(tg + 1) * 512], R("hT", tg), True)
                      S.dma("sp", xs[:, :, 0:256], ctxT[:, :, s * 256:(s + 1) * 256], writes=[R("xs")])
                      norm_group(xs[:, :, 0:256], R("xs"), 256, sq, rstd, None, 0,
                                 lambda c: A1[:, c, 2:3], lambda c: modp[:, c, 2:3],
                                 lambda c: hcT[:, c, :], R("hcT"), True)
                      prologue_steps(8)
                  S.barrier()
                  if DEBUG["stop"] == "A":
                      S.dead = True

                  with ExitStack() as pa:
                      qT = sb(pa, "qT", [128, 4, L], BF16)
                      kT = sb(pa, "kT", [128, 256 + L], BF16)
                      vtok = sb(pa, "vtok", [128, 18, 128], BF16)
                      amask_t = sb(pa, "amask_t", [128, 384], F32)
                      pb_ = ExitStack()
                      cos_t = sb(pb_, "cos_t", [128, L], F32)
                      sin_t = sb(pb_, "sin_t", [128, L], F32)
                      t1 = [sb(pb_, f"t1_{i}", [128, 512], F32) for i in range(2)]
                      t2 = [sb(pb_, f"t2_{i}", [128, 512], F32) for i in range(2)]
                      S.dma("sp", cos_t[:], rope_cos[:, :], writes=[R("cos")])
                      S.dma("sp", sin_t[:], rope_sin[:, :], writes=[R("sin")])
                      S.dma("sp", amask_t[:], amask[:, :], writes=[R("amask")])

                      it = 0

                      def proj_rope(wa, ra, wb_, rb_, dst_fn, rdst_fn):
                          nonlocal it
                          for tg in range(4):
                              ba, bb = (2, 3) if it % 2 == 0 else (4, 5)
                              tt1, tt2 = t1[it % 2], t2[it % 2]
                              r1, r2 = R("t1", it % 2), R("t2", it % 2)
                              it += 1
                              for kc in range(8):
                                  PE(lambda e: e.matmul(psum[:, ba, :], lhsT=wa[:, kc, :], rhs=hT[:, kc, tg * 512:(tg + 1) * 512],
                                                        start=(kc == 0), stop=(kc == 7)), [ra, R("hT", tg)], [RB(ba)])
                              for kc in range(8):
                                  PE(lambda e: e.matmul(psum[:, bb, :], lhsT=wb_[:, kc, :], rhs=hT[:, kc, tg * 512:(tg + 1) * 512],
                                                        start=(kc == 0), stop=(kc == 7)), [rb_, R("hT", tg)], [RB(bb)])
                              V(lambda e: e.tensor_tensor(out=tt1[:], in0=psum[:, ba, :], in1=cos_t[:, tg * 512:(tg + 1) * 512],
                                                          op=ALU.mult), [RB(ba), R("cos")], [r1])
                              V(lambda e: e.tensor_tensor(out=tt2[:], in0=psum[:, bb, :], in1=sin_t[:, tg * 512:(tg + 1) * 512],
                                                          op=ALU.mult), [RB(bb), R("sin")], [r2])
                              G(lambda e: e.tensor_tensor(out=dst_fn(tg), in0=tt1[:], in1=tt2[:], op=ALU.add),
                                [r1, r2], [rdst_fn(tg)])

                      for ch in range(4):
                          (wq, rq) = ws.get()
                          (wqs, rqs) = ws.get()
                          ws.prefetch(2)
                          proj_rope(wq, rq, wqs, rqs, lambda tg: qT[:, ch, tg * 512:(tg + 1) * 512],
                                    lambda tg: R("qT", ch, tg))
                          prologue_steps(4)
                      (wk, rk) = ws.get()
                      (wks, rks) = ws.get()
                      ws.prefetch(2)
                      for kc in range(8):
                          PE(lambda e: e.matmul(psum[:, 6, 0:256], lhsT=wk[:, kc, :], rhs=hcT[:, kc, :],
                                                start=(kc == 0), stop=(kc == 7)), [rk, R("hcT")], [RB(6)])
                      A(lambda e: e.activation(out=kT[:, 0:256], in_=psum[:, 6, 0:256], func=AF.Copy), [RB(6)], [R("kT", "ctx")])
                      proj_rope(wk, rk, wks, rks, lambda tg: kT[:, 256 + tg * 512: 256 + (tg + 1) * 512],
                                lambda tg: R("kT", tg))
                      (wv, rv) = ws.get()
                      ws.prefetch(3)
                      for blk in range(18):
                          bank = 6 + blk % 2
                          for kc in range(8):
                              if blk < 2:
                                  lh = hcT[:, kc, blk * 128:(blk + 1) * 128]
                                  rl = R("hcT")
                              else:
                                  lh = hT[:, kc, (blk - 2) * 128:(blk - 1) * 128]
                                  rl = R("hT", (blk - 2) // 4)
                              PE(lambda e: e.matmul(psum[:, bank, 0:128], lhsT=lh, rhs=wv[:, kc, :],
                                                    start=(kc == 0), stop=(kc == 7)), [rl, rv], [RB(bank)])
                          A(lambda e: e.activation(out=vtok[:, blk, :], in_=psum[:, bank, 0:128], func=AF.Copy),
                            [RB(bank)], [R("vtok", blk)])
                      prologue_steps(8)
                      pb_.close()
                      S.barrier()
                      if DEBUG["stop"] == "B":
                          S.dead = True

                      with ExitStack() as pc_:
                          sc = [sb(pc_, f"sc{i}", [128, 640], F32) for i in range(2)]
                          Pm = [sb(pc_, f"Pm{i}", [128, 640], BF16) for i in range(2)]
                          sm = [sb(pc_, f"sm{i}", [128, 8], F32) for i in range(2)]
                          dg = [sb(pc_, f"dg{i}", [128, 128], BF16) for i in range(2)]
                          PTs = [sb(pc_, f"PTs{i}", [128, 5, 4, 128], BF16) for i in range(2)]
                          hcnt = 0
                          pvc = 0
                          for n in range(16):
                              lo = max(n - 1, 0)
                              hi = min(n + 1, 15)
                              nlb = hi - lo + 1
                              nloc = nlb * 128
                              nk = nloc + 256
                              nkb = nlb + 2
                              moff = (lo - (n - 1)) * 128
                              krs = [R("kT", "ctx")] + [R("kT", t) for t in sorted(set([(lo * 128) // 512, (hi * 128 + 127) // 512]))]
                              for g in range(2):
                                  psl = slice(g * 64, (g + 1) * 64)
                                  pts = PTs[pvc % 2]
                                  rpts = R("PTs", pvc % 2)
                                  for c in range(4):
                                      hq = g * 4 + c
                                      i2 = hcnt % 2
                                      sa, sbk = (0, 1) if i2 == 0 else (2, 3)
                                      hcnt += 1
                                      scx, Px, smx, dgx = sc[i2], Pm[i2], sm[i2], dg[i2]
                                      rsc, rP, rsm, rdg = R("sc", i2), R("Pm", i2), R("sm", i2), R("dg", i2)
                                      lq = qT[psl, c, n * 128:(n + 1) * 128]
                                      PE(lambda e: e.matmul(psum[:, sa, 0:nloc], lhsT=lq,
                                                            rhs=kT[psl, 256 + lo * 128: 256 + (hi + 1) * 128],
                                                            start=True, stop=True), [R("qT", c, n // 4)] + krs, [RB(sa)])
                                      PE(lambda e: e.matmul(psum[:, sbk, 0:256], lhsT=lq, rhs=kT[psl, 0:256],
                                                            start=True, stop=True), [R("qT", c, n // 4)] + krs, [RB(sbk)])
                                      V(lambda e: e.tensor_tensor(out=scx[:, 0:nloc], in0=psum[:, sa, 0:nloc],
                                                                  in1=amask_t[:, moff:moff + nloc], op=ALU.add),
                                        [RB(sa), R("amask")], [rsc])
                                      A(lambda e: e.activation(out=scx[:, nloc:nk], in_=psum[:, sbk, 0:256], func=AF.Copy),
                                        [RB(sbk)], [rsc])
                                      V(lambda e: e.reduce_max(out=smx[:, 0:1], in_=scx[:, 0:nk], axis=AX.X), [rsc], [rsm])
                                      V(lambda e: e.tensor_scalar(out=smx[:, 1:2], in0=smx[:, 0:1], scalar1=-0.125,
                                                                  scalar2=nsink_t[:, hq:hq + 1], op0=ALU.mult, op1=ALU.min),
                                        [rsm, R("nsink")], [rsm])
                                      A(lambda e: e.activation(out=Px[:, 0:nk], in_=scx[:, 0:nk], func=AF.Exp, scale=0.125,
                                                               bias=smx[:, 1:2], accum_out=smx[:, 2:3]), [rsc, rsm], [rP, rsm])
                                      A(lambda e: e.activation(out=smx[:, 3:4], in_=sink_t[:, hq:hq + 1], func=AF.Exp, scale=1.0,
                                                               bias=smx[:, 1:2]), [rsm, R("sink")], [rsm])
                                      V(lambda e: e.tensor_tensor(out=smx[:, 4:5], in0=smx[:, 2:3], in1=smx[:, 3:4], op=ALU.add),
                                        [rsm], [rsm])
                                      V(lambda e: e.reciprocal(out=smx[:, 5:6], in_=smx[:, 4:5]), [rsm], [rsm])
                                      V(lambda e: e.tensor_scalar(out=dgx[:], in0=ident_b[:], scalar1=smx[:, 5:6], scalar2=None,
                                                                  op0=ALU.mult), [rsm, R("ident_b")], [rdg])
                                      for kb in range(nkb):
                                          bank = 4 + kb // 4
                                          off = (kb % 4) * 128
                                          PE(lambda e: e.matmul(psum[:, bank, off:off + 128], lhsT=Px[:, kb * 128:(kb + 1) * 128],
                                                                rhs=dgx[:], start=True, stop=True), [rP, rdg], [RB(bank)])
                                      A(lambda e: e.activation(out=pts[:, 0:4, c, :],
                                                               in_=psum[:, 4, :].rearrange("p (k q) -> p k q", q=128),
                                                               func=AF.Copy), [RB(4)], [rpts])
                                      if nkb == 5:
                                          V(lambda e: e.tensor_copy(out=pts[:, 4, c, :], in_=psum[:, 5, 0:128]), [RB(5)], [rpts])
                                  ob = 6 + pvc % 2
                                  pvc += 1
                                  for kb in range(nkb):
                                      blk = (2 + lo + kb) if kb < nlb else (kb - nlb)
                                      PE(lambda e: e.matmul(psum[0:64, ob, :], lhsT=vtok[:, blk, g * 64:(g + 1) * 64],
                                                            rhs=pts[:, kb, :, :].rearrange("p c q -> p (c q)"),
                                                            start=(kb == 0), stop=(kb == nkb - 1)),
                                         [R("vtok", blk), rpts], [RB(ob)])
                                  A(lambda e: e.activation(out=attnT[:, g * 4:(g + 1) * 4, n * 128:(n + 1) * 128],
                                                           in_=psum[0:64, ob, :].rearrange("p (c q) -> p c q", q=128),
                                                           func=AF.Copy), [RB(ob)], [R("attnT", n // 4)])
                              prologue_steps(3)
                          S.barrier()
                  S.barrier()
                  if DEBUG["stop"] == "C":
                      S.dead = True

                  with ExitStack() as pd:
                      cu = sb(pd, "cu", [128, L + 2], F32)
                      yc = sb(pd, "yc", [128, L], F32)
                      gbs = sb(pd, "gbs", [128, L], F32)
                      ut = [sb(pd, f"ut{i}", [128, 512], F32) for i in range(2)]
                      G(lambda e: e.memset(cu[:, 0:1], 0.0), [], [R("cu_pad")])
                      G(lambda e: e.memset(cu[:, L + 1:L + 2], 0.0), [], [R("cu_pad")])
                      cnt = 0
                      for c in range(4):
                          (wg, rg) = ws.get()
                          (wu, ru) = ws.get()
                          (wb_, rb_) = ws.get()
                          ws.prefetch(2)
                          for tg in range(4):
                              bk = (0, 1, 2) if cnt % 2 == 0 else (3, 4, 5)
                              utx = ut[cnt % 2]
                              rut = R("ut", cnt % 2)
                              cnt += 1
                              for (w_, r_, b_) in ((wg, rg, bk[0]), (wu, ru, bk[1]), (wb_, rb_, bk[2])):
                                  for kc in range(8):
                                      PE(lambda e: e.matmul(psum[:, b_, :], lhsT=w_[:, kc, :], rhs=hT[:, kc, tg * 512:(tg + 1) * 512],
                                                            start=(kc == 0), stop=(kc == 7)), [r_, R("hT", tg)], [RB(b_)])
                              A(lambda e: e.activation(out=utx[:], in_=psum[:, bk[1], :], func=AF.Copy), [RB(bk[1])], [rut])
                              V(lambda e: e.tensor_tensor(out=cu[:, 1 + tg * 512: 1 + (tg + 1) * 512], in0=psum[:, bk[0], :],
                                                          in1=utx[:], op=ALU.mult), [RB(bk[0]), rut], [R("cu", tg)])
                              A(lambda e: e.activation(out=gbs[:, tg * 512:(tg + 1) * 512], in_=psum[:, bk[2], :], func=AF.Copy),
                                [RB(bk[2])], [R("gbs", tg)])
                          cur = [R("cu", t) for t in range(4)] + [R("cu_pad")]
                          V(lambda e: e.tensor_scalar(out=yc[:], in0=cu[:, 0:L], scalar1=convw_t[:, c, 0:1], scalar2=None,
                                                      op0=ALU.mult), cur + [R("convw")], [R("yc")])
                          V(lambda e: e.scalar_tensor_tensor(out=yc[:], in0=cu[:, 1:L + 1], scalar=convw_t[:, c, 1:2], in1=yc[:],
                                                             op0=ALU.mult, op1=ALU.add), cur + [R("yc")], [R("yc")])
                          V(lambda e: e.scalar_tensor_tensor(out=yc[:], in0=cu[:, 2:L + 2], scalar=convw_t[:, c, 2:3], in1=yc[:],
                                                             op0=ALU.mult, op1=ALU.add), cur + [R("yc")], [R("yc")])
                          G(lambda e: e.tensor_tensor(out=convo[:, c, :], in0=yc[:], in1=gbs[:], op=ALU.mult),
                            [R("yc")] + [R("gbs", t) for t in range(4)], [R("convo")])
                          prologue_steps(4)
                  S.barrier()
                  if DEBUG["stop"] == "D":
                      S.dead = True

                  with ExitStack() as pe_:
                      xs = sb(pe_, "xs", [128, 8, 512], F32)
                      w_ao_b = sb(pe_, "w_ao_b", [64, 8, 1024], BF16)
                      w_co_b = sb(pe_, "w_co_b", [128, 4, 1024], BF16)
                      w_mo_b = sb(pe_, "w_mo_b", [128, 8, 1024], BF16)
                      load_big(lambda pc: w_ao_b[:, pc, :], lambda pc: w_ao[:, pc, :], 8, 64, "w_ao_b")
                      load_big(lambda pc: w_co_b[:, pc, :], lambda pc: w_co[:, pc, :], 4, 128, "w_co_b")
                      load_big(lambda pc: w_mo_b[:, pc, :], lambda pc: w_mo[:, pc, :], 8, 128, "w_mo_b")
                      mixT = sb(pe_, "mixT", [128, 8, 512], BF16)
                      sg = [sb(pe_, f"sg{i}", [128, 512], F32) for i in range(2)]
                      mm_ = [sb(pe_, f"mm{i}", [128, 512], F32) for i in range(2)]
                      for tg in range(4):
                          tsl = slice(tg * 512, (tg + 1) * 512)
                          S.dma("sp", xs[:], xT[:, :, s * L + tg * 512: s * L + (tg + 1) * 512], writes=[R("xs")])
                          for oc in range(8):
                              (wga, rga) = ws.get()
                              (wgv, rgv) = ws.get()
                              ws.prefetch(2)
                              osl = slice(oc * 128, (oc + 1) * 128)
                              for h in range(8):
                                  PE(lambda e: e.matmul(psum[:, 0, :], lhsT=w_ao_b[:, h, osl], rhs=attnT[:, h, tsl],
                                                        start=(h == 0), stop=(h == 7)), [R("w_ao_b"), R("attnT", tg)], [RB(0)])
                              for c in range(4):
                                  PE(lambda e: e.matmul(psum[:, 1, :], lhsT=w_co_b[:, c, osl], rhs=convo[:, c, tsl],
                                                        start=(c == 0), stop=(c == 3)), [R("w_co_b"), R("convo")], [RB(1)])
                              for kc in range(8):
                                  PE(lambda e: e.matmul(psum[:, 2, :], lhsT=wga[:, kc, :], rhs=hT[:, kc, tsl],
                                                        start=(kc == 0), stop=(kc == 7)), [rga, R("hT", tg)], [RB(2)])
                              for kc in range(8):
                                  PE(lambda e: e.matmul(psum[:, 3, :], lhsT=wgv[:, kc, :], rhs=hT[:, kc, tsl],
                                                        start=(kc == 0), stop=(kc == 7)), [rgv, R("hT", tg)], [RB(3)])
                              A(lambda e: e.activation(out=sg[0][:], in_=psum[:, 2, :], func=AF.Sigmoid), [RB(2)], [R("sg", 0)])
                              A(lambda e: e.activation(out=sg[1][:], in_=psum[:, 3, :], func=AF.Sigmoid), [RB(3)], [R("sg", 1)])
                              V(lambda e: e.tensor_tensor(out=mm_[0][:], in0=psum[:, 0, :], in1=sg[0][:], op=ALU.mult),
                                [RB(0), R("sg", 0)], [R("mm", 0)])
                              V(lambda e: e.tensor_tensor(out=mm_[1][:], in0=psum[:, 1, :], in1=sg[1][:], op=ALU.mult),
                                [RB(1), R("sg", 1)], [R("mm", 1)])
                              G(lambda e: e.tensor_tensor(out=mixT[:, oc, :], in0=mm_[0][:], in1=mm_[1][:], op=ALU.add),
                                [R("mm", 0), R("mm", 1)], [R("mixT")])
                          for oc in range(8):
                              ob = 4 + oc % 2
                              osl = slice(oc * 128, (oc + 1) * 128)
                              for c in range(8):
                                  PE(lambda e: e.matmul(psum[:, ob, :], lhsT=w_mo_b[:, c, osl], rhs=mixT[:, c, :],
                                                        start=(c == 0), stop=(c == 7)), [R("w_mo_b"), R("mixT")], [RB(ob)])
                              V(lambda e: e.scalar_tensor_tensor(out=xs[:, oc, :], in0=psum[:, ob, :],
                                                                 scalar=modp[:, 16 + oc, s:s + 1], in1=xs[:, oc, :],
                                                                 op0=ALU.mult, op1=ALU.add), [RB(ob), R("xs"), R("modp")], [R("xs")])
                          S.dma("sp", x1T[:, :, s * L + tg * 512: s * L + (tg + 1) * 512], xs[:], reads=[R("xs")],
                                writes=[R("x1T", s * 4 + tg)])
                          if DEBUG["x1"]:
                              S.dma("sp", dbg["x1"][:, :, s * L + tg * 512: s * L + (tg + 1) * 512], xs[:], reads=[R("xs")],
                                    writes=[R("dbg_x1")])
                          prologue_steps(6)
                  S.barrier()

              while prologue_step():
                  pass
          pro_stack.close()
          S.barrier()

          TG = 256
          NG = TPC // TG
          if DEBUG["stop_after_mixer"]:
              NG = 0
          with ExitStack() as p2:
              pwq_b = sb(p2, "pwq_b", [128, 8, 2048], BF16)
              keys_b = sb(p2, "keys_b", [128, 16, 128], BF16)
              x1g = sb(p2, "x1g", [128, 8, TG], F32)
              sq2 = sb(p2, "sq2", [128, 8, TG], BF16)
              rstd2 = sb(p2, "rstd2", [128, TG], F32)
              ntmp = [sb(p2, f"ntmp{i}", [128, TG], F32) for i in range(2)]
              h2T = sb(p2, "h2T", [128, 8, TG], BF16)
              qpT = sb(p2, "qpT", [128, 16, TG], BF16)
              s1 = sb(p2, "s1", [128, 16, 128], F32)
              m16 = sb(p2, "m16", [128, 16, 16], F32)
              ix = sb(p2, "ix", [128, 16, 16], U32)
              ixf = sb(p2, "ixf", [128, 16, 16], F32)
              cand = sb(p2, "cand", [128, 8, 256], F32)
              b16 = sb(p2, "b16", [128, 8, 16], F32)
              pos = sb(p2, "pos", [128, 8, 16], U32)
              pab = sb(p2, "pab", [128, 2, 128], U32)
              pabf = sb(p2, "pabf", [128, 2, 128], F32)
              ee = sb(p2, "ee", [128, 8, 16], F32)
              zs = sb(p2, "zs", [128, 8], F32)
              IJG = sb(p2, "IJG", [128, 3, 128], F32)
              IJGT = sb(p2, "IJGT", [128, 3, TG], F32)
              NB = 16
              Pmx = [sb(p2, f"Pmx{i}", [128, NB, 128], BF16) for i in range(2)]
              Qmx = [sb(p2, f"Qmx{i}", [128, NB, 128], BF16) for i in range(2)]
              Wsum = sb(p2, "Wsum", [128, 128, TG], BF16)
              NS = 3
              ub = [sb(p2, f"ub{i}", [128, 8, 128], BF16) for i in range(NS)]
              vb = [sb(p2, f"vb{i}", [128, 1024], BF16) for i in range(NS)]
              ge = [sb(p2, f"ge{i}", [128, TG], F32) for i in range(2)]
              Lj = [sb(p2, f"Lj{i}", [128, TG], BF16) for i in range(2)]

              with ExitStack() as pl:
                  stg = sb(pl, "stg", [128, 2048], F32)
                  for kc in range(8):
                      S.dma("sp", stg[:], pw_q[:, kc, :], writes=[R("stg")])
                      G(lambda e: e.tensor_copy(out=pwq_b[:, kc, :], in_=stg[:]), [R("stg")], [R("pwq_b")])
                  S.dma("sp", stg[:], keysT[:, :, :].rearrange("p a b -> p (a b)"), writes=[R("stg")])
                  G(lambda e: e.tensor_copy(out=keys_b[:].rearrange("p a b -> p (a b)"), in_=stg[:]), [R("stg")], [R("keys_b")])
              S.barrier()

              scnt = 0
              acnt = 0
              for gi in range(NG):
                  s = gi // (NG // 2) if NG >= 2 else 0
                  t0 = gi * TG
                  S.dma("sp", x1g[:], x1T[:, :, t0:t0 + TG], reads=[R("x1T", t0 // 512)], writes=[R("x1g")])
                  norm_group(x1g[:], R("x1g"), TG, sq2, rstd2, ntmp, 4,
                             lambda c: A2[:, c, s:s + 1], lambda c: modp[:, 24 + c, s:s + 1],
                             lambda c: h2T[:, c, :], R("h2T"), False)
                  for hp in range(16):
                      bank = 4 + (hp // 2) % 4
                      off = (hp % 2) * TG
                      for kc in range(8):
                          PE(lambda e: e.matmul(psum[:, bank, off:off + TG], lhsT=pwq_b[:, kc, hp * 128:(hp + 1) * 128],
                                                rhs=h2T[:, kc, :], start=(kc == 0), stop=(kc == 7)),
                             [R("pwq_b"), R("h2T")], [RB(bank)])
                      if hp % 2 == 1:
                          A(lambda e: e.activation(out=qpT[:, hp - 1:hp + 1, :],
                                                   in_=psum[:, bank, :].rearrange("p (a t) -> p a t", t=TG), func=AF.Copy),
                            [RB(bank)], [R("qpT")])
                  for tt in range(TG // 128):
                      tsl = slice(tt * 128, (tt + 1) * 128)
                      for hp in range(16):
                          bank = 4 + hp // 4
                          off = (hp % 4) * 128
                          PE(lambda e: e.matmul(psum[:, bank, off:off + 128], lhsT=qpT[:, hp, tsl], rhs=keys_b[:, hp, :],
                                                start=True, stop=True), [R("qpT"), R("keys_b")], [RB(bank)])
                      A(lambda e: e.activation(out=s1[:].rearrange("p a b -> p (a b)"),
                                               in_=psum[:, 4:8, :].rearrange("p a b -> p (a b)"), func=AF.Copy),
                        [RB(4), RB(5), RB(6), RB(7)], [R("s1")])
                      for hp in range(16):
                          V(lambda e: e.max(out=m16[:, hp, 0:8], in_=s1[:, hp, :]), [R("s1")], [R("m16")])
                          V(lambda e: e.max_index(out=ix[:, hp, 0:8], in_max=m16[:, hp, 0:8], in_values=s1[:, hp, :]),
                            [R("s1"), R("m16")], [R("ix")])
                          V(lambda e: e.match_replace(out=s1[:, hp, :], in_to_replace=m16[:, hp, 0:8], in_values=s1[:, hp, :],
                                                      imm_value=NEG), [R("s1"), R("m16")], [R("s1")])
                          V(lambda e: e.max(out=m16[:, hp, 8:16], in_=s1[:, hp, :]), [R("s1")], [R("m16")])
                          V(lambda e: e.max_index(out=ix[:, hp, 8:16], in_max=m16[:, hp, 8:16], in_values=s1[:, hp, :]),
                            [R("s1"), R("m16")], [R("ix")])
                      V(lambda e: e.tensor_copy(out=ixf[:], in_=ix[:]), [R("ix")], [R("ixf")])
                      m4 = m16[:].rearrange("p (h two) k -> p h two k", two=2)
                      i4 = ixf[:].rearrange("p (h two) k -> p h two k", two=2)
                      V(lambda e: e.tensor_tensor(out=cand[:].rearrange("p h (a b) -> p h a b", b=16),
                                                  in0=m4[:, :, 0, :].unsqueeze(3).to_broadcast([128, 8, 16, 16]),
                                                  in1=m4[:, :, 1, :].unsqueeze(2).to_broadcast([128, 8, 16, 16]), op=ALU.add),
                        [R("m16")], [R("cand")])
                      for h in range(8):
                          V(lambda e: e.max(out=b16[:, h, 0:8], in_=cand[:, h, :]), [R("cand")], [R("b16")])
                          V(lambda e: e.max_index(out=pos[:, h, 0:8], in_max=b16[:, h, 0:8], in_values=cand[:, h, :]),
                            [R("cand"), R("b16")], [R("pos")])
                          V(lambda e: e.match_replace(out=cand[:, h, :], in_to_replace=b16[:, h, 0:8], in_values=cand[:, h, :],
                                                      imm_value=NEG), [R("cand"), R("b16")], [R("cand")])
                          V(lambda e: e.max(out=b16[:, h, 8:16], in_=cand[:, h, :]), [R("cand")], [R("b16")])
                          V(lambda e: e.max_index(out=pos[:, h, 8:16], in_max=b16[:, h, 8:16], in_values=cand[:, h, :]),
                            [R("cand"), R("b16")], [R("pos")])
                      V(lambda e: e.tensor_tensor(out=ee[:], in0=b16[:], in1=b16[:, :, 0:1].to_broadcast([128, 8, 16]),
                                                  op=ALU.subtract), [R("b16")], [R("ee")])
                      A(lambda e: e.activation(out=ee[:], in_=ee[:], func=AF.Exp), [R("ee")], [R("ee")])
                      V(lambda e: e.reduce_sum(out=zs[:], in_=ee[:], axis=AX.X), [R("ee")], [R("zs")])
                      V(lambda e: e.reciprocal(out=zs[:], in_=zs[:]), [R("zs")], [R("zs")])
                      V(lambda e: e.tensor_tensor(out=IJG[:, 2, :].rearrange("p (h k) -> p h k", k=16), in0=ee[:],
                                                  in1=zs[:, :].unsqueeze(2).to_broadcast([128, 8, 16]), op=ALU.mult),
                        [R("ee"), R("zs")], [R("IJG")])
                      posf = pos[:].rearrange("p h k -> p (h k)")
                      V(lambda e: e.tensor_scalar(out=pab[:, 0, :], in0=posf, scalar1=cu32_t[:, 0:1], scalar2=None,
                                                  op0=ALU.logical_shift_right), [R("pos"), R("cu32")], [R("pab")])
                      V(lambda e: e.tensor_scalar(out=pab[:, 1, :], in0=posf, scalar1=cu32_t[:, 1:2], scalar2=None,
                                                  op0=ALU.bitwise_and), [R("pos"), R("cu32")], [R("pab")])
                      V(lambda e: e.tensor_copy(out=pabf[:], in_=pab[:]), [R("pab")], [R("pabf")])
                      eq = cand[:].rearrange("p h (a b) -> p h a b", b=16)
                      io = iota_t[:, 0:16].unsqueeze(1).unsqueeze(1).to_broadcast([128, 8, 16, 16])
                      for w in range(2):
                          sel = pabf[:, w, :].rearrange("p (h k) -> p h k", k=16).unsqueeze(3).to_broadcast([128, 8, 16, 16])
                          V(lambda e: e.tensor_tensor(out=eq, in0=sel, in1=io, op=ALU.is_equal),
                            [R("pabf"), R("iota"), R("cand")], [R("cand")])
                          V(lambda e: e.tensor_tensor(out=eq, in0=eq, in1=i4[:, :, w, :].unsqueeze(2).to_broadcast([128, 8, 16, 16]),
                                                      op=ALU.mult), [R("cand"), R("ixf")], [R("cand")])
                          V(lambda e: e.tensor_reduce(out=IJG[:, w, :].rearrange("p (h k) -> p h k", k=16), in_=eq,
                                                      axis=AX.X, op=ALU.add), [R("cand")], [R("IJG")])
                      if DEBUG["sel"] and gi == 0 and tt == 0:
                          S.dma("sp", dbg["sel"][:, :, :], IJG[:], reads=[R("IJG")], writes=[R("dbg_sel")])
                      for w in range(3):
                          PE(lambda e: e.transpose(out=psum[:, 4, w * 128:(w + 1) * 128], in_=IJG[:, w, :], identity=ident_f[:]),
                             [R("IJG"), R("ident_f")], [RB(4)])
                      A(lambda e: e.activation(out=IJGT[:, :, tsl], in_=psum[:, 4, 0:384].rearrange("p (w t) -> p w t", t=128),
                                               func=AF.Copy), [RB(4)], [R("IJGT")])
                      for bt in range(128 // NB):
                          i2 = scnt % 2
                          scnt += 1
                          Px_, Qx_ = Pmx[i2], Qmx[i2]
                          rPx, rQx = R("Pmx", i2), R("Qmx", i2)
                          wb0 = 4 * (i2)
                          for tl in range(NB):
                              t = tt * 128 + bt * NB + tl
                              V(lambda e: e.tensor_scalar(out=Px_[:, tl, :], in0=iota_t[:], scalar1=IJGT[:, 0, t:t + 1],
                                                          scalar2=None, op0=ALU.is_equal), [R("IJGT"), R("iota")], [rPx])
                              V(lambda e: e.tensor_scalar(out=Qx_[:, tl, :], in0=iota_t[:], scalar1=IJGT[:, 1, t:t + 1],
                                                          scalar2=IJGT[:, 2, t:t + 1], op0=ALU.is_equal, op1=ALU.mult),
                                [R("IJGT"), R("iota")], [rQx])
                          for tl in range(NB):
                              bank = 4 + tl // 4
                              off = (tl % 4) * 128
                              PE(lambda e: e.matmul(psum[:, bank, off:off + 128], lhsT=Px_[:, tl, :], rhs=Qx_[:, tl, :],
                                                    start=True, stop=True), [rPx, rQx], [RB(bank)])
                          tb = tt * 128 + bt * NB
                          A(lambda e: e.activation(out=Wsum[:, :, tb:tb + NB],
                                                   in_=psum[:, 4:8, :].rearrange("p a (t j) -> p j (a t)", j=128),
                                                   func=AF.Copy), [RB(4), RB(5), RB(6), RB(7)], [R("Wsum")])
                  for b in range(4):
                      PE(lambda e: e.matmul(psum[:, b, :], lhsT=zeros_b[:, 0:128], rhs=zeros_b[:], start=True, stop=False,
                                            skip_group_check=True), [R("zeros_b")], [RB(b)])
                  for j in range(128):
                      sl = acnt % NS
                      i2 = acnt % 2
                      acnt += 1
                      S.dma("sp", ub[sl][:].rearrange("p a b -> p (a b)"), uTb[j], reads=[R("scr_u", j)], writes=[R("ub", sl)])
                      S.dma("sp", vb[sl][:], vtb[j], reads=[R("scr_v", j)], writes=[R("vb", sl)])
                      ab = 4 + i2
                      for dc in range(8):
                          PE(lambda e: e.matmul(psum[:, ab, 0:TG], lhsT=ub[sl][:, dc, :], rhs=h2T[:, dc, :],
                                                start=(dc == 0), stop=(dc == 7)), [R("ub", sl), R("h2T")], [RB(ab)])
                      A(lambda e: e.activation(out=ge[i2][:], in_=psum[:, ab, 0:TG], func=AF.Gelu), [RB(ab)], [R("ge", i2)])
                      V(lambda e: e.tensor_tensor(out=Lj[i2][:], in0=ge[i2][:], in1=Wsum[:, j, :], op=ALU.mult),
                        [R("ge", i2), R("Wsum")], [R("Lj", i2)])
                      for dc in range(8):
                          PE(lambda e: e.matmul(psum[:, dc // 2, (dc % 2) * TG:(dc % 2 + 1) * TG],
                                                lhsT=vb[sl][:, dc * 128:(dc + 1) * 128], rhs=Lj[i2][:],
                                                start=False, stop=(j == 127), skip_group_check=True),
                             [R("vb", sl), R("Lj", i2)], [RB(dc // 2)])
                  for dc in range(8):
                      V(lambda e: e.scalar_tensor_tensor(out=x1g[:, dc, :], in0=psum[:, dc // 2, (dc % 2) * TG:(dc % 2 + 1) * TG],
                                                         scalar=modp[:, 40 + dc, s:s + 1], in1=x1g[:, dc, :],
                                                         op0=ALU.mult, op1=ALU.add), [RB(dc // 2), R("x1g"), R("modp")], [R("x1g")])
                  norm_group(x1g[:], R("x1g"), TG, sq2, rstd2, None, 4,
                             lambda c: fing_t[:, c:c + 1], None,
                             lambda c: x1g[:, c, :], R("x1g"), True)
                  S.dma("sp", outT[:, :, t0:t0 + TG], x1g[:], reads=[R("x1g")], writes=[R("outT", gi)])
        except _Stop:
            pass
        build.stats = dict(S.cnt); build.stats["dma"] = dict(S.dq)
        S.finish([R("outT", gi) for gi in range(NG)] + ([R("dbg_x1")] if DEBUG["x1"] else [])
                 + ([R("dbg_sel")] if DEBUG["sel"] else []) + [R("x1T", i) for i in range(8)])
    return nc


def _fm(a):
    rows, D = a.shape
    return np.ascontiguousarray(a.T.reshape(D // 128, 128, rows).transpose(1, 0, 2))


def _partner(d):
    return d + 16 if (d % 32) < 16 else d - 16


def prep_shared(inp):
    f = np.float32
    sh = {}
    w_mod = np.asarray(inp["w_mod"], f)[0]
    sh["w_mod"] = np.ascontiguousarray(w_mod.reshape(8, 128, 6144).transpose(1, 0, 2))
    sh["b_mod"] = np.ascontiguousarray(np.asarray(inp["b_mod"], f)[0].reshape(48, 128).T)
    sh["n1g"] = np.ascontiguousarray(np.asarray(inp["norm1_g"], f)[0].reshape(8, 128).T)
    sh["n2g"] = np.ascontiguousarray(np.asarray(inp["norm2_g"], f)[0].reshape(8, 128).T)
    sh["fing"] = np.ascontiguousarray(np.asarray(inp["final_g"], f).reshape(8, 128).T)
    w_in = np.asarray(inp["w_in"], f)[0]
    pr = np.array([_partner(d) for d in range(64)])
    chunks = []
    for c in range(4):
        chunks.append(np.concatenate([c * 64 + np.arange(64), (4 + c) * 64 + np.arange(64)]))
    for c in range(4):
        chunks.append(np.concatenate([c * 64 + pr, (4 + c) * 64 + pr]))
    chunks.append(512 + np.arange(128))
    chunks.append(512 + np.concatenate([pr, 64 + pr]))
    chunks.append(640 + np.arange(128))
    GB, GC, UU, GA, GV = 768, 1280, 1792, 2304, 3328
    for c in range(4):
        chunks.append(GC + c * 128 + np.arange(128))
        chunks.append(UU + c * 128 + np.arange(128))
        chunks.append(GB + c * 128 + np.arange(128))
    for c in range(8):
        chunks.append(GA + c * 128 + np.arange(128))
    for c in range(8):
        chunks.append(GV + c * 128 + np.arange(128))
    assert len(chunks) == 39
    wl = np.empty((39, 128, 8, 128), f)
    for i, cols in enumerate(chunks):
        wl[i] = w_in[:, cols].reshape(8, 128, 128).transpose(1, 0, 2)
    sh["w_in"] = wl
    sh["w_ao"] = np.ascontiguousarray(np.asarray(inp["w_attn_out"], f)[0].reshape(8, 64, 1024).transpose(1, 0, 2))
    sh["w_co"] = np.ascontiguousarray(np.asarray(inp["w_conv_out"], f)[0].reshape(4, 128, 1024).transpose(1, 0, 2))
    sh["w_mo"] = np.ascontiguousarray(np.asarray(inp["w_mix_out"], f)[0].reshape(8, 128, 1024).transpose(1, 0, 2))
    sh["conv_w"] = np.ascontiguousarray(np.asarray(inp["conv_w"], f)[0].reshape(3, 4, 128).transpose(2, 1, 0))
    sh["sinkv"] = np.ascontiguousarray(np.broadcast_to(np.asarray(inp["attn_sink"], f)[0][None, :], (128, 8)))
    sh["pw_q"] = np.ascontiguousarray(np.asarray(inp["peer_w_q"], f)[0].reshape(8, 128, 2048).transpose(1, 0, 2))
    sk = np.asarray(inp["peer_sub_keys"], f)[0]
    sh["keysT"] = np.ascontiguousarray(sk.reshape(16, 128, 128).transpose(2, 0, 1))
    pu = np.asarray(inp["peer_u"], f)[0]
    sh["uT"] = np.ascontiguousarray(pu.reshape(128, 128, 8, 128).transpose(1, 3, 2, 0)).reshape(128, 128, 1024)
    pv = np.asarray(inp["peer_v"], f)[0]
    sh["vt"] = np.ascontiguousarray(pv.reshape(128, 128, 1024).transpose(1, 0, 2))
    inv_freq = (np.float32(10000.0) ** (-np.arange(16, dtype=f) / np.float32(16))).astype(f)
    l = np.arange(L)
    cos_t = np.empty((128, L), f)
    sin_t = np.empty((128, L), f)
    for p in range(128):
        d = p % 64
        posn = (l % 64) if d >= 32 else (l // 64)
        ang = posn.astype(f) * inv_freq[d % 16]
        cos_t[p] = np.cos(ang)
        sin_t[p] = np.sin(ang) * (-1.0 if (d % 32) < 16 else 1.0)
    sh["rope_cos"] = cos_t
    sh["rope_sin"] = sin_t
    qi = np.arange(128)[:, None]
    kk = np.arange(384)[None, :]
    sh["amask"] = np.where((kk >= qi) & (kk <= qi + 256), 0.0, NEG).astype(f)
    sh["ident"] = np.eye(128, dtype=f)
    sh["iota128"] = np.ascontiguousarray(np.broadcast_to(np.arange(128, dtype=f)[None, :], (128, 128)))
    sh["cu32"] = np.ascontiguousarray(np.broadcast_to(np.array([4, 15], np.uint32)[None, :], (128, 2)))
    return sh


def prep_core(inp, core, sh):
    f = np.float32
    x = np.asarray(inp["x"], f)
    ctx = np.asarray(inp["ctx"], f)
    c = np.asarray(inp["c"], f)
    c_ctx = np.asarray(inp["c_ctx"], f)
    m = dict(sh)
    b0 = 2 * core
    m["xT"] = _fm(x[b0:b0 + 2].reshape(2 * L, 1024))
    m["ctxT"] = _fm(ctx[b0:b0 + 2].reshape(512, 1024))
    m["cT"] = _fm(np.stack([c[b0], c[b0 + 1], c_ctx], 0))
    return m


_NC_CACHE = {}


def kernel(**inputs):
    sh = prep_shared(inputs)
    in_maps = [prep_core(inputs, core, sh) for core in range(NCORES)]
    if "nc" not in _NC_CACHE:
        _NC_CACHE["nc"] = build()
    nc = _NC_CACHE["nc"]
    res = run_bass_kernel_spmd(nc, in_maps, core_ids=list(range(NCORES)))
    out = np.empty((16, L, 1024), np.float32)
    for core in range(NCORES):
        o = np.asarray(res.results[core]["outT"])
        o = o.transpose(2, 1, 0).reshape(2, L, 1024)
        out[2 * core:2 * core + 2] = o
    return out
```

```python
import numpy as np
from contextlib import ExitStack
import concourse.bass as bass
import concourse.mybir as mybir
from concourse.bass_utils import run_bass_kernel_spmd

F32 = mybir.dt.float32
BF16 = mybir.dt.bfloat16
U32 = mybir.dt.uint32
AF = mybir.ActivationFunctionType
ALU = mybir.AluOpType
AX = mybir.AxisListType

NCORES = 8
L = 2048
TPC = 2 * L
EPS = 1e-6
NEG = -1e30
DEBUG = {"x1": False, "sel": False, "stop_after_mixer": False, "stop": None, "no_prologue": False}


class _Stop(Exception):
    pass


class Res:
    __slots__ = ("name", "w", "r")

    def __init__(self, name):
        self.name = name
        self.w = None
        self.r = {}


class Sched:
    ENG = ("pe", "act", "dve", "pool", "sp")

    def __init__(self, nc, stack, ndma=12):
        self.nc = nc
        self.e = {"pe": nc.tensor, "act": nc.scalar, "dve": nc.vector, "pool": nc.gpsimd, "sp": nc.sync}
        self.sem = {}
        self.cnt = {}
        for k in self.ENG:
            self.sem[k] = stack.enter_context(nc.semaphore("sem_" + k))
            self.cnt[k] = 0
        self.ndma = ndma
        self.dq = {}
        for q in ("sp", "pool"):
            for i in range(ndma):
                self.sem[(q, i)] = stack.enter_context(nc.semaphore(f"dsem_{q}_{i}"))
            self.dq[q] = 0
        self.waited = {k: {} for k in self.ENG}
        self.dead = False

    def _wait(self, eng, tok):
        key, val = tok
        if self.waited[eng].get(key, 0) >= val:
            return
        self.e[eng].wait_ge(self.sem[key], val)
        self.waited[eng][key] = val

    def _deps(self, eng, reads, writes, attach=False):
        toks = {}

        def add(t):
            if t is None:
                return
            k, v = t
            if toks.get(k, 0) < v:
                toks[k] = v
        for r in reads:
            add(r.w)
        for w in writes:
            add(w.w)
            for k, v in w.r.items():
                add((k, v))
        need = [(k, v) for k, v in toks.items() if self.waited[eng].get(k, 0) < v]
        if attach and need:
            last = need.pop()
        else:
            last = None
        for k, v in need:
            self._wait(eng, (k, v))
        return last

    def _attach(self, eng, ins, last):
        if last is not None:
            ins._wait_ge(self.sem[last[0]], last[1])
            self.waited[eng][last[0]] = last[1]

    def _commit(self, tok, reads, writes):
        k, v = tok
        for r in reads:
            if r.r.get(k, 0) < v:
                r.r[k] = v
        for w in writes:
            w.w = tok
            w.r = {}

    def op(self, eng, fn, reads=(), writes=()):
        if self.dead:
            return None
        last = self._deps(eng, reads, writes, attach=True)
        ins = fn(self.e[eng])
        self._attach(eng, ins, last)
        self.cnt[eng] += 1
        ins.then_inc(self.sem[eng], 1)
        tok = (eng, self.cnt[eng])
        self._commit(tok, reads, writes)
        return tok

    def dma(self, q, out, in_, reads=(), writes=()):
        if self.dead:
            return None
        n = self.dq[q]
        self.dq[q] += 1
        slot = n % self.ndma
        rnd = n // self.ndma
        key = (q, slot)
        if rnd > 0:
            self._wait(q, (key, 16 * rnd))
        last = self._deps(q, reads, writes, attach=True)
        ins = self.e[q].dma_start(out=out, in_=in_)
        self._attach(q, ins, last)
        ins.then_inc(self.sem[key], 16)
        tok = (key, 16 * (rnd + 1))
        self._commit(tok, reads, writes)
        return tok

    def barrier(self):
        if self.dead:
            return
        toks = [(k, self.cnt[k]) for k in self.ENG if self.cnt[k] > 0]
        for q, n in self.dq.items():
            for slot in range(min(n, self.ndma)):
                rnd = (n - 1 - slot) // self.ndma
                toks.append(((q, slot), 16 * (rnd + 1)))
        for e in self.ENG:
            for t in toks:
                self._wait(e, t)

    def finish(self, ress):
        for r in ress:
            if r.w is not None:
                self._wait("sp", r.w)


def build():
    nc = bass.Bass("TRN2", target_bir_lowering=False)

    def din(name, shape, dt=F32):
        return nc.dram_tensor(name, list(shape), dt, kind="ExternalInput").ap()

    xT = din("xT", [128, 8, TPC])
    ctxT = din("ctxT", [128, 8, 512])
    cT = din("cT", [128, 8, 3])
    w_mod = din("w_mod", [128, 8, 6144])
    b_mod = din("b_mod", [128, 48])
    n1g = din("n1g", [128, 8])
    n2g = din("n2g", [128, 8])
    fing = din("fing", [128, 8])
    w_in = din("w_in", [39, 128, 8, 128])
    w_ao = din("w_ao", [64, 8, 1024])
    w_co = din("w_co", [128, 4, 1024])
    w_mo = din("w_mo", [128, 8, 1024])
    conv_w = din("conv_w", [128, 4, 3])
    sinkv = din("sinkv", [128, 8])
    pw_q = din("pw_q", [128, 8, 2048])
    keysT = din("keysT", [128, 16, 128])
    uT = din("uT", [128, 128, 1024])
    vt = din("vt", [128, 128, 1024])
    rope_cos = din("rope_cos", [128, L])
    rope_sin = din("rope_sin", [128, L])
    amask = din("amask", [128, 384])
    ident = din("ident", [128, 128])
    iota128 = din("iota128", [128, 128])
    cu32 = din("cu32", [128, 2], U32)
    outT = nc.dram_tensor("outT", [128, 8, TPC], F32, kind="ExternalOutput").ap()
    x1T = nc.dram_tensor("x1T_scr", [128, 8, TPC], F32, kind="Internal").ap()
    uTb = nc.dram_tensor("uTb_scr", [128, 128, 1024], BF16, kind="Internal").ap()
    vtb = nc.dram_tensor("vtb_scr", [128, 128, 1024], BF16, kind="Internal").ap()
    dbg = {}
    if DEBUG["x1"]:
        dbg["x1"] = nc.dram_tensor("dbg_x1", [128, 8, TPC], F32, kind="ExternalOutput").ap()
    if DEBUG["sel"]:
        dbg["sel"] = nc.dram_tensor("dbg_sel", [128, 3, 128], F32, kind="ExternalOutput").ap()

    with ExitStack() as st:
        S = Sched(nc, st)
        NG = 0
        RES = {}

        def R(*key):
            r = RES.get(key)
            if r is None:
                r = RES[key] = Res(str(key))
            return r

        _uid = [0]

        def sb(stack, name, shape, dt):
            _uid[0] += 1
            return stack.enter_context(nc.sbuf_tensor(f"{name}_{_uid[0]}", list(shape), dt))

        def V(fn, reads, writes):
            return S.op("dve", fn, reads, writes)

        def A(fn, reads, writes):
            return S.op("act", fn, reads, writes)

        def G(fn, reads, writes):
            return S.op("pool", fn, reads, writes)

        def PE(fn, reads, writes):
            return S.op("pe", fn, reads, writes)

        psum = st.enter_context(nc.psum_tensor("psum", [128, 8, 512], F32))

        def RB(b):
            return R("psb", b)

        ident_f = sb(st, "ident_f", [128, 128], F32)
        ident_b = sb(st, "ident_b", [128, 128], BF16)
        ones_b = sb(st, "ones_b", [128, 128], BF16)
        zeros_b = sb(st, "zeros_b", [128, 512], BF16)
        iota_t = sb(st, "iota_t", [128, 128], F32)
        cu32_t = sb(st, "cu32_t", [128, 2], U32)
        eps_t = sb(st, "eps_t", [128, 1], F32)
        modp = sb(st, "modp", [128, 48, 3], F32)
        A1 = sb(st, "A1", [128, 8, 3], F32)
        A2 = sb(st, "A2", [128, 8, 3], F32)
        bmod_t = sb(st, "bmod_t", [128, 48], F32)
        n1g_t = sb(st, "n1g_t", [128, 8], F32)
        n2g_t = sb(st, "n2g_t", [128, 8], F32)
        fing_t = sb(st, "fing_t", [128, 8], F32)
        sink_t = sb(st, "sink_t", [128, 8], F32)
        nsink_t = sb(st, "nsink_t", [128, 8], F32)
        convw_t = sb(st, "convw_t", [128, 4, 3], F32)

        S.dma("sp", ident_f[:], ident[:, :], writes=[R("ident_f")])
        S.dma("sp", iota_t[:], iota128[:, :], writes=[R("iota")])
        S.dma("sp", cu32_t[:], cu32[:, :], writes=[R("cu32")])
        S.dma("sp", bmod_t[:], b_mod[:, :], writes=[R("bmod")])
        S.dma("sp", n1g_t[:], n1g[:, :], writes=[R("n1g")])
        S.dma("sp", n2g_t[:], n2g[:, :], writes=[R("n2g")])
        S.dma("sp", fing_t[:], fing[:, :], writes=[R("fing")])
        S.dma("sp", sink_t[:], sinkv[:, :], writes=[R("sink")])
        S.dma("sp", convw_t[:], conv_w[:, :, :], writes=[R("convw")])
        V(lambda e: e.tensor_copy(out=ident_b[:], in_=ident_f[:]), [R("ident_f")], [R("ident_b")])
        V(lambda e: e.memset(ones_b[:], 1.0), [], [R("ones_b")])
        V(lambda e: e.memset(zeros_b[:], 0.0), [], [R("zeros_b")])
        V(lambda e: e.memset(eps_t[:], EPS), [], [R("eps")])
        V(lambda e: e.tensor_scalar(out=nsink_t[:], in0=sink_t[:], scalar1=-1.0, scalar2=None, op0=ALU.mult),
          [R("sink")], [R("nsink")])

        NPB = 2
        pro_stack = st.enter_context(ExitStack())
        pst = [sb(pro_stack, f"pst{i}", [128, 1024], F32) for i in range(NPB)]
        pbf = [sb(pro_stack, f"pbf{i}", [128, 1024], BF16) for i in range(NPB)]
        pro_state = {"k": 0}
        NPRO = 0 if DEBUG["no_prologue"] else 256

        def pro_src_dst(k):
            tab, j = divmod(k, 128)
            if tab == 0:
                return uT[j], uTb[j], R("scr_u", j)
            return vt[j], vtb[j], R("scr_v", j)

        def prologue_step():
            k = pro_state["k"]
            if k >= NPRO + 1:
                return False
            if k < NPRO:
                src, _, _ = pro_src_dst(k)
                S.dma("sp", pst[k % NPB][:], src, writes=[R("pst", k % NPB)])
            k2 = k - 1
            if 0 <= k2 < NPRO:
                _, dst, rr = pro_src_dst(k2)
                sl = k2 % NPB
                G(lambda e: e.tensor_copy(out=pbf[sl][:], in_=pst[sl][:]), [R("pst", sl)], [R("pbf", sl)])
                S.dma("sp", dst, pbf[sl][:], reads=[R("pbf", sl)], writes=[rr])
            pro_state["k"] = k + 1
            return True

        def prologue_steps(n):
            for _ in range(n):
                if not prologue_step():
                    break

        def norm_group(xs, rxs, N, sq, rstd, tmp2, bank, Acol, Bcol, outc, rout, in_place):
            A(lambda e: e.activation(out=sq[:, :, 0:N], in_=xs, func=AF.Square), [rxs], [R("sq")])
            for c in range(8):
                PE(lambda e: e.matmul(psum[:, bank, 0:N], lhsT=ones_b[:], rhs=sq[:, c, 0:N],
                                      start=(c == 0), stop=(c == 7)), [R("sq"), R("ones_b")], [RB(bank)])
            A(lambda e: e.activation(out=rstd[:, 0:N], in_=psum[:, bank, 0:N], func=AF.Sqrt,
                                     scale=1.0 / 1024.0, bias=eps_t[:, 0:1]), [RB(bank), R("eps")], [R("rstd")])
            V(lambda e: e.reciprocal(out=rstd[:, 0:N], in_=rstd[:, 0:N]), [R("rstd")], [R("rstd")])
            for c in range(8):
                if in_place:
                    t = xs[:, c, :]
                    rt = rxs
                else:
                    t = tmp2[c % 2][:, 0:N]
                    rt = R("ntmp", c % 2)
                V(lambda e: e.tensor_tensor(out=t, in0=xs[:, c, :], in1=rstd[:, 0:N], op=ALU.mult),
                  [rxs, R("rstd")], [rt])
                b = Bcol(c) if Bcol is not None else None
                if b is not None:
                    V(lambda e: e.tensor_scalar(out=outc(c), in0=t, scalar1=Acol(c), scalar2=b,
                                                op0=ALU.mult, op1=ALU.add), [rt], [rout])
                else:
                    V(lambda e: e.tensor_scalar(out=outc(c), in0=t, scalar1=Acol(c), scalar2=None,
                                                op0=ALU.mult), [rt], [rout])

        with ExitStack() as p0:
            cTt = sb(p0, "cTt", [128, 8, 3], F32)
            scT = sb(p0, "scT", [128, 8, 3], F32)
            wm = [sb(p0, f"wm{i}", [128, 8, 512], F32) for i in range(2)]
            S.dma("sp", cTt[:], cT[:, :, :], writes=[R("cTt")])
            A(lambda e: e.activation(out=scT[:], in_=cTt[:], func=AF.Silu), [R("cTt")], [R("scT")])
            for pc in range(12):
                b = pc % 2
                S.dma("sp", wm[b][:], w_mod[:, :, pc * 512:(pc + 1) * 512], writes=[R("wm", b)])
                for cc in range(4):
                    j = pc * 4 + cc
                    for kc in range(8):
                        PE(lambda e: e.matmul(psum[:, 0, j * 3:(j + 1) * 3], lhsT=wm[b][:, kc, cc * 128:(cc + 1) * 128],
                                              rhs=scT[:, kc, :], start=(kc == 0), stop=(kc == 7)),
                           [R("wm", b), R("scT")], [RB(0)])
            V(lambda e: e.tensor_tensor(out=modp[:], in0=psum[:, 0, 0:144].rearrange("p (a b) -> p a b", b=3),
                                        in1=bmod_t[:, :].unsqueeze(2).to_broadcast([128, 48, 3]), op=ALU.add),
              [RB(0), R("bmod")], [R("modp")])
            for (Ax, off, gt, rg) in ((A1, 8, n1g_t, "n1g"), (A2, 32, n2g_t, "n2g")):
                V(lambda e: e.tensor_scalar(out=Ax[:], in0=modp[:, off:off + 8, :], scalar1=1.0, scalar2=None,
                                            op0=ALU.add), [R("modp")], [R("A12")])
                V(lambda e: e.tensor_tensor(out=Ax[:], in0=Ax[:], in1=gt[:, :].unsqueeze(2).to_broadcast([128, 8, 3]),
                                            op=ALU.mult), [R("A12"), R(rg)], [R("A12")])
        S.barrier()

        try:
          with ExitStack() as p1:
              NW = 5
              wst = [sb(p1, f"wst{i}", [128, 8, 128], F32) for i in range(NW)]
              wbf = [sb(p1, f"wbf{i}", [128, 8, 128], BF16) for i in range(NW)]
              wcount = [0]

              def load_w(idx):
                  i = wcount[0] % NW
                  wcount[0] += 1
                  S.dma("sp", wst[i][:], w_in[idx], writes=[R("wst", i)])
                  G(lambda e: e.tensor_copy(out=wbf[i][:], in_=wst[i][:]), [R("wst", i)], [R("wbf", i)])
                  return wbf[i], R("wbf", i)

              class WStream:
                  def __init__(self, order):
                      self.order = order
                      self.pos = 0
                      self.q = []

                  def prefetch(self, n):
                      while len(self.q) < n and self.pos < len(self.order):
                          self.q.append(load_w(self.order[self.pos]))
                          self.pos += 1

                  def get(self):
                      self.prefetch(1)
                      return self.q.pop(0)

              hT = sb(p1, "hT", [128, 8, L], BF16)
              hcT = sb(p1, "hcT", [128, 8, 256], BF16)
              attnT = sb(p1, "attnT", [64, 8, L], BF16)
              convo = sb(p1, "convo", [128, 4, L], BF16)
              rstd = sb(p1, "rstd", [128, 512], F32)

              def load_big(dst_fn, src_fn, npieces, parts, rname):
                  for pc in range(npieces):
                      i = wcount[0] % NW
                      wcount[0] += 1
                      stg = wst[i][:].rearrange("p a b -> p (a b)")
                      S.dma("sp", stg[0:parts, :], src_fn(pc), writes=[R("wst", i)])
                      G(lambda e: e.tensor_copy(out=dst_fn(pc), in_=stg[0:parts, :]), [R("wst", i)], [R(rname)])


              for s in range(2):
                  order = [0, 4, 1, 5, 2, 6, 3, 7, 8, 9, 10] + list(range(11, 23))
                  for tg in range(4):
                      for oc in range(8):
                          order += [23 + oc, 31 + oc]
                  ws = WStream(order)
                  ws.prefetch(2)

                  with ExitStack() as psa:
                      xs = sb(psa, "xs", [128, 8, 512], F32)
                      sq = sb(psa, "sq", [128, 8, 512], BF16)
                      for tg in range(4):
                          S.dma("sp", xs[:], xT[:, :, s * L + tg * 512: s * L + (tg + 1) * 512], writes=[R("xs")])
                          norm_group(xs[:], R("xs"), 512, sq, rstd, None, tg % 2,
                                     lambda c: A1[:, c, s:s + 1], lambda c: modp[:, c, s:s + 1],
                                     lambda c: hT[:, c, tg * 512:(tg + 1) * 512], R("hT", tg), True)
                      S.dma("sp", xs[:, :, 0:256], ctxT[:, :, s * 256:(s + 1) * 256], writes=[R("xs")])
                      norm_group(xs[:, :, 0:256], R("xs"), 256, sq, rstd, None, 0,
                                 lambda c: A1[:, c, 2:3], lambda c: modp[:, c, 2:3],
                                 lambda c: hcT[:, c, :], R("hcT"), True)
                      prologue_steps(8)
                  S.barrier()
                  if DEBUG["stop"] == "A":
                      S.dead = True

                  with ExitStack() as pa:
                      qT = sb(pa, "qT", [128, 4, L], BF16)
                      kT = sb(pa, "kT", [128, 256 + L], BF16)
                      vtok = sb(pa, "vtok", [128, 18, 128], BF16)
                      amask_t = sb(pa, "amask_t", [128, 384], F32)
                      pb_ = ExitStack()
                      cos_t = sb(pb_, "cos_t", [128, L], F32)
                      sin_t = sb(pb_, "sin_t", [128, L], F32)
                      t1 = [sb(pb_, f"t1_{i}", [128, 512], F32) for i in range(2)]
                      t2 = [sb(pb_, f"t2_{i}", [128, 512], F32) for i in range(2)]
                      S.dma("sp", cos_t[:], rope_cos[:, :], writes=[R("cos")])
                      S.dma("sp", sin_t[:], rope_sin[:, :], writes=[R("sin")])
                      S.dma("sp", amask_t[:], amask[:, :], writes=[R("amask")])

                      it = 0

                      def proj_rope(wa, ra, wb_, rb_, dst_fn, rdst_fn):
                          nonlocal it
                          for tg in range(4):
                              ba, bb = (2, 3) if it % 2 == 0 else (4, 5)
                              tt1, tt2 = t1[it % 2], t2[it % 2]
                              r1, r2 = R("t1", it % 2), R("t2", it % 2)
                              it += 1
                              for kc in range(8):
                                  PE(lambda e: e.matmul(psum[:, ba, :], lhsT=wa[:, kc, :], rhs=hT[:, kc, tg * 512:(tg + 1) * 512],
                                                        start=(kc == 0), stop=(kc == 7)), [ra, R("hT", tg)], [RB(ba)])
                              for kc in range(8):
                                  PE(lambda e: e.matmul(psum[:, bb, :], lhsT=wb_[:, kc, :], rhs=hT[:, kc, tg * 512:(tg + 1) * 512],
                                                        start=(kc == 0), stop=(kc == 7)), [rb_, R("hT", tg)], [RB(bb)])
                              V(lambda e: e.tensor_tensor(out=tt1[:], in0=psum[:, ba, :], in1=cos_t[:, tg * 512:(tg + 1) * 512],
                                                          op=ALU.mult), [RB(ba), R("cos")], [r1])
                              V(lambda e: e.tensor_tensor(out=tt2[:], in0=psum[:, bb, :], in1=sin_t[:, tg * 512:(tg + 1) * 512],
                                                          op=ALU.mult), [RB(bb), R("sin")], [r2])
                              G(lambda e: e.tensor_tensor(out=dst_fn(tg), in0=tt1[:], in1=tt2[:], op=ALU.add),
                                [r1, r2], [rdst_fn(tg)])

                      for ch in range(4):
                          (wq, rq) = ws.get()
                          (wqs, rqs) = ws.get()
                          ws.prefetch(2)
                          proj_rope(wq, rq, wqs, rqs, lambda tg: qT[:, ch, tg * 512:(tg + 1) * 512],
                                    lambda tg: R("qT", ch, tg))
                          prologue_steps(4)
                      (wk, rk) = ws.get()
                      (wks, rks) = ws.get()
                      ws.prefetch(2)
                      for kc in range(8):
                          PE(lambda e: e.matmul(psum[:, 6, 0:256], lhsT=wk[:, kc, :], rhs=hcT[:, kc, :],
                                                start=(kc == 0), stop=(kc == 7)), [rk, R("hcT")], [RB(6)])
                      A(lambda e: e.activation(out=kT[:, 0:256], in_=psum[:, 6, 0:256], func=AF.Copy), [RB(6)], [R("kT", "ctx")])
                      proj_rope(wk, rk, wks, rks, lambda tg: kT[:, 256 + tg * 512: 256 + (tg + 1) * 512],
                                lambda tg: R("kT", tg))
                      (wv, rv) = ws.get()
                      ws.prefetch(3)
                      for blk in range(18):
                          bank = 6 + blk % 2
                          for kc in range(8):
                              if blk < 2:
                                  lh = hcT[:, kc, blk * 128:(blk + 1) * 128]
                                  rl = R("hcT")
                              else:
                                  lh = hT[:, kc, (blk - 2) * 128:(blk - 1) * 128]
                                  rl = R("hT", (blk - 2) // 4)
                              PE(lambda e: e.matmul(psum[:, bank, 0:128], lhsT=lh, rhs=wv[:, kc, :],
                                                    start=(kc == 0), stop=(kc == 7)), [rl, rv], [RB(bank)])
                          A(lambda e: e.activation(out=vtok[:, blk, :], in_=psum[:, bank, 0:128], func=AF.Copy),
                            [RB(bank)], [R("vtok", blk)])
                      prologue_steps(8)
                      pb_.close()
                      S.barrier()
                      if DEBUG["stop"] == "B":
                          S.dead = True

                      with ExitStack() as pc_:
                          sc = [sb(pc_, f"sc{i}", [128, 640], F32) for i in range(2)]
                          Pm = [sb(pc_, f"Pm{i}", [128, 640], BF16) for i in range(2)]
                          sm = [sb(pc_, f"sm{i}", [128, 8], F32) for i in range(2)]
                          dg = [sb(pc_, f"dg{i}", [128, 128], BF16) for i in range(2)]
                          PTs = [sb(pc_, f"PTs{i}", [128, 5, 4, 128], BF16) for i in range(2)]
                          hcnt = 0
                          pvc = 0
                          for n in range(16):
                              lo = max(n - 1, 0)
                              hi = min(n + 1, 15)
                              nlb = hi - lo + 1
                              nloc = nlb * 128
                              nk = nloc + 256
                              nkb = nlb + 2
                              moff = (lo - (n - 1)) * 128
                              krs = [R("kT", "ctx")] + [R("kT", t) for t in sorted(set([(lo * 128) // 512, (hi * 128 + 127) // 512]))]
                              for g in range(2):
                                  psl = slice(g * 64, (g + 1) * 64)
                                  pts = PTs[pvc % 2]
                                  rpts = R("PTs", pvc % 2)
                                  for c in range(4):
                                      hq = g * 4 + c
                                      i2 = hcnt % 2
                                      sa, sbk = (0, 1) if i2 == 0 else (2, 3)
                                      hcnt += 1
                                      scx, Px, smx, dgx = sc[i2], Pm[i2], sm[i2], dg[i2]
                                      rsc, rP, rsm, rdg = R("sc", i2), R("Pm", i2), R("sm", i2), R("dg", i2)
                                      lq = qT[psl, c, n * 128:(n + 1) * 128]
                                      PE(lambda e: e.matmul(psum[:, sa, 0:nloc], lhsT=lq,
                                                            rhs=kT[psl, 256 + lo * 128: 256 + (hi + 1) * 128],
                                                            start=True, stop=True), [R("qT", c, n // 4)] + krs, [RB(sa)])
                                      PE(lambda e: e.matmul(psum[:, sbk, 0:256], lhsT=lq, rhs=kT[psl, 0:256],
                                                            start=True, stop=True), [R("qT", c, n // 4)] + krs, [RB(sbk)])
                                      V(lambda e: e.tensor_tensor(out=scx[:, 0:nloc], in0=psum[:, sa, 0:nloc],
                                                                  in1=amask_t[:, moff:moff + nloc], op=ALU.add),
                                        [RB(sa), R("amask")], [rsc])
                                      A(lambda e: e.activation(out=scx[:, nloc:nk], in_=psum[:, sbk, 0:256], func=AF.Copy),
                                        [RB(sbk)], [rsc])
                                      V(lambda e: e.reduce_max(out=smx[:, 0:1], in_=scx[:, 0:nk], axis=AX.X), [rsc], [rsm])
                                      V(lambda e: e.tensor_scalar(out=smx[:, 1:2], in0=smx[:, 0:1], scalar1=-0.125,
                                                                  scalar2=nsink_t[:, hq:hq + 1], op0=ALU.mult, op1=ALU.min),
                                        [rsm, R("nsink")], [rsm])
                                      A(lambda e: e.activation(out=Px[:, 0:nk], in_=scx[:, 0:nk], func=AF.Exp, scale=0.125,
                                                               bias=smx[:, 1:2], accum_out=smx[:, 2:3]), [rsc, rsm], [rP, rsm])
                                      A(lambda e: e.activation(out=smx[:, 3:4], in_=sink_t[:, hq:hq + 1], func=AF.Exp, scale=1.0,
                                                               bias=smx[:, 1:2]), [rsm, R("sink")], [rsm])
                                      V(lambda e: e.tensor_tensor(out=smx[:, 4:5], in0=smx[:, 2:3], in1=smx[:, 3:4], op=ALU.add),
                                        [rsm], [rsm])
                                      V(lambda e: e.reciprocal(out=smx[:, 5:6], in_=smx[:, 4:5]), [rsm], [rsm])
                                      V(lambda e: e.tensor_scalar(out=dgx[:], in0=ident_b[:], scalar1=smx[:, 5:6], scalar2=None,
                                                                  op0=ALU.mult), [rsm, R("ident_b")], [rdg])
                                      for kb in range(nkb):
                                          bank = 4 + kb // 4
                                          off = (kb % 4) * 128
                                          PE(lambda e: e.matmul(psum[:, bank, off:off + 128], lhsT=Px[:, kb * 128:(kb + 1) * 128],
                                                                rhs=dgx[:], start=True, stop=True), [rP, rdg], [RB(bank)])
                                      A(lambda e: e.activation(out=pts[:, 0:4, c, :],
                                                               in_=psum[:, 4, :].rearrange("p (k q) -> p k q", q=128),
                                                               func=AF.Copy), [RB(4)], [rpts])
                                      if nkb == 5:
                                          V(lambda e: e.tensor_copy(out=pts[:, 4, c, :], in_=psum[:, 5, 0:128]), [RB(5)], [rpts])
                                  ob = 6 + pvc % 2
                                  pvc += 1
                                  for kb in range(nkb):
                                      blk = (2 + lo + kb) if kb < nlb else (kb - nlb)
                                      PE(lambda e: e.matmul(psum[0:64, ob, :], lhsT=vtok[:, blk, g * 64:(g + 1) * 64],
                                                            rhs=pts[:, kb, :, :].rearrange("p c q -> p (c q)"),
                                                            start=(kb == 0), stop=(kb == nkb - 1)),
                                         [R("vtok", blk), rpts], [RB(ob)])
                                  A(lambda e: e.activation(out=attnT[:, g * 4:(g + 1) * 4, n * 128:(n + 1) * 128],
                                                           in_=psum[0:64, ob, :].rearrange("p (c q) -> p c q", q=128),
                                                           func=AF.Copy), [RB(ob)], [R("attnT", n // 4)])
                              prologue_steps(3)
                          S.barrier()
                  S.barrier()
                  if DEBUG["stop"] == "C":
                      S.dead = True

                  with ExitStack() as pd:
                      cu = sb(pd, "cu", [128, L + 2], F32)
                      yc = sb(pd, "yc", [128, L], F32)
                      gbs = sb(pd, "gbs", [128, L], F32)
                      ut = [sb(pd, f"ut{i}", [128, 512], F32) for i in range(2)]
                      G(lambda e: e.memset(cu[:, 0:1], 0.0), [], [R("cu_pad")])
                      G(lambda e: e.memset(cu[:, L + 1:L + 2], 0.0), [], [R("cu_pad")])
                      cnt = 0
                      for c in range(4):
                          (wg, rg) = ws.get()
                          (wu, ru) = ws.get()
                          (wb_, rb_) = ws.get()
                          ws.prefetch(2)
                          for tg in range(4):
                              bk = (0, 1, 2) if cnt % 2 == 0 else (3, 4, 5)
                              utx = ut[cnt % 2]
                              rut = R("ut", cnt % 2)
                              cnt += 1
                              for (w_, r_, b_) in ((wg, rg, bk[0]), (wu, ru, bk[1]), (wb_, rb_, bk[2])):
                                  for kc in range(8):
                                      PE(lambda e: e.matmul(psum[:, b_, :], lhsT=w_[:, kc, :], rhs=hT[:, kc, tg * 512:(tg + 1) * 512],
                                                            start=(kc == 0), stop=(kc == 7)), [r_, R("hT", tg)], [RB(b_)])
                              A(lambda e: e.activation(out=utx[:], in_=psum[:, bk[1], :], func=AF.Copy), [RB(bk[1])], [rut])
                              V(lambda e: e.tensor_tensor(out=cu[:, 1 + tg * 512: 1 + (tg + 1) * 512], in0=psum[:, bk[0], :],
                                                          in1=utx[:], op=ALU.mult), [RB(bk[0]), rut], [R("cu", tg)])
                              A(lambda e: e.activation(out=gbs[:, tg * 512:(tg + 1) * 512], in_=psum[:, bk[2], :], func=AF.Copy),
                                [RB(bk[2])], [R("gbs", tg)])
                          cur = [R("cu", t) for t in range(4)] + [R("cu_pad")]
                          V(lambda e: e.tensor_scalar(out=yc[:], in0=cu[:, 0:L], scalar1=convw_t[:, c, 0:1], scalar2=None,
                                                      op0=ALU.mult), cur + [R("convw")], [R("yc")])
                          V(lambda e: e.scalar_tensor_tensor(out=yc[:], in0=cu[:, 1:L + 1], scalar=convw_t[:, c, 1:2], in1=yc[:],
                                                             op0=ALU.mult, op1=ALU.add), cur + [R("yc")], [R("yc")])
                          V(lambda e: e.scalar_tensor_tensor(out=yc[:], in0=cu[:, 2:L + 2], scalar=convw_t[:, c, 2:3], in1=yc[:],
                                                             op0=ALU.mult, op1=ALU.add), cur + [R("yc")], [R("yc")])
                          G(lambda e: e.tensor_tensor(out=convo[:, c, :], in0=yc[:], in1=gbs[:], op=ALU.mult),
                            [R("yc")] + [R("gbs", t) for t in range(4)], [R("convo")])
                          prologue_steps(4)
                  S.barrier()
                  if DEBUG["stop"] == "D":
                      S.dead = True

                  with ExitStack() as pe_:
                      xs = sb(pe_, "xs", [128, 8, 512], F32)
                      w_ao_b = sb(pe_, "w_ao_b", [64, 8, 1024], BF16)
                      w_co_b = sb(pe_, "w_co_b", [128, 4, 1024], BF16)
                      w_mo_b = sb(pe_, "w_mo_b", [128, 8, 1024], BF16)
                      load_big(lambda pc: w_ao_b[:, pc, :], lambda pc: w_ao[:, pc, :], 8, 64, "w_ao_b")
                      load_big(lambda pc: w_co_b[:, pc, :], lambda pc: w_co[:, pc, :], 4, 128, "w_co_b")
                      load_big(lambda pc: w_mo_b[:, pc, :], lambda pc: w_mo[:, pc, :], 8, 128, "w_mo_b")
                      mixT = sb(pe_, "mixT", [128, 8, 512], BF16)
                      sg = [sb(pe_, f"sg{i}", [128, 512], F32) for i in range(2)]
                      mm_ = [sb(pe_, f"mm{i}", [128, 512], F32) for i in range(2)]
                      for tg in range(4):
                          tsl = slice(tg * 512, (tg + 1) * 512)
                          S.dma("sp", xs[:], xT[:, :, s * L + tg * 512: s * L + (tg + 1) * 512], writes=[R("xs")])
                          for oc in range(8):
                              (wga, rga) = ws.get()
                              (wgv, rgv) = ws.get()
                              ws.prefetch(2)
                              osl = slice(oc * 128, (oc + 1) * 128)
                              for h in range(8):
                                  PE(lambda e: e.matmul(psum[:, 0, :], lhsT=w_ao_b[:, h, osl], rhs=attnT[:, h, tsl],
                                                        start=(h == 0), stop=(h == 7)), [R("w_ao_b"), R("attnT", tg)], [RB(0)])
                              for c in range(4):
                                  PE(lambda e: e.matmul(psum[:, 1, :], lhsT=w_co_b[:, c, osl], rhs=convo[:, c, tsl],
                                                        start=(c == 0), stop=(c == 3)), [R("w_co_b"), R("convo")], [RB(1)])
                              for kc in range(8):
                                  PE(lambda e: e.matmul(psum[:, 2, :], lhsT=wga[:, kc, :], rhs=hT[:, kc, tsl],
                                                        start=(kc == 0), stop=(kc == 7)), [rga, R("hT", tg)], [RB(2)])
                              for kc in range(8):
                                  PE(lambda e: e.matmul(psum[:, 3, :], lhsT=wgv[:, kc, :], rhs=hT[:, kc, tsl],
                                                        start=(kc == 0), stop=(kc == 7)), [rgv, R("hT", tg)], [RB(3)])
                              A(lambda e: e.activation(out=sg[0][:], in_=psum[:, 2, :], func=AF.Sigmoid), [RB(2)], [R("sg", 0)])
                              A(lambda e: e.activation(out=sg[1][:], in_=psum[:, 3, :], func=AF.Sigmoid), [RB(3)], [R("sg", 1)])
                              V(lambda e: e.tensor_tensor(out=mm_[0][:], in0=psum[:, 0, :], in1=sg[0][:], op=ALU.mult),
                                [RB(0), R("sg", 0)], [R("mm", 0)])
                              V(lambda e: e.tensor_tensor(out=mm_[1][:], in0=psum[:, 1, :], in1=sg[1][:], op=ALU.mult),
                                [RB(1), R("sg", 1)], [R("mm", 1)])
                              G(lambda e: e.tensor_tensor(out=mixT[:, oc, :], in0=mm_[0][:], in1=mm_[1][:], op=ALU.add),
                                [R("mm", 0), R("mm", 1)], [R("mixT")])
                          for oc in range(8):
                              ob = 4 + oc % 2
                              osl = slice(oc * 128, (oc + 1) * 128)
                              for c in range(8):
                                  PE(lambda e: e.matmul(psum[:, ob, :], lhsT=w_mo_b[:, c, osl], rhs=mixT[:, c, :],
                                                        start=(c == 0), stop=(c == 7)), [R("w_mo_b"), R("mixT")], [RB(ob)])
                              V(lambda e: e.scalar_tensor_tensor(out=xs[:, oc, :], in0=psum[:, ob, :],
                                                                 scalar=modp[:, 16 + oc, s:s + 1], in1=xs[:, oc, :],
                                                                 op0=ALU.mult, op1=ALU.add), [RB(ob), R("xs"), R("modp")], [R("xs")])
                          S.dma("sp", x1T[:, :, s * L + tg * 512: s * L + (tg + 1) * 512], xs[:], reads=[R("xs")],
                                writes=[R("x1T", s * 4 + tg)])
                          if DEBUG["x1"]:
                              S.dma("sp", dbg["x1"][:, :, s * L + tg * 512: s * L + (tg + 1) * 512], xs[:], reads=[R("xs")],
                                    writes=[R("dbg_x1")])
                          prologue_steps(6)
                  S.barrier()

              while prologue_step():
                  pass
          pro_stack.close()
          S.barrier()

          TG = 256
          NG = TPC // TG
          if DEBUG["stop_after_mixer"]:
              NG = 0
          with ExitStack() as p2:
              pwq_b = sb(p2, "pwq_b", [128, 8, 2048], BF16)
              keys_b = sb(p2, "keys_b", [128, 16, 128], BF16)
              x1g = sb(p2, "x1g", [128, 8, TG], F32)
              sq2 = sb(p2, "sq2", [128, 8, TG], BF16)
              rstd2 = sb(p2, "rstd2", [128, TG], F32)
              ntmp = [sb(p2, f"ntmp{i}", [128, TG], F32) for i in range(2)]
              h2T = sb(p2, "h2T", [128, 8, TG], BF16)
              qpT = sb(p2, "qpT", [128, 16, TG], BF16)
              s1 = sb(p2, "s1", [128, 16, 128], F32)
              m16 = sb(p2, "m16", [128, 16, 16], F32)
              ix = sb(p2, "ix", [128, 16, 16], U32)
              ixf = sb(p2, "ixf", [128, 16, 16], F32)
              cand = sb(p2, "cand", [128, 8, 256], F32)
              b16 = sb(p2, "b16", [128, 8, 16], F32)
              pos = sb(p2, "pos", [128, 8, 16], U32)
              pab = sb(p2, "pab", [128, 2, 128], U32)
              pabf = sb(p2, "pabf", [128, 2, 128], F32)
              ee = sb(p2, "ee", [128, 8, 16], F32)
              zs = sb(p2, "zs", [128, 8], F32)
              IJG = sb(p2, "IJG", [128, 3, 128], F32)
              IJGT = sb(p2, "IJGT", [128, 3, TG], F32)
              NB = 16
              Pmx = [sb(p2, f"Pmx{i}", [128, NB, 128], BF16) for i in range(2)]
              Qmx = [sb(p2, f"Qmx{i}", [128, NB, 128], BF16) for i in range(2)]
              Wsum = sb(p2, "Wsum", [128, 128, TG], BF16)
              NS = 3
              ub = [sb(p2, f"ub{i}", [128, 8, 128], BF16) for i in range(NS)]
              vb = [sb(p2, f"vb{i}", [128, 1024], BF16) for i in range(NS)]
              ge = [sb(p2, f"ge{i}", [128, TG], F32) for i in range(2)]
              Lj = [sb(p2, f"Lj{i}", [128, TG], BF16) for i in range(2)]

              with ExitStack() as pl:
                  stg = sb(pl, "stg", [128, 2048], F32)
                  for kc in range(8):
                      S.dma("sp", stg[:], pw_q[:, kc, :], writes=[R("stg")])
                      G(lambda e: e.tensor_copy(out=pwq_b[:, kc, :], in_=stg[:]), [R("stg")], [R("pwq_b")])
                  S.dma("sp", stg[:], keysT[:, :, :].rearrange("p a b -> p (a b)"), writes=[R("stg")])
                  G(lambda e: e.tensor_copy(out=keys_b[:].rearrange("p a b -> p (a b)"), in_=stg[:]), [R("stg")], [R("keys_b")])
              S.barrier()

              scnt = 0
              acnt = 0
              for gi in range(NG):
                  s = gi // (NG // 2) if NG >= 2 else 0
                  t0 = gi * TG
                  S.dma("sp", x1g[:], x1T[:, :, t0:t0 + TG], reads=[R("x1T", t0 // 512)], writes=[R("x1g")])
                  norm_group(x1g[:], R("x1g"), TG, sq2, rstd2, ntmp, 4,
                             lambda c: A2[:, c, s:s + 1], lambda c: modp[:, 24 + c, s:s + 1],
                             lambda c: h2T[:, c, :], R("h2T"), False)
                  for hp in range(16):
                      bank = 4 + (hp // 2) % 4
                      off = (hp % 2) * TG
                      for kc in range(8):
                          PE(lambda e: e.matmul(psum[:, bank, off:off + TG], lhsT=pwq_b[:, kc, hp * 128:(hp + 1) * 128],
                                                rhs=h2T[:, kc, :], start=(kc == 0), stop=(kc == 7)),
                             [R("pwq_b"), R("h2T")], [RB(bank)])
                      if hp % 2 == 1:
                          A(lambda e: e.activation(out=qpT[:, hp - 1:hp + 1, :],
                                                   in_=psum[:, bank, :].rearrange("p (a t) -> p a t", t=TG), func=AF.Copy),
                            [RB(bank)], [R("qpT")])
                  for tt in range(TG // 128):
                      tsl = slice(tt * 128, (tt + 1) * 128)
                      for hp in range(16):
                          bank = 4 + hp // 4
                          off = (hp % 4) * 128
                          PE(lambda e: e.matmul(psum[:, bank, off:off + 128], lhsT=qpT[:, hp, tsl], rhs=keys_b[:, hp, :],
                                                start=True, stop=True), [R("qpT"), R("keys_b")], [RB(bank)])
                      A(lambda e: e.activation(out=s1[:].rearrange("p a b -> p (a b)"),
                                               in_=psum[:, 4:8, :].rearrange("p a b -> p (a b)"), func=AF.Copy),
                        [RB(4), RB(5), RB(6), RB(7)], [R("s1")])
                      for hp in range(16):
                          V(lambda e: e.max(out=m16[:, hp, 0:8], in_=s1[:, hp, :]), [R("s1")], [R("m16")])
                          V(lambda e: e.max_index(out=ix[:, hp, 0:8], in_max=m16[:, hp, 0:8], in_values=s1[:, hp, :]),
                            [R("s1"), R("m16")], [R("ix")])
                          V(lambda e: e.match_replace(out=s1[:, hp, :], in_to_replace=m16[:, hp, 0:8], in_values=s1[:, hp, :],
                                                      imm_value=NEG), [R("s1"), R("m16")], [R("s1")])
                          V(lambda e: e.max(out=m16[:, hp, 8:16], in_=s1[:, hp, :]), [R("s1")], [R("m16")])
                          V(lambda e: e.max_index(out=ix[:, hp, 8:16], in_max=m16[:, hp, 8:16], in_values=s1[:, hp, :]),
                            [R("s1"), R("m16")], [R("ix")])
                      V(lambda e: e.tensor_copy(out=ixf[:], in_=ix[:]), [R("ix")], [R("ixf")])
                      m4 = m16[:].rearrange("p (h two) k -> p h two k", two=2)
                      i4 = ixf[:].rearrange("p (h two) k -> p h two k", two=2)
                      V(lambda e: e.tensor_tensor(out=cand[:].rearrange("p h (a b) -> p h a b", b=16),
                                                  in0=m4[:, :, 0, :].unsqueeze(3).to_broadcast([128, 8, 16, 16]),
                                                  in1=m4[:, :, 1, :].unsqueeze(2).to_broadcast([128, 8, 16, 16]), op=ALU.add),
                        [R("m16")], [R("cand")])
                      for h in range(8):
                          V(lambda e: e.max(out=b16[:, h, 0:8], in_=cand[:, h, :]), [R("cand")], [R("b16")])
                          V(lambda e: e.max_index(out=pos[:, h, 0:8], in_max=b16[:, h, 0:8], in_values=cand[:, h, :]),
                            [R("cand"), R("b16")], [R("pos")])
                          V(lambda e: e.match_replace(out=cand[:, h, :], in_to_replace=b16[:, h, 0:8], in_values=cand[:, h, :],
                                                      imm_value=NEG), [R("cand"), R("b16")], [R("cand")])
                          V(lambda e: e.max(out=b16[:, h, 8:16], in_=cand[:, h, :]), [R("cand")], [R("b16")])
                          V(lambda e: e.max_index(out=pos[:, h, 8:16], in_max=b16[:, h, 8:16], in_values=cand[:, h, :]),
                            [R("cand"), R("b16")], [R("pos")])
                      V(lambda e: e.tensor_tensor(out=ee[:], in0=b16[:], in1=b16[:, :, 0:1].to_broadcast([128, 8, 16]),
                                                  op=ALU.subtract), [R("b16")], [R("ee")])
                      A(lambda e: e.activation(out=ee[:], in_=ee[:], func=AF.Exp), [R("ee")], [R("ee")])
                      V(lambda e: e.reduce_sum(out=zs[:], in_=ee[:], axis=AX.X), [R("ee")], [R("zs")])
                      V(lambda e: e.reciprocal(out=zs[:], in_=zs[:]), [R("zs")], [R("zs")])
                      V(lambda e: e.tensor_tensor(out=IJG[:, 2, :].rearrange("p (h k) -> p h k", k=16), in0=ee[:],
                                                  in1=zs[:, :].unsqueeze(2).to_broadcast([128, 8, 16]), op=ALU.mult),
                        [R("ee"), R("zs")], [R("IJG")])
                      posf = pos[:].rearrange("p h k -> p (h k)")
                      V(lambda e: e.tensor_scalar(out=pab[:, 0, :], in0=posf, scalar1=cu32_t[:, 0:1], scalar2=None,
                                                  op0=ALU.logical_shift_right), [R("pos"), R("cu32")], [R("pab")])
                      V(lambda e: e.tensor_scalar(out=pab[:, 1, :], in0=posf, scalar1=cu32_t[:, 1:2], scalar2=None,
                                                  op0=ALU.bitwise_and), [R("pos"), R("cu32")], [R("pab")])
                      V(lambda e: e.tensor_copy(out=pabf[:], in_=pab[:]), [R("pab")], [R("pabf")])
                      eq = cand[:].rearrange("p h (a b) -> p h a b", b=16)
                      io = iota_t[:, 0:16].unsqueeze(1).unsqueeze(1).to_broadcast([128, 8, 16, 16])
                      for w in range(2):
                          sel = pabf[:, w, :].rearrange("p (h k) -> p h k", k=16).unsqueeze(3).to_broadcast([128, 8, 16, 16])
                          V(lambda e: e.tensor_tensor(out=eq, in0=sel, in1=io, op=ALU.is_equal),
                            [R("pabf"), R("iota"), R("cand")], [R("cand")])
                          V(lambda e: e.tensor_tensor(out=eq, in0=eq, in1=i4[:, :, w, :].unsqueeze(2).to_broadcast([128, 8, 16, 16]),
                                                      op=ALU.mult), [R("cand"), R("ixf")], [R("cand")])
                          V(lambda e: e.tensor_reduce(out=IJG[:, w, :].rearrange("p (h k) -> p h k", k=16), in_=eq,
                                                      axis=AX.X, op=ALU.add), [R("cand")], [R("IJG")])
                      if DEBUG["sel"] and gi == 0 and tt == 0:
                          S.dma("sp", dbg["sel"][:, :, :], IJG[:], reads=[R("IJG")], writes=[R("dbg_sel")])
                      for w in range(3):
                          PE(lambda e: e.transpose(out=psum[:, 4, w * 128:(w + 1) * 128], in_=IJG[:, w, :], identity=ident_f[:]),
                             [R("IJG"), R("ident_f")], [RB(4)])
                      A(lambda e: e.activation(out=IJGT[:, :, tsl], in_=psum[:, 4, 0:384].rearrange("p (w t) -> p w t", t=128),
                                               func=AF.Copy), [RB(4)], [R("IJGT")])
                      for bt in range(128 // NB):
                          i2 = scnt % 2
                          scnt += 1
                          Px_, Qx_ = Pmx[i2], Qmx[i2]
                          rPx, rQx = R("Pmx", i2), R("Qmx", i2)
                          wb0 = 4 * (i2)
                          tb0 = tt * 128 + bt * NB
                          iob = iota_t[:, :].unsqueeze(1).to_broadcast([128, NB, 128])
                          V(lambda e: e.tensor_tensor(out=Px_[:], in0=IJGT[:, 0, tb0:tb0 + NB].unsqueeze(2).to_broadcast([128, NB, 128]),
                                                      in1=iob, op=ALU.is_equal), [R("IJGT"), R("iota")], [rPx])
                          V(lambda e: e.tensor_tensor(out=Qx_[:], in0=IJGT[:, 1, tb0:tb0 + NB].unsqueeze(2).to_broadcast([128, NB, 128]),
                                                      in1=iob, op=ALU.is_equal), [R("IJGT"), R("iota")], [rQx])
                          V(lambda e: e.tensor_tensor(out=Qx_[:], in0=Qx_[:],
                                                      in1=IJGT[:, 2, tb0:tb0 + NB].unsqueeze(2).to_broadcast([128, NB, 128]),
                                                      op=ALU.mult), [R("IJGT"), rQx], [rQx])
                          for tl in range(NB):
                              bank = 4 + tl // 4
                              off = (tl % 4) * 128
                              PE(lambda e: e.matmul(psum[:, bank, off:off + 128], lhsT=Px_[:, tl, :], rhs=Qx_[:, tl, :],
                                                    start=True, stop=True), [rPx, rQx], [RB(bank)])
                          tb = tt * 128 + bt * NB
                          A(lambda e: e.activation(out=Wsum[:, :, tb:tb + NB],
                                                   in_=psum[:, 4:8, :].rearrange("p a (t j) -> p j (a t)", j=128),
                                                   func=AF.Copy), [RB(4), RB(5), RB(6), RB(7)], [R("Wsum")])
                  for b in range(4):
                      PE(lambda e: e.matmul(psum[:, b, :], lhsT=zeros_b[:, 0:128], rhs=zeros_b[:], start=True, stop=False,
                                            skip_group_check=True), [R("zeros_b")], [RB(b)])
                  for j in range(128):
                      sl = acnt % NS
                      i2 = acnt % 2
                      acnt += 1
                      S.dma("sp", ub[sl][:].rearrange("p a b -> p (a b)"), uTb[j], reads=[R("scr_u", j)], writes=[R("ub", sl)])
                      S.dma("sp", vb[sl][:], vtb[j], reads=[R("scr_v", j)], writes=[R("vb", sl)])
                      ab = 4 + i2
                      for dc in range(8):
                          PE(lambda e: e.matmul(psum[:, ab, 0:TG], lhsT=ub[sl][:, dc, :], rhs=h2T[:, dc, :],
                                                start=(dc == 0), stop=(dc == 7)), [R("ub", sl), R("h2T")], [RB(ab)])
                      A(lambda e: e.activation(out=ge[i2][:], in_=psum[:, ab, 0:TG], func=AF.Gelu), [RB(ab)], [R("ge", i2)])
                      V(lambda e: e.tensor_tensor(out=Lj[i2][:], in0=ge[i2][:], in1=Wsum[:, j, :], op=ALU.mult),
                        [R("ge", i2), R("Wsum")], [R("Lj", i2)])
                      for dc in range(8):
                          PE(lambda e: e.matmul(psum[:, dc // 2, (dc % 2) * TG:(dc % 2 + 1) * TG],
                                                lhsT=vb[sl][:, dc * 128:(dc + 1) * 128], rhs=Lj[i2][:],
                                                start=False, stop=(j == 127), skip_group_check=True),
                             [R("vb", sl), R("Lj", i2)], [RB(dc // 2)])
                  for dc in range(8):
                      V(lambda e: e.scalar_tensor_tensor(out=x1g[:, dc, :], in0=psum[:, dc // 2, (dc % 2) * TG:(dc % 2 + 1) * TG],
                                                         scalar=modp[:, 40 + dc, s:s + 1], in1=x1g[:, dc, :],
                                                         op0=ALU.mult, op1=ALU.add), [RB(dc // 2), R("x1g"), R("modp")], [R("x1g")])
                  norm_group(x1g[:], R("x1g"), TG, sq2, rstd2, None, 4,
                             lambda c: fing_t[:, c:c + 1], None,
                             lambda c: x1g[:, c, :], R("x1g"), True)
                  S.dma("sp", outT[:, :, t0:t0 + TG], x1g[:], reads=[R("x1g")], writes=[R("outT", gi)])
        except _Stop:
            pass
        build.stats = dict(S.cnt); build.stats["dma"] = dict(S.dq)
        S.finish([R("outT", gi) for gi in range(NG)] + ([R("dbg_x1")] if DEBUG["x1"] else [])
                 + ([R("dbg_sel")] if DEBUG["sel"] else []) + [R("x1T", i) for i in range(8)])
    return nc


def _fm(a):
    rows, D = a.shape
    return np.ascontiguousarray(a.T.reshape(D // 128, 128, rows).transpose(1, 0, 2))


def _partner(d):
    return d + 16 if (d % 32) < 16 else d - 16


def prep_shared(inp):
    f = np.float32
    sh = {}
    w_mod = np.asarray(inp["w_mod"], f)[0]
    sh["w_mod"] = np.ascontiguousarray(w_mod.reshape(8, 128, 6144).transpose(1, 0, 2))
    sh["b_mod"] = np.ascontiguousarray(np.asarray(inp["b_mod"], f)[0].reshape(48, 128).T)
    sh["n1g"] = np.ascontiguousarray(np.asarray(inp["norm1_g"], f)[0].reshape(8, 128).T)
    sh["n2g"] = np.ascontiguousarray(np.asarray(inp["norm2_g"], f)[0].reshape(8, 128).T)
    sh["fing"] = np.ascontiguousarray(np.asarray(inp["final_g"], f).reshape(8, 128).T)
    w_in = np.asarray(inp["w_in"], f)[0]
    pr = np.array([_partner(d) for d in range(64)])
    chunks = []
    for c in range(4):
        chunks.append(np.concatenate([c * 64 + np.arange(64), (4 + c) * 64 + np.arange(64)]))
    for c in range(4):
        chunks.append(np.concatenate([c * 64 + pr, (4 + c) * 64 + pr]))
    chunks.append(512 + np.arange(128))
    chunks.append(512 + np.concatenate([pr, 64 + pr]))
    chunks.append(640 + np.arange(128))
    GB, GC, UU, GA, GV = 768, 1280, 1792, 2304, 3328
    for c in range(4):
        chunks.append(GC + c * 128 + np.arange(128))
        chunks.append(UU + c * 128 + np.arange(128))
        chunks.append(GB + c * 128 + np.arange(128))
    for c in range(8):
        chunks.append(GA + c * 128 + np.arange(128))
    for c in range(8):
        chunks.append(GV + c * 128 + np.arange(128))
    assert len(chunks) == 39
    wl = np.empty((39, 128, 8, 128), f)
    for i, cols in enumerate(chunks):
        wl[i] = w_in[:, cols].reshape(8, 128, 128).transpose(1, 0, 2)
    sh["w_in"] = wl
    sh["w_ao"] = np.ascontiguousarray(np.asarray(inp["w_attn_out"], f)[0].reshape(8, 64, 1024).transpose(1, 0, 2))
    sh["w_co"] = np.ascontiguousarray(np.asarray(inp["w_conv_out"], f)[0].reshape(4, 128, 1024).transpose(1, 0, 2))
    sh["w_mo"] = np.ascontiguousarray(np.asarray(inp["w_mix_out"], f)[0].reshape(8, 128, 1024).transpose(1, 0, 2))
    sh["conv_w"] = np.ascontiguousarray(np.asarray(inp["conv_w"], f)[0].reshape(3, 4, 128).transpose(2, 1, 0))
    sh["sinkv"] = np.ascontiguousarray(np.broadcast_to(np.asarray(inp["attn_sink"], f)[0][None, :], (128, 8)))
    sh["pw_q"] = np.ascontiguousarray(np.asarray(inp["peer_w_q"], f)[0].reshape(8, 128, 2048).transpose(1, 0, 2))
    sk = np.asarray(inp["peer_sub_keys"], f)[0]
    sh["keysT"] = np.ascontiguousarray(sk.reshape(16, 128, 128).transpose(2, 0, 1))
    pu = np.asarray(inp["peer_u"], f)[0]
    sh["uT"] = np.ascontiguousarray(pu.reshape(128, 128, 8, 128).transpose(1, 3, 2, 0)).reshape(128, 128, 1024)
    pv = np.asarray(inp["peer_v"], f)[0]
    sh["vt"] = np.ascontiguousarray(pv.reshape(128, 128, 1024).transpose(1, 0, 2))
    inv_freq = (np.float32(10000.0) ** (-np.arange(16, dtype=f) / np.float32(16))).astype(f)
    l = np.arange(L)
    cos_t = np.empty((128, L), f)
    sin_t = np.empty((128, L), f)
    for p in range(128):
        d = p % 64
        posn = (l % 64) if d >= 32 else (l // 64)
        ang = posn.astype(f) * inv_freq[d % 16]
        cos_t[p] = np.cos(ang)
        sin_t[p] = np.sin(ang) * (-1.0 if (d % 32) < 16 else 1.0)
    sh["rope_cos"] = cos_t
    sh["rope_sin"] = sin_t
    qi = np.arange(128)[:, None]
    kk = np.arange(384)[None, :]
    sh["amask"] = np.where((kk >= qi) & (kk <= qi + 256), 0.0, NEG).astype(f)
    sh["ident"] = np.eye(128, dtype=f)
    sh["iota128"] = np.ascontiguousarray(np.broadcast_to(np.arange(128, dtype=f)[None, :], (128, 128)))
    sh["cu32"] = np.ascontiguousarray(np.broadcast_to(np.array([4, 15], np.uint32)[None, :], (128, 2)))
    return sh


def prep_core(inp, core, sh):
    f = np.float32
    x = np.asarray(inp["x"], f)
    ctx = np.asarray(inp["ctx"], f)
    c = np.asarray(inp["c"], f)
    c_ctx = np.asarray(inp["c_ctx"], f)
    m = dict(sh)
    b0 = 2 * core
    m["xT"] = _fm(x[b0:b0 + 2].reshape(2 * L, 1024))
    m["ctxT"] = _fm(ctx[b0:b0 + 2].reshape(512, 1024))
    m["cT"] = _fm(np.stack([c[b0], c[b0 + 1], c_ctx], 0))
    return m


_NC_CACHE = {}


def kernel(**inputs):
    sh = prep_shared(inputs)
    in_maps = [prep_core(inputs, core, sh) for core in range(NCORES)]
    if "nc" not in _NC_CACHE:
        _NC_CACHE["nc"] = build()
    nc = _NC_CACHE["nc"]
    res = run_bass_kernel_spmd(nc, in_maps, core_ids=list(range(NCORES)))
    out = np.empty((16, L, 1024), np.float32)
    for core in range(NCORES):
        o = np.asarray(res.results[core]["outT"])
        o = o.transpose(2, 1, 0).reshape(2, L, 1024)
        out[2 * core:2 * core + 2] = o
    return out
```

```python
import numpy as np
from contextlib import ExitStack
import concourse.bass as bass
import concourse.mybir as mybir
from concourse.bass_utils import run_bass_kernel_spmd

F32 = mybir.dt.float32
BF16 = mybir.dt.bfloat16
U32 = mybir.dt.uint32
AF = mybir.ActivationFunctionType
ALU = mybir.AluOpType
AX = mybir.AxisListType

NCORES = 8
L = 2048
TPC = 2 * L
EPS = 1e-6
NEG = -1e30
DEBUG = {"x1": False, "sel": False, "stop_after_mixer": False, "stop": None, "no_prologue": False}


class _Stop(Exception):
    pass


class Res:
    __slots__ = ("name", "w", "r")

    def __init__(self, name):
        self.name = name
        self.w = None
        self.r = {}


class Sched:
    ENG = ("pe", "act", "dve", "pool", "sp")

    def __init__(self, nc, stack, ndma=12):
        self.nc = nc
        self.e = {"pe": nc.tensor, "act": nc.scalar, "dve": nc.vector, "pool": nc.gpsimd, "sp": nc.sync}
        self.sem = {}
        self.cnt = {}
        for k in self.ENG:
            self.sem[k] = stack.enter_context(nc.semaphore("sem_" + k))
            self.cnt[k] = 0
        self.ndma = ndma
        self.dq = {}
        for q in ("sp", "pool"):
            for i in range(ndma):
                self.sem[(q, i)] = stack.enter_context(nc.semaphore(f"dsem_{q}_{i}"))
            self.dq[q] = 0
        self.waited = {k: {} for k in self.ENG}
        self.dead = False

    def _wait(self, eng, tok):
        key, val = tok
        if self.waited[eng].get(key, 0) >= val:
            return
        self.e[eng].wait_ge(self.sem[key], val)
        self.waited[eng][key] = val

    def _deps(self, eng, reads, writes, attach=False):
        toks = {}

        def add(t):
            if t is None:
                return
            k, v = t
            if toks.get(k, 0) < v:
                toks[k] = v
        for r in reads:
            add(r.w)
        for w in writes:
            add(w.w)
            for k, v in w.r.items():
                add((k, v))
        need = [(k, v) for k, v in toks.items() if self.waited[eng].get(k, 0) < v and not (eng == "pe" and k == "pe")]
        if attach and need:
            last = need.pop()
        else:
            last = None
        for k, v in need:
            self._wait(eng, (k, v))
        return last

    def _attach(self, eng, ins, last):
        if last is not None:
            ins._wait_ge(self.sem[last[0]], last[1])
            self.waited[eng][last[0]] = last[1]

    def _commit(self, tok, reads, writes):
        k, v = tok
        for r in reads:
            if r.r.get(k, 0) < v:
                r.r[k] = v
        for w in writes:
            w.w = tok
            w.r = {}

    def op(self, eng, fn, reads=(), writes=()):
        if self.dead:
            return None
        last = self._deps(eng, reads, writes, attach=True)
        ins = fn(self.e[eng])
        self._attach(eng, ins, last)
        self.cnt[eng] += 1
        ins.then_inc(self.sem[eng], 1)
        tok = (eng, self.cnt[eng])
        self._commit(tok, reads, writes)
        return tok

    def dma(self, q, out, in_, reads=(), writes=()):
        if self.dead:
            return None
        n = self.dq[q]
        self.dq[q] += 1
        slot = n % self.ndma
        rnd = n // self.ndma
        key = (q, slot)
        if rnd > 0:
            self._wait(q, (key, 16 * rnd))
        last = self._deps(q, reads, writes, attach=True)
        ins = self.e[q].dma_start(out=out, in_=in_)
        self._attach(q, ins, last)
        ins.then_inc(self.sem[key], 16)
        tok = (key, 16 * (rnd + 1))
        self._commit(tok, reads, writes)
        return tok

    def barrier(self):
        if self.dead:
            return
        toks = [(k, self.cnt[k]) for k in self.ENG if self.cnt[k] > 0]
        for q, n in self.dq.items():
            for slot in range(min(n, self.ndma)):
                rnd = (n - 1 - slot) // self.ndma
                toks.append(((q, slot), 16 * (rnd + 1)))
        for e in self.ENG:
            for t in toks:
                self._wait(e, t)

    def finish(self, ress):
        for r in ress:
            if r.w is not None:
                self._wait("sp", r.w)


def build():
    nc = bass.Bass("TRN2", target_bir_lowering=False)

    def din(name, shape, dt=F32):
        return nc.dram_tensor(name, list(shape), dt, kind="ExternalInput").ap()

    xT = din("xT", [128, 8, TPC])
    ctxT = din("ctxT", [128, 8, 512])
    cT = din("cT", [128, 8, 3])
    w_mod = din("w_mod", [128, 8, 6144])
    b_mod = din("b_mod", [128, 48])
    n1g = din("n1g", [128, 8])
    n2g = din("n2g", [128, 8])
    fing = din("fing", [128, 8])
    w_in = din("w_in", [39, 128, 8, 128])
    w_ao = din("w_ao", [64, 8, 1024])
    w_co = din("w_co", [128, 4, 1024])
    w_mo = din("w_mo", [128, 8, 1024])
    conv_w = din("conv_w", [128, 4, 3])
    sinkv = din("sinkv", [128, 8])
    pw_q = din("pw_q", [128, 8, 2048])
    keysT = din("keysT", [128, 16, 128])
    uT = din("uT", [128, 128, 1024])
    vt = din("vt", [128, 128, 1024])
    rope_cos = din("rope_cos", [128, L])
    rope_sin = din("rope_sin", [128, L])
    amask = din("amask", [128, 384])
    ident = din("ident", [128, 128])
    iota128 = din("iota128", [128, 128])
    cu32 = din("cu32", [128, 2], U32)
    outT = nc.dram_tensor("outT", [128, 8, TPC], F32, kind="ExternalOutput").ap()
    x1T = nc.dram_tensor("x1T_scr", [128, 8, TPC], F32, kind="Internal").ap()
    uTb = nc.dram_tensor("uTb_scr", [128, 128, 1024], BF16, kind="Internal").ap()
    vtb = nc.dram_tensor("vtb_scr", [128, 128, 1024], BF16, kind="Internal").ap()
    dbg = {}
    if DEBUG["x1"]:
        dbg["x1"] = nc.dram_tensor("dbg_x1", [128, 8, TPC], F32, kind="ExternalOutput").ap()
    if DEBUG["sel"]:
        dbg["sel"] = nc.dram_tensor("dbg_sel", [128, 3, 128], F32, kind="ExternalOutput").ap()

    with ExitStack() as st:
        S = Sched(nc, st)
        NG = 0
        RES = {}

        def R(*key):
            r = RES.get(key)
            if r is None:
                r = RES[key] = Res(str(key))
            return r

        _uid = [0]

        def sb(stack, name, shape, dt):
            _uid[0] += 1
            return stack.enter_context(nc.sbuf_tensor(f"{name}_{_uid[0]}", list(shape), dt))

        def V(fn, reads, writes):
            return S.op("dve", fn, reads, writes)

        def A(fn, reads, writes):
            return S.op("act", fn, reads, writes)

        def G(fn, reads, writes):
            return S.op("pool", fn, reads, writes)

        def PE(fn, reads, writes):
            return S.op("pe", fn, reads, writes)

        psum = st.enter_context(nc.psum_tensor("psum", [128, 8, 512], F32))

        def RB(b):
            return R("psb", b)

        ident_f = sb(st, "ident_f", [128, 128], F32)
        ident_b = sb(st, "ident_b", [128, 128], BF16)
        ones_b = sb(st, "ones_b", [128, 128], BF16)
        zeros_b = sb(st, "zeros_b", [128, 512], BF16)
        iota_t = sb(st, "iota_t", [128, 128], F32)
        cu32_t = sb(st, "cu32_t", [128, 2], U32)
        eps_t = sb(st, "eps_t", [128, 1], F32)
        modp = sb(st, "modp", [128, 48, 3], F32)
        A1 = sb(st, "A1", [128, 8, 3], F32)
        A2 = sb(st, "A2", [128, 8, 3], F32)
        bmod_t = sb(st, "bmod_t", [128, 48], F32)
        n1g_t = sb(st, "n1g_t", [128, 8], F32)
        n2g_t = sb(st, "n2g_t", [128, 8], F32)
        fing_t = sb(st, "fing_t", [128, 8], F32)
        sink_t = sb(st, "sink_t", [128, 8], F32)
        nsink_t = sb(st, "nsink_t", [128, 8], F32)
        convw_t = sb(st, "convw_t", [128, 4, 3], F32)

        S.dma("sp", ident_f[:], ident[:, :], writes=[R("ident_f")])
        S.dma("sp", iota_t[:], iota128[:, :], writes=[R("iota")])
        S.dma("sp", cu32_t[:], cu32[:, :], writes=[R("cu32")])
        S.dma("sp", bmod_t[:], b_mod[:, :], writes=[R("bmod")])
        S.dma("sp", n1g_t[:], n1g[:, :], writes=[R("n1g")])
        S.dma("sp", n2g_t[:], n2g[:, :], writes=[R("n2g")])
        S.dma("sp", fing_t[:], fing[:, :], writes=[R("fing")])
        S.dma("sp", sink_t[:], sinkv[:, :], writes=[R("sink")])
        S.dma("sp", convw_t[:], conv_w[:, :, :], writes=[R("convw")])
        V(lambda e: e.tensor_copy(out=ident_b[:], in_=ident_f[:]), [R("ident_f")], [R("ident_b")])
        V(lambda e: e.memset(ones_b[:], 1.0), [], [R("ones_b")])
        V(lambda e: e.memset(zeros_b[:], 0.0), [], [R("zeros_b")])
        V(lambda e: e.memset(eps_t[:], EPS), [], [R("eps")])
        V(lambda e: e.tensor_scalar(out=nsink_t[:], in0=sink_t[:], scalar1=-1.0, scalar2=None, op0=ALU.mult),
          [R("sink")], [R("nsink")])

        NPB = 2
        pro_stack = st.enter_context(ExitStack())
        pst = [sb(pro_stack, f"pst{i}", [128, 1024], F32) for i in range(NPB)]
        pbf = [sb(pro_stack, f"pbf{i}", [128, 1024], BF16) for i in range(NPB)]
        pro_state = {"k": 0}
        NPRO = 0 if DEBUG["no_prologue"] else 256

        def pro_src_dst(k):
            tab, j = divmod(k, 128)
            if tab == 0:
                return uT[j], uTb[j], R("scr_u", j)
            return vt[j], vtb[j], R("scr_v", j)

        def prologue_step():
            k = pro_state["k"]
            if k >= NPRO + 1:
                return False
            if k < NPRO:
                src, _, _ = pro_src_dst(k)
                S.dma("sp", pst[k % NPB][:], src, writes=[R("pst", k % NPB)])
            k2 = k - 1
            if 0 <= k2 < NPRO:
                _, dst, rr = pro_src_dst(k2)
                sl = k2 % NPB
                G(lambda e: e.tensor_copy(out=pbf[sl][:], in_=pst[sl][:]), [R("pst", sl)], [R("pbf", sl)])
                S.dma("sp", dst, pbf[sl][:], reads=[R("pbf", sl)], writes=[rr])
            pro_state["k"] = k + 1
            return True

        def prologue_steps(n):
            for _ in range(n):
                if not prologue_step():
                    break

        def norm_group(xs, rxs, N, sq, rstd, tmp2, bank, Acol, Bcol, outc, rout, in_place):
            A(lambda e: e.activation(out=sq[:, :, 0:N], in_=xs, func=AF.Square), [rxs], [R("sq")])
            for c in range(8):
                PE(lambda e: e.matmul(psum[:, bank, 0:N], lhsT=ones_b[:], rhs=sq[:, c, 0:N],
                                      start=(c == 0), stop=(c == 7)), [R("sq"), R("ones_b")], [RB(bank)])
            A(lambda e: e.activation(out=rstd[:, 0:N], in_=psum[:, bank, 0:N], func=AF.Sqrt,
                                     scale=1.0 / 1024.0, bias=eps_t[:, 0:1]), [RB(bank), R("eps")], [R("rstd")])
            V(lambda e: e.reciprocal(out=rstd[:, 0:N], in_=rstd[:, 0:N]), [R("rstd")], [R("rstd")])
            for c in range(8):
                if in_place:
                    t = xs[:, c, :]
                    rt = rxs
                else:
                    t = tmp2[c % 2][:, 0:N]
                    rt = R("ntmp", c % 2)
                V(lambda e: e.tensor_tensor(out=t, in0=xs[:, c, :], in1=rstd[:, 0:N], op=ALU.mult),
                  [rxs, R("rstd")], [rt])
                b = Bcol(c) if Bcol is not None else None
                if b is not None:
                    V(lambda e: e.tensor_scalar(out=outc(c), in0=t, scalar1=Acol(c), scalar2=b,
                                                op0=ALU.mult, op1=ALU.add), [rt], [rout])
                else:
                    V(lambda e: e.tensor_scalar(out=outc(c), in0=t, scalar1=Acol(c), scalar2=None,
                                                op0=ALU.mult), [rt], [rout])

        with ExitStack() as p0:
            cTt = sb(p0, "cTt", [128, 8, 3], F32)
            scT = sb(p0, "scT", [128, 8, 3], F32)
            wm = [sb(p0, f"wm{i}", [128, 8, 512], F32) for i in range(2)]
            S.dma("sp", cTt[:], cT[:, :, :], writes=[R("cTt")])
            A(lambda e: e.activation(out=scT[:], in_=cTt[:], func=AF.Silu), [R("cTt")], [R("scT")])
            for pc in range(12):
                b = pc % 2
                S.dma("sp", wm[b][:], w_mod[:, :, pc * 512:(pc + 1) * 512], writes=[R("wm", b)])
                for cc in range(4):
                    j = pc * 4 + cc
                    for kc in range(8):
                        PE(lambda e: e.matmul(psum[:, 0, j * 3:(j + 1) * 3], lhsT=wm[b][:, kc, cc * 128:(cc + 1) * 128],
                                              rhs=scT[:, kc, :], start=(kc == 0), stop=(kc == 7)),
                           [R("wm", b), R("scT")], [RB(0)])
            V(lambda e: e.tensor_tensor(out=modp[:], in0=psum[:, 0, 0:144].rearrange("p (a b) -> p a b", b=3),
                                        in1=bmod_t[:, :].unsqueeze(2).to_broadcast([128, 48, 3]), op=ALU.add),
              [RB(0), R("bmod")], [R("modp")])
            for (Ax, off, gt, rg) in ((A1, 8, n1g_t, "n1g"), (A2, 32, n2g_t, "n2g")):
                V(lambda e: e.tensor_scalar(out=Ax[:], in0=modp[:, off:off + 8, :], scalar1=1.0, scalar2=None,
                                            op0=ALU.add), [R("modp")], [R("A12")])
                V(lambda e: e.tensor_tensor(out=Ax[:], in0=Ax[:], in1=gt[:, :].unsqueeze(2).to_broadcast([128, 8, 3]),
                                            op=ALU.mult), [R("A12"), R(rg)], [R("A12")])
        S.barrier()

        try:
          with ExitStack() as p1:
              NW = 5
              wst = [sb(p1, f"wst{i}", [128, 8, 128], F32) for i in range(NW)]
              wbf = [sb(p1, f"wbf{i}", [128, 8, 128], BF16) for i in range(NW)]
              wcount = [0]

              def load_w(idx):
                  i = wcount[0] % NW
                  wcount[0] += 1
                  S.dma("sp", wst[i][:], w_in[idx], writes=[R("wst", i)])
                  G(lambda e: e.tensor_copy(out=wbf[i][:], in_=wst[i][:]), [R("wst", i)], [R("wbf", i)])
                  return wbf[i], R("wbf", i)

              class WStream:
                  def __init__(self, order):
                      self.order = order
                      self.pos = 0
                      self.q = []

                  def prefetch(self, n):
                      while len(self.q) < n and self.pos < len(self.order):
                          self.q.append(load_w(self.order[self.pos]))
                          self.pos += 1

                  def get(self):
                      self.prefetch(1)
                      return self.q.pop(0)

              hT = sb(p1, "hT", [128, 8, L], BF16)
              hcT = sb(p1, "hcT", [128, 8, 256], BF16)
              attnT = sb(p1, "attnT", [64, 8, L], BF16)
              convo = sb(p1, "convo", [128, 4, L], BF16)
              rstd = sb(p1, "rstd", [128, 512], F32)

              def load_big(dst_fn, src_fn, npieces, parts, rname):
                  for pc in range(npieces):
                      i = wcount[0] % NW
                      wcount[0] += 1
                      stg = wst[i][:].rearrange("p a b -> p (a b)")
                      S.dma("sp", stg[0:parts, :], src_fn(pc), writes=[R("wst", i)])
                      G(lambda e: e.tensor_copy(out=dst_fn(pc), in_=stg[0:parts, :]), [R("wst", i)], [R(rname)])


              for s in range(2):
                  order = [0, 4, 1, 5, 2, 6, 3, 7, 8, 9, 10] + list(range(11, 23))
                  for tg in range(4):
                      for oc in range(8):
                          order += [23 + oc, 31 + oc]
                  ws = WStream(order)
                  ws.prefetch(2)

                  with ExitStack() as psa:
                      xs = sb(psa, "xs", [128, 8, 512], F32)
                      sq = sb(psa, "sq", [128, 8, 512], BF16)
                      for tg in range(4):
                          S.dma("sp", xs[:], xT[:, :, s * L + tg * 512: s * L + (tg + 1) * 512], writes=[R("xs")])
                          norm_group(xs[:], R("xs"), 512, sq, rstd, None, tg % 2,
                                     lambda c: A1[:, c, s:s + 1], lambda c: modp[:, c, s:s + 1],
                                     lambda c: hT[:, c, tg * 512:(tg + 1) * 512], R("hT", tg), True)
                      S.dma("sp", xs[:, :, 0:256], ctxT[:, :, s * 256:(s + 1) * 256], writes=[R("xs")])
                      norm_group(xs[:, :, 0:256], R("xs"), 256, sq, rstd, None, 0,
                                 lambda c: A1[:, c, 2:3], lambda c: modp[:, c, 2:3],
                                 lambda c: hcT[:, c, :], R("hcT"), True)
                      prologue_steps(8)
                  S.barrier()
                  if DEBUG["stop"] == "A":
                      S.dead = True

                  with ExitStack() as pa:
                      qT = sb(pa, "qT", [128, 4, L], BF16)
                      kT = sb(pa, "kT", [128, 256 + L], BF16)
                      vtok = sb(pa, "vtok", [128, 18, 128], BF16)
                      amask_t = sb(pa, "amask_t", [128, 384], F32)
                      pb_ = ExitStack()
                      cos_t = sb(pb_, "cos_t", [128, L], F32)
                      sin_t = sb(pb_, "sin_t", [128, L], F32)
                      t1 = [sb(pb_, f"t1_{i}", [128, 512], F32) for i in range(2)]
                      t2 = [sb(pb_, f"t2_{i}", [128, 512], F32) for i in range(2)]
                      S.dma("sp", cos_t[:], rope_cos[:, :], writes=[R("cos")])
                      S.dma("sp", sin_t[:], rope_sin[:, :], writes=[R("sin")])
                      S.dma("sp", amask_t[:], amask[:, :], writes=[R("amask")])

                      it = 0

                      def proj_rope(wa, ra, wb_, rb_, dst_fn, rdst_fn):
                          nonlocal it
                          for tg in range(4):
                              ba, bb = (2, 3) if it % 2 == 0 else (4, 5)
                              tt1, tt2 = t1[it % 2], t2[it % 2]
                              r1, r2 = R("t1", it % 2), R("t2", it % 2)
                              it += 1
                              for kc in range(8):
                                  PE(lambda e: e.matmul(psum[:, ba, :], lhsT=wa[:, kc, :], rhs=hT[:, kc, tg * 512:(tg + 1) * 512],
                                                        start=(kc == 0), stop=(kc == 7)), [ra, R("hT", tg)], [RB(ba)])
                              for kc in range(8):
                                  PE(lambda e: e.matmul(psum[:, bb, :], lhsT=wb_[:, kc, :], rhs=hT[:, kc, tg * 512:(tg + 1) * 512],
                                                        start=(kc == 0), stop=(kc == 7)), [rb_, R("hT", tg)], [RB(bb)])
                              V(lambda e: e.tensor_tensor(out=tt1[:], in0=psum[:, ba, :], in1=cos_t[:, tg * 512:(tg + 1) * 512],
                                                          op=ALU.mult), [RB(ba), R("cos")], [r1])
                              V(lambda e: e.tensor_tensor(out=tt2[:], in0=psum[:, bb, :], in1=sin_t[:, tg * 512:(tg + 1) * 512],
                                                          op=ALU.mult), [RB(bb), R("sin")], [r2])
                              G(lambda e: e.tensor_tensor(out=dst_fn(tg), in0=tt1[:], in1=tt2[:], op=ALU.add),
                                [r1, r2], [rdst_fn(tg)])

                      for ch in range(4):
                          (wq, rq) = ws.get()
                          (wqs, rqs) = ws.get()
                          ws.prefetch(2)
                          proj_rope(wq, rq, wqs, rqs, lambda tg: qT[:, ch, tg * 512:(tg + 1) * 512],
                                    lambda tg: R("qT", ch, tg))
                          prologue_steps(4)
                      (wk, rk) = ws.get()
                      (wks, rks) = ws.get()
                      ws.prefetch(2)
                      for kc in range(8):
                          PE(lambda e: e.matmul(psum[:, 6, 0:256], lhsT=wk[:, kc, :], rhs=hcT[:, kc, :],
                                                start=(kc == 0), stop=(kc == 7)), [rk, R("hcT")], [RB(6)])
                      A(lambda e: e.activation(out=kT[:, 0:256], in_=psum[:, 6, 0:256], func=AF.Copy), [RB(6)], [R("kT", "ctx")])
                      proj_rope(wk, rk, wks, rks, lambda tg: kT[:, 256 + tg * 512: 256 + (tg + 1) * 512],
                                lambda tg: R("kT", tg))
                      (wv, rv) = ws.get()
                      ws.prefetch(3)
                      for blk in range(18):
                          bank = 6 + blk % 2
                          for kc in range(8):
                              if blk < 2:
                                  lh = hcT[:, kc, blk * 128:(blk + 1) * 128]
                                  rl = R("hcT")
                              else:
                                  lh = hT[:, kc, (blk - 2) * 128:(blk - 1) * 128]
                                  rl = R("hT", (blk - 2) // 4)
                              PE(lambda e: e.matmul(psum[:, bank, 0:128], lhsT=lh, rhs=wv[:, kc, :],
                                                    start=(kc == 0), stop=(kc == 7)), [rl, rv], [RB(bank)])
                          A(lambda e: e.activation(out=vtok[:, blk, :], in_=psum[:, bank, 0:128], func=AF.Copy),
                            [RB(bank)], [R("vtok", blk)])
                      prologue_steps(8)
                      pb_.close()
                      S.barrier()
                      if DEBUG["stop"] == "B":
                          S.dead = True

                      with ExitStack() as pc_:
                          sc = [sb(pc_, f"sc{i}", [128, 640], F32) for i in range(2)]
                          Pm = [sb(pc_, f"Pm{i}", [128, 640], BF16) for i in range(2)]
                          sm = [sb(pc_, f"sm{i}", [128, 8], F32) for i in range(2)]
                          dg = [sb(pc_, f"dg{i}", [128, 128], BF16) for i in range(2)]
                          PTs = [sb(pc_, f"PTs{i}", [128, 5, 4, 128], BF16) for i in range(2)]
                          hcnt = 0
                          pvc = 0
                          for n in range(16):
                              lo = max(n - 1, 0)
                              hi = min(n + 1, 15)
                              nlb = hi - lo + 1
                              nloc = nlb * 128
                              nk = nloc + 256
                              nkb = nlb + 2
                              moff = (lo - (n - 1)) * 128
                              krs = [R("kT", "ctx")] + [R("kT", t) for t in sorted(set([(lo * 128) // 512, (hi * 128 + 127) // 512]))]
                              for g in range(2):
                                  psl = slice(g * 64, (g + 1) * 64)
                                  pts = PTs[pvc % 2]
                                  rpts = R("PTs", pvc % 2)
                                  for c in range(4):
                                      hq = g * 4 + c
                                      i2 = hcnt % 2
                                      sa, sbk = (0, 1) if i2 == 0 else (2, 3)
                                      hcnt += 1
                                      scx, Px, smx, dgx = sc[i2], Pm[i2], sm[i2], dg[i2]
                                      rsc, rP, rsm, rdg = R("sc", i2), R("Pm", i2), R("sm", i2), R("dg", i2)
                                      lq = qT[psl, c, n * 128:(n + 1) * 128]
                                      PE(lambda e: e.matmul(psum[:, sa, 0:nloc], lhsT=lq,
                                                            rhs=kT[psl, 256 + lo * 128: 256 + (hi + 1) * 128],
                                                            start=True, stop=True), [R("qT", c, n // 4)] + krs, [RB(sa)])
                                      PE(lambda e: e.matmul(psum[:, sbk, 0:256], lhsT=lq, rhs=kT[psl, 0:256],
                                                            start=True, stop=True), [R("qT", c, n // 4)] + krs, [RB(sbk)])
                                      V(lambda e: e.tensor_tensor(out=scx[:, 0:nloc], in0=psum[:, sa, 0:nloc],
                                                                  in1=amask_t[:, moff:moff + nloc], op=ALU.add),
                                        [RB(sa), R("amask")], [rsc])
                                      A(lambda e: e.activation(out=scx[:, nloc:nk], in_=psum[:, sbk, 0:256], func=AF.Copy),
                                        [RB(sbk)], [rsc])
                                      V(lambda e: e.reduce_max(out=smx[:, 0:1], in_=scx[:, 0:nk], axis=AX.X), [rsc], [rsm])
                                      V(lambda e: e.tensor_scalar(out=smx[:, 1:2], in0=smx[:, 0:1], scalar1=-0.125,
                                                                  scalar2=nsink_t[:, hq:hq + 1], op0=ALU.mult, op1=ALU.min),
                                        [rsm, R("nsink")], [rsm])
                                      A(lambda e: e.activation(out=Px[:, 0:nk], in_=scx[:, 0:nk], func=AF.Exp, scale=0.125,
                                                               bias=smx[:, 1:2], accum_out=smx[:, 2:3]), [rsc, rsm], [rP, rsm])
                                      A(lambda e: e.activation(out=smx[:, 3:4], in_=sink_t[:, hq:hq + 1], func=AF.Exp, scale=1.0,
                                                               bias=smx[:, 1:2]), [rsm, R("sink")], [rsm])
                                      V(lambda e: e.tensor_tensor(out=smx[:, 4:5], in0=smx[:, 2:3], in1=smx[:, 3:4], op=ALU.add),
                                        [rsm], [rsm])
                                      V(lambda e: e.reciprocal(out=smx[:, 5:6], in_=smx[:, 4:5]), [rsm], [rsm])
                                      V(lambda e: e.tensor_scalar(out=dgx[:], in0=ident_b[:], scalar1=smx[:, 5:6], scalar2=None,
                                                                  op0=ALU.mult), [rsm, R("ident_b")], [rdg])
                                      for kb in range(nkb):
                                          bank = 4 + kb // 4
                                          off = (kb % 4) * 128
                                          PE(lambda e: e.matmul(psum[:, bank, off:off + 128], lhsT=Px[:, kb * 128:(kb + 1) * 128],
                                                                rhs=dgx[:], start=True, stop=True), [rP, rdg], [RB(bank)])
                                      A(lambda e: e.activation(out=pts[:, 0:4, c, :],
                                                               in_=psum[:, 4, :].rearrange("p (k q) -> p k q", q=128),
                                                               func=AF.Copy), [RB(4)], [rpts])
                                      if nkb == 5:
                                          V(lambda e: e.tensor_copy(out=pts[:, 4, c, :], in_=psum[:, 5, 0:128]), [RB(5)], [rpts])
                                  ob = 6 + pvc % 2
                                  pvc += 1
                                  for kb in range(nkb):
                                      blk = (2 + lo + kb) if kb < nlb else (kb - nlb)
                                      PE(lambda e: e.matmul(psum[0:64, ob, :], lhsT=vtok[:, blk, g * 64:(g + 1) * 64],
                                                            rhs=pts[:, kb, :, :].rearrange("p c q -> p (c q)"),
                                                            start=(kb == 0), stop=(kb == nkb - 1)),
                                         [R("vtok", blk), rpts], [RB(ob)])
                                  A(lambda e: e.activation(out=attnT[:, g * 4:(g + 1) * 4, n * 128:(n + 1) * 128],
                                                           in_=psum[0:64, ob, :].rearrange("p (c q) -> p c q", q=128),
                                                           func=AF.Copy), [RB(ob)], [R("attnT", n // 4)])
                              prologue_steps(3)
                          S.barrier()
                  S.barrier()
                  if DEBUG["stop"] == "C":
                      S.dead = True

                  with ExitStack() as pd:
                      cu = sb(pd, "cu", [128, L + 2], F32)
                      yc = sb(pd, "yc", [128, L], F32)
                      gbs = sb(pd, "gbs", [128, L], F32)
                      ut = [sb(pd, f"ut{i}", [128, 512], F32) for i in range(2)]
                      G(lambda e: e.memset(cu[:, 0:1], 0.0), [], [R("cu_pad")])
                      G(lambda e: e.memset(cu[:, L + 1:L + 2], 0.0), [], [R("cu_pad")])
                      cnt = 0
                      for c in range(4):
                          (wg, rg) = ws.get()
                          (wu, ru) = ws.get()
                          (wb_, rb_) = ws.get()
                          ws.prefetch(2)
                          for tg in range(4):
                              bk = (0, 1, 2) if cnt % 2 == 0 else (3, 4, 5)
                              utx = ut[cnt % 2]
                              rut = R("ut", cnt % 2)
                              cnt += 1
                              for (w_, r_, b_) in ((wg, rg, bk[0]), (wu, ru, bk[1]), (wb_, rb_, bk[2])):
                                  for kc in range(8):
                                      PE(lambda e: e.matmul(psum[:, b_, :], lhsT=w_[:, kc, :], rhs=hT[:, kc, tg * 512:(tg + 1) * 512],
                                                            start=(kc == 0), stop=(kc == 7)), [r_, R("hT", tg)], [RB(b_)])
                              A(lambda e: e.activation(out=utx[:], in_=psum[:, bk[1], :], func=AF.Copy), [RB(bk[1])], [rut])
                              V(lambda e: e.tensor_tensor(out=cu[:, 1 + tg * 512: 1 + (tg + 1) * 512], in0=psum[:, bk[0], :],
                                                          in1=utx[:], op=ALU.mult), [RB(bk[0]), rut], [R("cu", tg)])
                              A(lambda e: e.activation(out=gbs[:, tg * 512:(tg + 1) * 512], in_=psum[:, bk[2], :], func=AF.Copy),
                                [RB(bk[2])], [R("gbs", tg)])
                          cur = [R("cu", t) for t in range(4)] + [R("cu_pad")]
                          V(lambda e: e.tensor_scalar(out=yc[:], in0=cu[:, 0:L], scalar1=convw_t[:, c, 0:1], scalar2=None,
                                                      op0=ALU.mult), cur + [R("convw")], [R("yc")])
                          V(lambda e: e.scalar_tensor_tensor(out=yc[:], in0=cu[:, 1:L + 1], scalar=convw_t[:, c, 1:2], in1=yc[:],
                                                             op0=ALU.mult, op1=ALU.add), cur + [R("yc")], [R("yc")])
                          V(lambda e: e.scalar_tensor_tensor(out=yc[:], in0=cu[:, 2:L + 2], scalar=convw_t[:, c, 2:3], in1=yc[:],
                                                             op0=ALU.mult, op1=ALU.add), cur + [R("yc")], [R("yc")])
                          G(lambda e: e.tensor_tensor(out=convo[:, c, :], in0=yc[:], in1=gbs[:], op=ALU.mult),
                            [R("yc")] + [R("gbs", t) for t in range(4)], [R("convo")])
                          prologue_steps(4)
                  S.barrier()
                  if DEBUG["stop"] == "D":
                      S.dead = True

                  with ExitStack() as pe_:
                      xs = sb(pe_, "xs", [128, 8, 512], F32)
                      w_ao_b = sb(pe_, "w_ao_b", [64, 8, 1024], BF16)
                      w_co_b = sb(pe_, "w_co_b", [128, 4, 1024], BF16)
                      w_mo_b = sb(pe_, "w_mo_b", [128, 8, 1024], BF16)
                      load_big(lambda pc: w_ao_b[:, pc, :], lambda pc: w_ao[:, pc, :], 8, 64, "w_ao_b")
                      load_big(lambda pc: w_co_b[:, pc, :], lambda pc: w_co[:, pc, :], 4, 128, "w_co_b")
                      load_big(lambda pc: w_mo_b[:, pc, :], lambda pc: w_mo[:, pc, :], 8, 128, "w_mo_b")
                      mixT = sb(pe_, "mixT", [128, 8, 512], BF16)
                      sg = [sb(pe_, f"sg{i}", [128, 512], F32) for i in range(2)]
                      mm_ = [sb(pe_, f"mm{i}", [128, 512], F32) for i in range(2)]
                      for tg in range(4):
                          tsl = slice(tg * 512, (tg + 1) * 512)
                          S.dma("sp", xs[:], xT[:, :, s * L + tg * 512: s * L + (tg + 1) * 512], writes=[R("xs")])
                          for oc in range(8):
                              (wga, rga) = ws.get()
                              (wgv, rgv) = ws.get()
                              ws.prefetch(2)
                              osl = slice(oc * 128, (oc + 1) * 128)
                              for h in range(8):
                                  PE(lambda e: e.matmul(psum[:, 0, :], lhsT=w_ao_b[:, h, osl], rhs=attnT[:, h, tsl],
                                                        start=(h == 0), stop=(h == 7)), [R("w_ao_b"), R("attnT", tg)], [RB(0)])
                              for c in range(4):
                                  PE(lambda e: e.matmul(psum[:, 1, :], lhsT=w_co_b[:, c, osl], rhs=convo[:, c, tsl],
                                                        start=(c == 0), stop=(c == 3)), [R("w_co_b"), R("convo")], [RB(1)])
                              for kc in range(8):
                                  PE(lambda e: e.matmul(psum[:, 2, :], lhsT=wga[:, kc, :], rhs=hT[:, kc, tsl],
                                                        start=(kc == 0), stop=(kc == 7)), [rga, R("hT", tg)], [RB(2)])
                              for kc in range(8):
                                  PE(lambda e: e.matmul(psum[:, 3, :], lhsT=wgv[:, kc, :], rhs=hT[:, kc, tsl],
                                                        start=(kc == 0), stop=(kc == 7)), [rgv, R("hT", tg)], [RB(3)])
                              A(lambda e: e.activation(out=sg[0][:], in_=psum[:, 2, :], func=AF.Sigmoid), [RB(2)], [R("sg", 0)])
                              A(lambda e: e.activation(out=sg[1][:], in_=psum[:, 3, :], func=AF.Sigmoid), [RB(3)], [R("sg", 1)])
                              V(lambda e: e.tensor_tensor(out=mm_[0][:], in0=psum[:, 0, :], in1=sg[0][:], op=ALU.mult),
                                [RB(0), R("sg", 0)], [R("mm", 0)])
                              V(lambda e: e.tensor_tensor(out=mm_[1][:], in0=psum[:, 1, :], in1=sg[1][:], op=ALU.mult),
                                [RB(1), R("sg", 1)], [R("mm", 1)])
                              G(lambda e: e.tensor_tensor(out=mixT[:, oc, :], in0=mm_[0][:], in1=mm_[1][:], op=ALU.add),
                                [R("mm", 0), R("mm", 1)], [R("mixT")])
                          for oc in range(8):
                              ob = 4 + oc % 2
                              osl = slice(oc * 128, (oc + 1) * 128)
                              for c in range(8):
                                  PE(lambda e: e.matmul(psum[:, ob, :], lhsT=w_mo_b[:, c, osl], rhs=mixT[:, c, :],
                                                        start=(c == 0), stop=(c == 7)), [R("w_mo_b"), R("mixT")], [RB(ob)])
                              V(lambda e: e.scalar_tensor_tensor(out=xs[:, oc, :], in0=psum[:, ob, :],
                                                                 scalar=modp[:, 16 + oc, s:s + 1], in1=xs[:, oc, :],
                                                                 op0=ALU.mult, op1=ALU.add), [RB(ob), R("xs"), R("modp")], [R("xs")])
                          S.dma("sp", x1T[:, :, s * L + tg * 512: s * L + (tg + 1) * 512], xs[:], reads=[R("xs")],
                                writes=[R("x1T", s * 4 + tg)])
                          if DEBUG["x1"]:
                              S.dma("sp", dbg["x1"][:, :, s * L + tg * 512: s * L + (tg + 1) * 512], xs[:], reads=[R("xs")],
                                    writes=[R("dbg_x1")])
                          prologue_steps(6)
                  S.barrier()

              while prologue_step():
                  pass
          pro_stack.close()
          S.barrier()

          TG = 256
          NG = TPC // TG
          if DEBUG["stop_after_mixer"]:
              NG = 0
          with ExitStack() as p2:
              pwq_b = sb(p2, "pwq_b", [128, 8, 2048], BF16)
              keys_b = sb(p2, "keys_b", [128, 16, 128], BF16)
              x1g = sb(p2, "x1g", [128, 8, TG], F32)
              sq2 = sb(p2, "sq2", [128, 8, TG], BF16)
              rstd2 = sb(p2, "rstd2", [128, TG], F32)
              ntmp = [sb(p2, f"ntmp{i}", [128, TG], F32) for i in range(2)]
              h2T = sb(p2, "h2T", [128, 8, TG], BF16)
              qpT = sb(p2, "qpT", [128, 16, TG], BF16)
              s1 = sb(p2, "s1", [128, 16, 128], F32)
              m16 = sb(p2, "m16", [128, 16, 16], F32)
              ix = sb(p2, "ix", [128, 16, 16], U32)
              ixf = sb(p2, "ixf", [128, 16, 16], F32)
              cand = sb(p2, "cand", [128, 8, 256], F32)
              b16 = sb(p2, "b16", [128, 8, 16], F32)
              pos = sb(p2, "pos", [128, 8, 16], U32)
              pab = sb(p2, "pab", [128, 2, 128], U32)
              pabf = sb(p2, "pabf", [128, 2, 128], F32)
              ee = sb(p2, "ee", [128, 8, 16], F32)
              zs = sb(p2, "zs", [128, 8], F32)
              IJG = sb(p2, "IJG", [128, 3, 128], F32)
              IJGT = sb(p2, "IJGT", [128, 3, TG], F32)
              NB = 16
              Pmx = [sb(p2, f"Pmx{i}", [128, NB, 128], BF16) for i in range(2)]
              Qmx = [sb(p2, f"Qmx{i}", [128, NB, 128], BF16) for i in range(2)]
              Wsum = sb(p2, "Wsum", [128, 128, TG], BF16)
              NS = 3
              ub = [sb(p2, f"ub{i}", [128, 8, 128], BF16) for i in range(NS)]
              vb = [sb(p2, f"vb{i}", [128, 1024], BF16) for i in range(NS)]
              ge = [sb(p2, f"ge{i}", [128, TG], F32) for i in range(2)]
              Lj = [sb(p2, f"Lj{i}", [128, TG], BF16) for i in range(2)]

              with ExitStack() as pl:
                  stg = sb(pl, "stg", [128, 2048], F32)
                  for kc in range(8):
                      S.dma("sp", stg[:], pw_q[:, kc, :], writes=[R("stg")])
                      G(lambda e: e.tensor_copy(out=pwq_b[:, kc, :], in_=stg[:]), [R("stg")], [R("pwq_b")])
                  S.dma("sp", stg[:], keysT[:, :, :].rearrange("p a b -> p (a b)"), writes=[R("stg")])
                  G(lambda e: e.tensor_copy(out=keys_b[:].rearrange("p a b -> p (a b)"), in_=stg[:]), [R("stg")], [R("keys_b")])
              S.barrier()

              scnt = 0
              acnt = 0
              for gi in range(NG):
                  s = gi // (NG // 2) if NG >= 2 else 0
                  t0 = gi * TG
                  S.dma("sp", x1g[:], x1T[:, :, t0:t0 + TG], reads=[R("x1T", t0 // 512)], writes=[R("x1g")])
                  norm_group(x1g[:], R("x1g"), TG, sq2, rstd2, ntmp, 4,
                             lambda c: A2[:, c, s:s + 1], lambda c: modp[:, 24 + c, s:s + 1],
                             lambda c: h2T[:, c, :], R("h2T"), False)
                  for hp in range(16):
                      bank = 4 + (hp // 2) % 4
                      off = (hp % 2) * TG
                      for kc in range(8):
                          PE(lambda e: e.matmul(psum[:, bank, off:off + TG], lhsT=pwq_b[:, kc, hp * 128:(hp + 1) * 128],
                                                rhs=h2T[:, kc, :], start=(kc == 0), stop=(kc == 7)),
                             [R("pwq_b"), R("h2T")], [RB(bank)])
                      if hp % 2 == 1:
                          A(lambda e: e.activation(out=qpT[:, hp - 1:hp + 1, :],
                                                   in_=psum[:, bank, :].rearrange("p (a t) -> p a t", t=TG), func=AF.Copy),
                            [RB(bank)], [R("qpT")])
                  for tt in range(TG // 128):
                      tsl = slice(tt * 128, (tt + 1) * 128)
                      for hp in range(16):
                          bank = 4 + hp // 4
                          off = (hp % 4) * 128
                          PE(lambda e: e.matmul(psum[:, bank, off:off + 128], lhsT=qpT[:, hp, tsl], rhs=keys_b[:, hp, :],
                                                start=True, stop=True), [R("qpT"), R("keys_b")], [RB(bank)])
                      A(lambda e: e.activation(out=s1[:].rearrange("p a b -> p (a b)"),
                                               in_=psum[:, 4:8, :].rearrange("p a b -> p (a b)"), func=AF.Copy),
                        [RB(4), RB(5), RB(6), RB(7)], [R("s1")])
                      for hp in range(16):
                          V(lambda e: e.max(out=m16[:, hp, 0:8], in_=s1[:, hp, :]), [R("s1")], [R("m16")])
                          V(lambda e: e.max_index(out=ix[:, hp, 0:8], in_max=m16[:, hp, 0:8], in_values=s1[:, hp, :]),
                            [R("s1"), R("m16")], [R("ix")])
                          V(lambda e: e.match_replace(out=s1[:, hp, :], in_to_replace=m16[:, hp, 0:8], in_values=s1[:, hp, :],
                                                      imm_value=NEG), [R("s1"), R("m16")], [R("s1")])
                          V(lambda e: e.max(out=m16[:, hp, 8:16], in_=s1[:, hp, :]), [R("s1")], [R("m16")])
                          V(lambda e: e.max_index(out=ix[:, hp, 8:16], in_max=m16[:, hp, 8:16], in_values=s1[:, hp, :]),
                            [R("s1"), R("m16")], [R("ix")])
                      V(lambda e: e.tensor_copy(out=ixf[:], in_=ix[:]), [R("ix")], [R("ixf")])
                      m4 = m16[:].rearrange("p (h two) k -> p h two k", two=2)
                      i4 = ixf[:].rearrange("p (h two) k -> p h two k", two=2)
                      V(lambda e: e.tensor_tensor(out=cand[:].rearrange("p h (a b) -> p h a b", b=16),
                                                  in0=m4[:, :, 0, :].unsqueeze(3).to_broadcast([128, 8, 16, 16]),
                                                  in1=m4[:, :, 1, :].unsqueeze(2).to_broadcast([128, 8, 16, 16]), op=ALU.add),
                        [R("m16")], [R("cand")])
                      for h in range(8):
                          V(lambda e: e.max(out=b16[:, h, 0:8], in_=cand[:, h, :]), [R("cand")], [R("b16")])
                          V(lambda e: e.max_index(out=pos[:, h, 0:8], in_max=b16[:, h, 0:8], in_values=cand[:, h, :]),
                            [R("cand"), R("b16")], [R("pos")])
                          V(lambda e: e.match_replace(out=cand[:, h, :], in_to_replace=b16[:, h, 0:8], in_values=cand[:, h, :],
                                                      imm_value=NEG), [R("cand"), R("b16")], [R("cand")])
                          V(lambda e: e.max(out=b16[:, h, 8:16], in_=cand[:, h, :]), [R("cand")], [R("b16")])
                          V(lambda e: e.max_index(out=pos[:, h, 8:16], in_max=b16[:, h, 8:16], in_values=cand[:, h, :]),
                            [R("cand"), R("b16")], [R("pos")])
                      V(lambda e: e.tensor_tensor(out=ee[:], in0=b16[:], in1=b16[:, :, 0:1].to_broadcast([128, 8, 16]),
                                                  op=ALU.subtract), [R("b16")], [R("ee")])
                      A(lambda e: e.activation(out=ee[:], in_=ee[:], func=AF.Exp), [R("ee")], [R("ee")])
                      V(lambda e: e.reduce_sum(out=zs[:], in_=ee[:], axis=AX.X), [R("ee")], [R("zs")])
                      V(lambda e: e.reciprocal(out=zs[:], in_=zs[:]), [R("zs")], [R("zs")])
                      V(lambda e: e.tensor_tensor(out=IJG[:, 2, :].rearrange("p (h k) -> p h k", k=16), in0=ee[:],
                                                  in1=zs[:, :].unsqueeze(2).to_broadcast([128, 8, 16]), op=ALU.mult),
                        [R("ee"), R("zs")], [R("IJG")])
                      posf = pos[:].rearrange("p h k -> p (h k)")
                      V(lambda e: e.tensor_scalar(out=pab[:, 0, :], in0=posf, scalar1=cu32_t[:, 0:1], scalar2=None,
                                                  op0=ALU.logical_shift_right), [R("pos"), R("cu32")], [R("pab")])
                      V(lambda e: e.tensor_scalar(out=pab[:, 1, :], in0=posf, scalar1=cu32_t[:, 1:2], scalar2=None,
                                                  op0=ALU.bitwise_and), [R("pos"), R("cu32")], [R("pab")])
                      V(lambda e: e.tensor_copy(out=pabf[:], in_=pab[:]), [R("pab")], [R("pabf")])
                      eq = cand[:].rearrange("p h (a b) -> p h a b", b=16)
                      io = iota_t[:, 0:16].unsqueeze(1).unsqueeze(1).to_broadcast([128, 8, 16, 16])
                      for w in range(2):
                          sel = pabf[:, w, :].rearrange("p (h k) -> p h k", k=16).unsqueeze(3).to_broadcast([128, 8, 16, 16])
                          V(lambda e: e.tensor_tensor(out=eq, in0=sel, in1=io, op=ALU.is_equal),
                            [R("pabf"), R("iota"), R("cand")], [R("cand")])
                          V(lambda e: e.tensor_tensor(out=eq, in0=eq, in1=i4[:, :, w, :].unsqueeze(2).to_broadcast([128, 8, 16, 16]),
                                                      op=ALU.mult), [R("cand"), R("ixf")], [R("cand")])
                          V(lambda e: e.tensor_reduce(out=IJG[:, w, :].rearrange("p (h k) -> p h k", k=16), in_=eq,
                                                      axis=AX.X, op=ALU.add), [R("cand")], [R("IJG")])
                      if DEBUG["sel"] and gi == 0 and tt == 0:
                          S.dma("sp", dbg["sel"][:, :, :], IJG[:], reads=[R("IJG")], writes=[R("dbg_sel")])
                      for w in range(3):
                          PE(lambda e: e.transpose(out=psum[:, 4, w * 128:(w + 1) * 128], in_=IJG[:, w, :], identity=ident_f[:]),
                             [R("IJG"), R("ident_f")], [RB(4)])
                      A(lambda e: e.activation(out=IJGT[:, :, tsl], in_=psum[:, 4, 0:384].rearrange("p (w t) -> p w t", t=128),
                                               func=AF.Copy), [RB(4)], [R("IJGT")])
                      for bt in range(128 // NB):
                          i2 = scnt % 2
                          scnt += 1
                          Px_, Qx_ = Pmx[i2], Qmx[i2]
                          rPx, rQx = R("Pmx", i2), R("Qmx", i2)
                          wb0 = 4 * (i2)
                          tb0 = tt * 128 + bt * NB
                          iob = iota_t[:, :].unsqueeze(1).to_broadcast([128, NB, 128])
                          V(lambda e: e.tensor_tensor(out=Px_[:], in0=IJGT[:, 0, tb0:tb0 + NB].unsqueeze(2).to_broadcast([128, NB, 128]),
                                                      in1=iob, op=ALU.is_equal), [R("IJGT"), R("iota")], [rPx])
                          V(lambda e: e.tensor_tensor(out=Qx_[:], in0=IJGT[:, 1, tb0:tb0 + NB].unsqueeze(2).to_broadcast([128, NB, 128]),
                                                      in1=iob, op=ALU.is_equal), [R("IJGT"), R("iota")], [rQx])
                          V(lambda e: e.tensor_tensor(out=Qx_[:], in0=Qx_[:],
                                                      in1=IJGT[:, 2, tb0:tb0 + NB].unsqueeze(2).to_broadcast([128, NB, 128]),
                                                      op=ALU.mult), [R("IJGT"), rQx], [rQx])
                          for tl in range(NB):
                              bank = 4 + tl // 4
                              off = (tl % 4) * 128
                              PE(lambda e: e.matmul(psum[:, bank, off:off + 128], lhsT=Px_[:, tl, :], rhs=Qx_[:, tl, :],
                                                    start=True, stop=True), [rPx, rQx], [RB(bank)])
                          tb = tt * 128 + bt * NB
                          A(lambda e: e.activation(out=Wsum[:, :, tb:tb + NB],
                                                   in_=psum[:, 4:8, :].rearrange("p a (t j) -> p j (a t)", j=128),
                                                   func=AF.Copy), [RB(4), RB(5), RB(6), RB(7)], [R("Wsum")])
                  for b in range(4):
                      PE(lambda e: e.matmul(psum[:, b, :], lhsT=zeros_b[:, 0:128], rhs=zeros_b[:], start=True, stop=False,
                                            skip_group_check=True), [R("zeros_b")], [RB(b)])
                  for j in range(128):
                      sl = acnt % NS
                      i2 = acnt % 2
                      acnt += 1
                      S.dma("sp", ub[sl][:].rearrange("p a b -> p (a b)"), uTb[j], reads=[R("scr_u", j)], writes=[R("ub", sl)])
                      S.dma("sp", vb[sl][:], vtb[j], reads=[R("scr_v", j)], writes=[R("vb", sl)])
                      ab = 4 + i2
                      for dc in range(8):
                          PE(lambda e: e.matmul(psum[:, ab, 0:TG], lhsT=ub[sl][:, dc, :], rhs=h2T[:, dc, :],
                                                start=(dc == 0), stop=(dc == 7)), [R("ub", sl), R("h2T")], [RB(ab)])
                      A(lambda e: e.activation(out=ge[i2][:], in_=psum[:, ab, 0:TG], func=AF.Gelu), [RB(ab)], [R("ge", i2)])
                      V(lambda e: e.tensor_tensor(out=Lj[i2][:], in0=ge[i2][:], in1=Wsum[:, j, :], op=ALU.mult),
                        [R("ge", i2), R("Wsum")], [R("Lj", i2)])
                      for dc in range(8):
                          PE(lambda e: e.matmul(psum[:, dc // 2, (dc % 2) * TG:(dc % 2 + 1) * TG],
                                                lhsT=vb[sl][:, dc * 128:(dc + 1) * 128], rhs=Lj[i2][:],
                                                start=False, stop=(j == 127), skip_group_check=True),
                             [R("vb", sl), R("Lj", i2)], [RB(dc // 2)])
                  for dc in range(8):
                      V(lambda e: e.scalar_tensor_tensor(out=x1g[:, dc, :], in0=psum[:, dc // 2, (dc % 2) * TG:(dc % 2 + 1) * TG],
                                                         scalar=modp[:, 40 + dc, s:s + 1], in1=x1g[:, dc, :],
                                                         op0=ALU.mult, op1=ALU.add), [RB(dc // 2), R("x1g"), R("modp")], [R("x1g")])
                  norm_group(x1g[:], R("x1g"), TG, sq2, rstd2, None, 4,
                             lambda c: fing_t[:, c:c + 1], None,
                             lambda c: x1g[:, c, :], R("x1g"), True)
                  S.dma("sp", outT[:, :, t0:t0 + TG], x1g[:], reads=[R("x1g")], writes=[R("outT", gi)])
        except _Stop:
            pass
        build.stats = dict(S.cnt); build.stats["dma"] = dict(S.dq)
        S.finish([R("outT", gi) for gi in range(NG)] + ([R("dbg_x1")] if DEBUG["x1"] else [])
                 + ([R("dbg_sel")] if DEBUG["sel"] else []) + [R("x1T", i) for i in range(8)])
    return nc


def _fm(a):
    rows, D = a.shape
    return np.ascontiguousarray(a.T.reshape(D // 128, 128, rows).transpose(1, 0, 2))


def _partner(d):
    return d + 16 if (d % 32) < 16 else d - 16


def prep_shared(inp):
    f = np.float32
    sh = {}
    w_mod = np.asarray(inp["w_mod"], f)[0]
    sh["w_mod"] = np.ascontiguousarray(w_mod.reshape(8, 128, 6144).transpose(1, 0, 2))
    sh["b_mod"] = np.ascontiguousarray(np.asarray(inp["b_mod"], f)[0].reshape(48, 128).T)
    sh["n1g"] = np.ascontiguousarray(np.asarray(inp["norm1_g"], f)[0].reshape(8, 128).T)
    sh["n2g"] = np.ascontiguousarray(np.asarray(inp["norm2_g"], f)[0].reshape(8, 128).T)
    sh["fing"] = np.ascontiguousarray(np.asarray(inp["final_g"], f).reshape(8, 128).T)
    w_in = np.asarray(inp["w_in"], f)[0]
    pr = np.array([_partner(d) for d in range(64)])
    chunks = []
    for c in range(4):
        chunks.append(np.concatenate([c * 64 + np.arange(64), (4 + c) * 64 + np.arange(64)]))
    for c in range(4):
        chunks.append(np.concatenate([c * 64 + pr, (4 + c) * 64 + pr]))
    chunks.append(512 + np.arange(128))
    chunks.append(512 + np.concatenate([pr, 64 + pr]))
    chunks.append(640 + np.arange(128))
    GB, GC, UU, GA, GV = 768, 1280, 1792, 2304, 3328
    for c in range(4):
        chunks.append(GC + c * 128 + np.arange(128))
        chunks.append(UU + c * 128 + np.arange(128))
        chunks.append(GB + c * 128 + np.arange(128))
    for c in range(8):
        chunks.append(GA + c * 128 + np.arange(128))
    for c in range(8):
        chunks.append(GV + c * 128 + np.arange(128))
    assert len(chunks) == 39
    wl = np.empty((39, 128, 8, 128), f)
    for i, cols in enumerate(chunks):
        wl[i] = w_in[:, cols].reshape(8, 128, 128).transpose(1, 0, 2)
    sh["w_in"] = wl
    sh["w_ao"] = np.ascontiguousarray(np.asarray(inp["w_attn_out"], f)[0].reshape(8, 64, 1024).transpose(1, 0, 2))
    sh["w_co"] = np.ascontiguousarray(np.asarray(inp["w_conv_out"], f)[0].reshape(4, 128, 1024).transpose(1, 0, 2))
    sh["w_mo"] = np.ascontiguousarray(np.asarray(inp["w_mix_out"], f)[0].reshape(8, 128, 1024).transpose(1, 0, 2))
    sh["conv_w"] = np.ascontiguousarray(np.asarray(inp["conv_w"], f)[0].reshape(3, 4, 128).transpose(2, 1, 0))
    sh["sinkv"] = np.ascontiguousarray(np.broadcast_to(np.asarray(inp["attn_sink"], f)[0][None, :], (128, 8)))
    sh["pw_q"] = np.ascontiguousarray(np.asarray(inp["peer_w_q"], f)[0].reshape(8, 128, 2048).transpose(1, 0, 2))
    sk = np.asarray(inp["peer_sub_keys"], f)[0]
    sh["keysT"] = np.ascontiguousarray(sk.reshape(16, 128, 128).transpose(2, 0, 1))
    pu = np.asarray(inp["peer_u"], f)[0]
    sh["uT"] = np.ascontiguousarray(pu.reshape(128, 128, 8, 128).transpose(1, 3, 2, 0)).reshape(128, 128, 1024)
    pv = np.asarray(inp["peer_v"], f)[0]
    sh["vt"] = np.ascontiguousarray(pv.reshape(128, 128, 1024).transpose(1, 0, 2))
    inv_freq = (np.float32(10000.0) ** (-np.arange(16, dtype=f) / np.float32(16))).astype(f)
    l = np.arange(L)
    cos_t = np.empty((128, L), f)
    sin_t = np.empty((128, L), f)
    for p in range(128):
        d = p % 64
        posn = (l % 64) if d >= 32 else (l // 64)
        ang = posn.astype(f) * inv_freq[d % 16]
        cos_t[p] = np.cos(ang)
        sin_t[p] = np.sin(ang) * (-1.0 if (d % 32) < 16 else 1.0)
    sh["rope_cos"] = cos_t
    sh["rope_sin"] = sin_t
    qi = np.arange(128)[:, None]
    kk = np.arange(384)[None, :]
    sh["amask"] = np.where((kk >= qi) & (kk <= qi + 256), 0.0, NEG).astype(f)
    sh["ident"] = np.eye(128, dtype=f)
    sh["iota128"] = np.ascontiguousarray(np.broadcast_to(np.arange(128, dtype=f)[None, :], (128, 128)))
    sh["cu32"] = np.ascontiguousarray(np.broadcast_to(np.array([4, 15], np.uint32)[None, :], (128, 2)))
    return sh


def prep_core(inp, core, sh):
    f = np.float32
    x = np.asarray(inp["x"], f)
    ctx = np.asarray(inp["ctx"], f)
    c = np.asarray(inp["c"], f)
    c_ctx = np.asarray(inp["c_ctx"], f)
    m = dict(sh)
    b0 = 2 * core
    m["xT"] = _fm(x[b0:b0 + 2].reshape(2 * L, 1024))
    m["ctxT"] = _fm(ctx[b0:b0 + 2].reshape(512, 1024))
    m["cT"] = _fm(np.stack([c[b0], c[b0 + 1], c_ctx], 0))
    return m


_NC_CACHE = {}


def kernel(**inputs):
    sh = prep_shared(inputs)
    in_maps = [prep_core(inputs, core, sh) for core in range(NCORES)]
    if "nc" not in _NC_CACHE:
        _NC_CACHE["nc"] = build()
    nc = _NC_CACHE["nc"]
    res = run_bass_kernel_spmd(nc, in_maps, core_ids=list(range(NCORES)))
    out = np.empty((16, L, 1024), np.float32)
    for core in range(NCORES):
        o = np.asarray(res.results[core]["outT"])
        o = o.transpose(2, 1, 0).reshape(2, L, 1024)
        out[2 * core:2 * core + 2] = o
    return out
```

```python
import numpy as np
from contextlib import ExitStack
import concourse.bass as bass
import concourse.mybir as mybir
from concourse.bass_utils import run_bass_kernel_spmd

F32 = mybir.dt.float32
BF16 = mybir.dt.bfloat16
U32 = mybir.dt.uint32
AF = mybir.ActivationFunctionType
ALU = mybir.AluOpType
AX = mybir.AxisListType

NCORES = 8
L = 2048
TPC = 2 * L
EPS = 1e-6
NEG = -1e30
DEBUG = {"x1": False, "sel": False, "stop_after_mixer": False, "stop": None, "no_prologue": False}


class _Stop(Exception):
    pass


class Res:
    __slots__ = ("name", "w", "r")

    def __init__(self, name):
        self.name = name
        self.w = None
        self.r = {}


class Sched:
    ENG = ("pe", "act", "dve", "pool", "sp")

    def __init__(self, nc, stack, ndma=12, used=None):
        self.nc = nc
        self.used_in = used
        self.used = set()
        self.sig = {}
        self.vmap = {}
        self.e = {"pe": nc.tensor, "act": nc.scalar, "dve": nc.vector, "pool": nc.gpsimd, "sp": nc.sync}
        self.sem = {}
        self.cnt = {}
        for k in self.ENG:
            self.sem[k] = stack.enter_context(nc.semaphore("sem_" + k))
            self.cnt[k] = 0
        self.ndma = ndma
        self.dq = {}
        for q in ("sp", "pool"):
            for i in range(ndma):
                self.sem[(q, i)] = stack.enter_context(nc.semaphore(f"dsem_{q}_{i}"))
            self.dq[q] = 0
        self.waited = {k: {} for k in self.ENG}
        self.dead = False

    def _hw(self, tok):
        key, val = tok
        self.used.add(tok)
        if self.used_in is not None and isinstance(key, str):
            return self.vmap[tok]
        return val

    def _wait(self, eng, tok):
        key, val = tok
        if self.waited[eng].get(key, 0) >= val:
            return
        self.e[eng].wait_ge(self.sem[key], self._hw(tok))
        self.waited[eng][key] = val

    def _deps(self, eng, reads, writes, attach=False):
        toks = {}

        def add(t):
            if t is None:
                return
            k, v = t
            if toks.get(k, 0) < v:
                toks[k] = v
        for r in reads:
            add(r.w)
        for w in writes:
            add(w.w)
            for k, v in w.r.items():
                add((k, v))
        need = [(k, v) for k, v in toks.items() if self.waited[eng].get(k, 0) < v and not (eng == "pe" and k == "pe")]
        if attach and need:
            last = need.pop()
        else:
            last = None
        for k, v in need:
            self._wait(eng, (k, v))
        return last

    def _attach(self, eng, ins, last):
        if last is not None:
            ins._wait_ge(self.sem[last[0]], self._hw(last))
            self.waited[eng][last[0]] = last[1]

    def _commit(self, tok, reads, writes):
        k, v = tok
        for r in reads:
            if r.r.get(k, 0) < v:
                r.r[k] = v
        for w in writes:
            w.w = tok
            w.r = {}

    def op(self, eng, fn, reads=(), writes=()):
        if self.dead:
            return None
        last = self._deps(eng, reads, writes, attach=True)
        ins = fn(self.e[eng])
        self._attach(eng, ins, last)
        self.cnt[eng] += 1
        tok = (eng, self.cnt[eng])
        if self.used_in is None:
            ins.then_inc(self.sem[eng], 1)
        elif tok in self.used_in:
            self.sig[eng] = self.sig.get(eng, 0) + 1
            self.vmap[tok] = self.sig[eng]
            ins.then_inc(self.sem[eng], 1)
        self._commit(tok, reads, writes)
        return tok

    def dma(self, q, out, in_, reads=(), writes=()):
        if self.dead:
            return None
        n = self.dq[q]
        self.dq[q] += 1
        slot = n % self.ndma
        rnd = n // self.ndma
        key = (q, slot)
        if rnd > 0:
            self._wait(q, (key, 16 * rnd))
        last = self._deps(q, reads, writes, attach=True)
        ins = self.e[q].dma_start(out=out, in_=in_)
        self._attach(q, ins, last)
        ins.then_inc(self.sem[key], 16)
        tok = (key, 16 * (rnd + 1))
        self._commit(tok, reads, writes)
        return tok

    def barrier(self):
        if self.dead:
            return
        toks = [(k, self.cnt[k]) for k in self.ENG if self.cnt[k] > 0]
        for q, n in self.dq.items():
            for slot in range(min(n, self.ndma)):
                rnd = (n - 1 - slot) // self.ndma
                toks.append(((q, slot), 16 * (rnd + 1)))
        for e in self.ENG:
            for t in toks:
                self._wait(e, t)

    def finish(self, ress):
        for r in ress:
            if r.w is not None:
                self._wait("sp", r.w)


def build(used=None):
    nc = bass.Bass("TRN2", target_bir_lowering=False)

    def din(name, shape, dt=F32):
        return nc.dram_tensor(name, list(shape), dt, kind="ExternalInput").ap()

    xT = din("xT", [128, 8, TPC])
    ctxT = din("ctxT", [128, 8, 512])
    cT = din("cT", [128, 8, 3])
    w_mod = din("w_mod", [128, 8, 6144])
    b_mod = din("b_mod", [128, 48])
    n1g = din("n1g", [128, 8])
    n2g = din("n2g", [128, 8])
    fing = din("fing", [128, 8])
    w_in = din("w_in", [39, 128, 8, 128])
    w_ao = din("w_ao", [64, 8, 1024])
    w_co = din("w_co", [128, 4, 1024])
    w_mo = din("w_mo", [128, 8, 1024])
    conv_w = din("conv_w", [128, 4, 3])
    sinkv = din("sinkv", [128, 8])
    pw_q = din("pw_q", [128, 8, 2048])
    keysT = din("keysT", [128, 16, 128])
    uT = din("uT", [128, 128, 1024])
    vt = din("vt", [128, 128, 1024])
    rope_cos = din("rope_cos", [128, L])
    rope_sin = din("rope_sin", [128, L])
    amask = din("amask", [128, 384])
    ident = din("ident", [128, 128])
    iota128 = din("iota128", [128, 128])
    cu32 = din("cu32", [128, 2], U32)
    outT = nc.dram_tensor("outT", [128, 8, TPC], F32, kind="ExternalOutput").ap()
    x1T = nc.dram_tensor("x1T_scr", [128, 8, TPC], F32, kind="Internal").ap()
    uTb = nc.dram_tensor("uTb_scr", [128, 128, 1024], BF16, kind="Internal").ap()
    vtb = nc.dram_tensor("vtb_scr", [128, 128, 1024], BF16, kind="Internal").ap()
    dbg = {}
    if DEBUG["x1"]:
        dbg["x1"] = nc.dram_tensor("dbg_x1", [128, 8, TPC], F32, kind="ExternalOutput").ap()
    if DEBUG["sel"]:
        dbg["sel"] = nc.dram_tensor("dbg_sel", [128, 3, 128], F32, kind="ExternalOutput").ap()

    with ExitStack() as st:
        S = Sched(nc, st, used=used)
        NG = 0
        RES = {}

        def R(*key):
            r = RES.get(key)
            if r is None:
                r = RES[key] = Res(str(key))
            return r

        _uid = [0]

        def sb(stack, name, shape, dt):
            _uid[0] += 1
            return stack.enter_context(nc.sbuf_tensor(f"{name}_{_uid[0]}", list(shape), dt))

        def V(fn, reads, writes):
            return S.op("dve", fn, reads, writes)

        def A(fn, reads, writes):
            return S.op("act", fn, reads, writes)

        def G(fn, reads, writes):
            return S.op("pool", fn, reads, writes)

        def PE(fn, reads, writes):
            return S.op("pe", fn, reads, writes)

        psum = st.enter_context(nc.psum_tensor("psum", [128, 8, 512], F32))

        def RB(b):
            return R("psb", b)

        ident_f = sb(st, "ident_f", [128, 128], F32)
        ident_b = sb(st, "ident_b", [128, 128], BF16)
        ones_b = sb(st, "ones_b", [128, 128], BF16)
        zeros_b = sb(st, "zeros_b", [128, 512], BF16)
        iota_t = sb(st, "iota_t", [128, 128], F32)
        cu32_t = sb(st, "cu32_t", [128, 2], U32)
        eps_t = sb(st, "eps_t", [128, 1], F32)
        modp = sb(st, "modp", [128, 48, 3], F32)
        A1 = sb(st, "A1", [128, 8, 3], F32)
        A2 = sb(st, "A2", [128, 8, 3], F32)
        bmod_t = sb(st, "bmod_t", [128, 48], F32)
        n1g_t = sb(st, "n1g_t", [128, 8], F32)
        n2g_t = sb(st, "n2g_t", [128, 8], F32)
        fing_t = sb(st, "fing_t", [128, 8], F32)
        sink_t = sb(st, "sink_t", [128, 8], F32)
        nsink_t = sb(st, "nsink_t", [128, 8], F32)
        convw_t = sb(st, "convw_t", [128, 4, 3], F32)

        S.dma("sp", ident_f[:], ident[:, :], writes=[R("ident_f")])
        S.dma("sp", iota_t[:], iota128[:, :], writes=[R("iota")])
        S.dma("sp", cu32_t[:], cu32[:, :], writes=[R("cu32")])
        S.dma("sp", bmod_t[:], b_mod[:, :], writes=[R("bmod")])
        S.dma("sp", n1g_t[:], n1g[:, :], writes=[R("n1g")])
        S.dma("sp", n2g_t[:], n2g[:, :], writes=[R("n2g")])
        S.dma("sp", fing_t[:], fing[:, :], writes=[R("fing")])
        S.dma("sp", sink_t[:], sinkv[:, :], writes=[R("sink")])
        S.dma("sp", convw_t[:], conv_w[:, :, :], writes=[R("convw")])
        V(lambda e: e.tensor_copy(out=ident_b[:], in_=ident_f[:]), [R("ident_f")], [R("ident_b")])
        V(lambda e: e.memset(ones_b[:], 1.0), [], [R("ones_b")])
        V(lambda e: e.memset(zeros_b[:], 0.0), [], [R("zeros_b")])
        V(lambda e: e.memset(eps_t[:], EPS), [], [R("eps")])
        V(lambda e: e.tensor_scalar(out=nsink_t[:], in0=sink_t[:], scalar1=-1.0, scalar2=None, op0=ALU.mult),
          [R("sink")], [R("nsink")])

        NPB = 2
        pro_stack = st.enter_context(ExitStack())
        pst = [sb(pro_stack, f"pst{i}", [128, 1024], F32) for i in range(NPB)]
        pbf = [sb(pro_stack, f"pbf{i}", [128, 1024], BF16) for i in range(NPB)]
        pro_state = {"k": 0}
        NPRO = 0 if DEBUG["no_prologue"] else 256

        def pro_src_dst(k):
            tab, j = divmod(k, 128)
            if tab == 0:
                return uT[j], uTb[j], R("scr_u", j)
            return vt[j], vtb[j], R("scr_v", j)

        def prologue_step():
            k = pro_state["k"]
            if k >= NPRO + 1:
                return False
            if k < NPRO:
                src, _, _ = pro_src_dst(k)
                S.dma("sp", pst[k % NPB][:], src, writes=[R("pst", k % NPB)])
            k2 = k - 1
            if 0 <= k2 < NPRO:
                _, dst, rr = pro_src_dst(k2)
                sl = k2 % NPB
                G(lambda e: e.tensor_copy(out=pbf[sl][:], in_=pst[sl][:]), [R("pst", sl)], [R("pbf", sl)])
                S.dma("sp", dst, pbf[sl][:], reads=[R("pbf", sl)], writes=[rr])
            pro_state["k"] = k + 1
            return True

        def prologue_steps(n):
            for _ in range(n):
                if not prologue_step():
                    break

        def norm_group(xs, rxs, N, sq, rstd, tmp2, bank, Acol, Bcol, outc, rout, in_place):
            A(lambda e: e.activation(out=sq[:, :, 0:N], in_=xs, func=AF.Square), [rxs], [R("sq")])
            for c in range(8):
                PE(lambda e: e.matmul(psum[:, bank, 0:N], lhsT=ones_b[:], rhs=sq[:, c, 0:N],
                                      start=(c == 0), stop=(c == 7)), [R("sq"), R("ones_b")], [RB(bank)])
            A(lambda e: e.activation(out=rstd[:, 0:N], in_=psum[:, bank, 0:N], func=AF.Sqrt,
                                     scale=1.0 / 1024.0, bias=eps_t[:, 0:1]), [RB(bank), R("eps")], [R("rstd")])
            V(lambda e: e.reciprocal(out=rstd[:, 0:N], in_=rstd[:, 0:N]), [R("rstd")], [R("rstd")])
            for c in range(8):
                if in_place:
                    t = xs[:, c, :]
                    rt = rxs
                else:
                    t = tmp2[c % 2][:, 0:N]
                    rt = R("ntmp", c % 2)
                V(lambda e: e.tensor_tensor(out=t, in0=xs[:, c, :], in1=rstd[:, 0:N], op=ALU.mult),
                  [rxs, R("rstd")], [rt])
                b = Bcol(c) if Bcol is not None else None
                if b is not None:
                    V(lambda e: e.tensor_scalar(out=outc(c), in0=t, scalar1=Acol(c), scalar2=b,
                                                op0=ALU.mult, op1=ALU.add), [rt], [rout])
                else:
                    V(lambda e: e.tensor_scalar(out=outc(c), in0=t, scalar1=Acol(c), scalar2=None,
                                                op0=ALU.mult), [rt], [rout])

        with ExitStack() as p0:
            cTt = sb(p0, "cTt", [128, 8, 3], F32)
            scT = sb(p0, "scT", [128, 8, 3], F32)
            wm = [sb(p0, f"wm{i}", [128, 8, 512], F32) for i in range(2)]
            S.dma("sp", cTt[:], cT[:, :, :], writes=[R("cTt")])
            A(lambda e: e.activation(out=scT[:], in_=cTt[:], func=AF.Silu), [R("cTt")], [R("scT")])
            for pc in range(12):
                b = pc % 2
                S.dma("sp", wm[b][:], w_mod[:, :, pc * 512:(pc + 1) * 512], writes=[R("wm", b)])
                for cc in range(4):
                    j = pc * 4 + cc
                    for kc in range(8):
                        PE(lambda e: e.matmul(psum[:, 0, j * 3:(j + 1) * 3], lhsT=wm[b][:, kc, cc * 128:(cc + 1) * 128],
                                              rhs=scT[:, kc, :], start=(kc == 0), stop=(kc == 7)),
                           [R("wm", b), R("scT")], [RB(0)])
            V(lambda e: e.tensor_tensor(out=modp[:], in0=psum[:, 0, 0:144].rearrange("p (a b) -> p a b", b=3),
                                        in1=bmod_t[:, :].unsqueeze(2).to_broadcast([128, 48, 3]), op=ALU.add),
              [RB(0), R("bmod")], [R("modp")])
            for (Ax, off, gt, rg) in ((A1, 8, n1g_t, "n1g"), (A2, 32, n2g_t, "n2g")):
                V(lambda e: e.tensor_scalar(out=Ax[:], in0=modp[:, off:off + 8, :], scalar1=1.0, scalar2=None,
                                            op0=ALU.add), [R("modp")], [R("A12")])
                V(lambda e: e.tensor_tensor(out=Ax[:], in0=Ax[:], in1=gt[:, :].unsqueeze(2).to_broadcast([128, 8, 3]),
                                            op=ALU.mult), [R("A12"), R(rg)], [R("A12")])
        S.barrier()

        try:
          with ExitStack() as p1:
              NW = 5
              wst = [sb(p1, f"wst{i}", [128, 8, 128], F32) for i in range(NW)]
              wbf = [sb(p1, f"wbf{i}", [128, 8, 128], BF16) for i in range(NW)]
              wcount = [0]

              def load_w(idx):
                  i = wcount[0] % NW
                  wcount[0] += 1
                  S.dma("sp", wst[i][:], w_in[idx], writes=[R("wst", i)])
                  G(lambda e: e.tensor_copy(out=wbf[i][:], in_=wst[i][:]), [R("wst", i)], [R("wbf", i)])
                  return wbf[i], R("wbf", i)

              class WStream:
                  def __init__(self, order):
                      self.order = order
                      self.pos = 0
                      self.q = []

                  def prefetch(self, n):
                      while len(self.q) < n and self.pos < len(self.order):
                          self.q.append(load_w(self.order[self.pos]))
                          self.pos += 1

                  def get(self):
                      self.prefetch(1)
                      return self.q.pop(0)

              hT = sb(p1, "hT", [128, 8, L], BF16)
              hcT = sb(p1, "hcT", [128, 8, 256], BF16)
              attnT = sb(p1, "attnT", [64, 8, L], BF16)
              convo = sb(p1, "convo", [128, 4, L], BF16)
              rstd = sb(p1, "rstd", [128, 512], F32)

              def load_big(dst_fn, src_fn, npieces, parts, rname):
                  for pc in range(npieces):
                      i = wcount[0] % NW
                      wcount[0] += 1
                      stg = wst[i][:].rearrange("p a b -> p (a b)")
                      S.dma("sp", stg[0:parts, :], src_fn(pc), writes=[R("wst", i)])
                      G(lambda e: e.tensor_copy(out=dst_fn(pc), in_=stg[0:parts, :]), [R("wst", i)], [R(rname)])


              for s in range(2):
                  order = [0, 4, 1, 5, 2, 6, 3, 7, 8, 9, 10] + list(range(11, 23))
                  for tg in range(4):
                      for oc in range(8):
                          order += [23 + oc, 31 + oc]
                  ws = WStream(order)
                  ws.prefetch(2)

                  with ExitStack() as psa:
                      xs = sb(psa, "xs", [128, 8, 512], F32)
                      sq = sb(psa, "sq", [128, 8, 512], BF16)
                      for tg in range(4):
                          S.dma("sp", xs[:], xT[:, :, s * L + tg * 512: s * L + (tg + 1) * 512], writes=[R("xs")])
                          norm_group(xs[:], R("xs"), 512, sq, rstd, None, tg % 2,
                                     lambda c: A1[:, c, s:s + 1], lambda c: modp[:, c, s:s + 1],
                                     lambda c: hT[:, c, tg * 512:(tg + 1) * 512], R("hT", tg), True)
                      S.dma("sp", xs[:, :, 0:256], ctxT[:, :, s * 256:(s + 1) * 256], writes=[R("xs")])
                      norm_group(xs[:, :, 0:256], R("xs"), 256, sq, rstd, None, 0,
                                 lambda c: A1[:, c, 2:3], lambda c: modp[:, c, 2:3],
                                 lambda c: hcT[:, c, :], R("hcT"), True)
                      prologue_steps(8)
                  S.barrier()
                  if DEBUG["stop"] == "A":
                      S.dead = True

                  with ExitStack() as pa:
                      qT = sb(pa, "qT", [128, 4, L], BF16)
                      kT = sb(pa, "kT", [128, 256 + L], BF16)
                      vtok = sb(pa, "vtok", [128, 18, 128], BF16)
                      amask_t = sb(pa, "amask_t", [128, 384], F32)
                      pb_ = ExitStack()
                      cos_t = sb(pb_, "cos_t", [128, L], F32)
                      sin_t = sb(pb_, "sin_t", [128, L], F32)
                      t1 = [sb(pb_, f"t1_{i}", [128, 512], F32) for i in range(2)]
                      t2 = [sb(pb_, f"t2_{i}", [128, 512], F32) for i in range(2)]
                      S.dma("sp", cos_t[:], rope_cos[:, :], writes=[R("cos")])
                      S.dma("sp", sin_t[:], rope_sin[:, :], writes=[R("sin")])
                      S.dma("sp", amask_t[:], amask[:, :], writes=[R("amask")])

                      it = 0

                      def proj_rope(wa, ra, wb_, rb_, dst_fn, rdst_fn):
                          nonlocal it
                          for tg in range(4):
                              ba, bb = (2, 3) if it % 2 == 0 else (4, 5)
                              tt1, tt2 = t1[it % 2], t2[it % 2]
                              r1, r2 = R("t1", it % 2), R("t2", it % 2)
                              it += 1
                              for kc in range(8):
                                  PE(lambda e: e.matmul(psum[:, ba, :], lhsT=wa[:, kc, :], rhs=hT[:, kc, tg * 512:(tg + 1) * 512],
                                                        start=(kc == 0), stop=(kc == 7)), [ra, R("hT", tg)], [RB(ba)])
                              for kc in range(8):
                                  PE(lambda e: e.matmul(psum[:, bb, :], lhsT=wb_[:, kc, :], rhs=hT[:, kc, tg * 512:(tg + 1) * 512],
                                                        start=(kc == 0), stop=(kc == 7)), [rb_, R("hT", tg)], [RB(bb)])
                              V(lambda e: e.tensor_tensor(out=tt1[:], in0=psum[:, ba, :], in1=cos_t[:, tg * 512:(tg + 1) * 512],
                                                          op=ALU.mult), [RB(ba), R("cos")], [r1])
                              V(lambda e: e.tensor_tensor(out=tt2[:], in0=psum[:, bb, :], in1=sin_t[:, tg * 512:(tg + 1) * 512],
                                                          op=ALU.mult), [RB(bb), R("sin")], [r2])
                              G(lambda e: e.tensor_tensor(out=dst_fn(tg), in0=tt1[:], in1=tt2[:], op=ALU.add),
                                [r1, r2], [rdst_fn(tg)])

                      for ch in range(4):
                          (wq, rq) = ws.get()
                          (wqs, rqs) = ws.get()
                          ws.prefetch(2)
                          proj_rope(wq, rq, wqs, rqs, lambda tg: qT[:, ch, tg * 512:(tg + 1) * 512],
                                    lambda tg: R("qT", ch, tg))
                          prologue_steps(4)
                      (wk, rk) = ws.get()
                      (wks, rks) = ws.get()
                      ws.prefetch(2)
                      for kc in range(8):
                          PE(lambda e: e.matmul(psum[:, 6, 0:256], lhsT=wk[:, kc, :], rhs=hcT[:, kc, :],
                                                start=(kc == 0), stop=(kc == 7)), [rk, R("hcT")], [RB(6)])
                      A(lambda e: e.activation(out=kT[:, 0:256], in_=psum[:, 6, 0:256], func=AF.Copy), [RB(6)], [R("kT", "ctx")])
                      proj_rope(wk, rk, wks, rks, lambda tg: kT[:, 256 + tg * 512: 256 + (tg + 1) * 512],
                                lambda tg: R("kT", tg))
                      (wv, rv) = ws.get()
                      ws.prefetch(3)
                      for blk in range(18):
                          bank = 6 + blk % 2
                          for kc in range(8):
                              if blk < 2:
                                  lh = hcT[:, kc, blk * 128:(blk + 1) * 128]
                                  rl = R("hcT")
                              else:
                                  lh = hT[:, kc, (blk - 2) * 128:(blk - 1) * 128]
                                  rl = R("hT", (blk - 2) // 4)
                              PE(lambda e: e.matmul(psum[:, bank, 0:128], lhsT=lh, rhs=wv[:, kc, :],
                                                    start=(kc == 0), stop=(kc == 7)), [rl, rv], [RB(bank)])
                          A(lambda e: e.activation(out=vtok[:, blk, :], in_=psum[:, bank, 0:128], func=AF.Copy),
                            [RB(bank)], [R("vtok", blk)])
                      prologue_steps(8)
                      pb_.close()
                      S.barrier()
                      if DEBUG["stop"] == "B":
                          S.dead = True

                      with ExitStack() as pc_:
                          sc = [sb(pc_, f"sc{i}", [128, 640], F32) for i in range(2)]
                          Pm = [sb(pc_, f"Pm{i}", [128, 640], BF16) for i in range(2)]
                          sm = [sb(pc_, f"sm{i}", [128, 8], F32) for i in range(2)]
                          dg = [sb(pc_, f"dg{i}", [128, 128], BF16) for i in range(2)]
                          PTs = [sb(pc_, f"PTs{i}", [128, 5, 4, 128], BF16) for i in range(2)]
                          hcnt = 0
                          pvc = 0
                          for n in range(16):
                              lo = max(n - 1, 0)
                              hi = min(n + 1, 15)
                              nlb = hi - lo + 1
                              nloc = nlb * 128
                              nk = nloc + 256
                              nkb = nlb + 2
                              moff = (lo - (n - 1)) * 128
                              krs = [R("kT", "ctx")] + [R("kT", t) for t in sorted(set([(lo * 128) // 512, (hi * 128 + 127) // 512]))]
                              for g in range(2):
                                  psl = slice(g * 64, (g + 1) * 64)
                                  pts = PTs[pvc % 2]
                                  rpts = R("PTs", pvc % 2)
                                  for c in range(4):
                                      hq = g * 4 + c
                                      i2 = hcnt % 2
                                      sa, sbk = (0, 1) if i2 == 0 else (2, 3)
                                      hcnt += 1
                                      scx, Px, smx, dgx = sc[i2], Pm[i2], sm[i2], dg[i2]
                                      rsc, rP, rsm, rdg = R("sc", i2), R("Pm", i2), R("sm", i2), R("dg", i2)
                                      lq = qT[psl, c, n * 128:(n + 1) * 128]
                                      PE(lambda e: e.matmul(psum[:, sa, 0:nloc], lhsT=lq,
                                                            rhs=kT[psl, 256 + lo * 128: 256 + (hi + 1) * 128],
                                                            start=True, stop=True), [R("qT", c, n // 4)] + krs, [RB(sa)])
                                      PE(lambda e: e.matmul(psum[:, sbk, 0:256], lhsT=lq, rhs=kT[psl, 0:256],
                                                            start=True, stop=True), [R("qT", c, n // 4)] + krs, [RB(sbk)])
                                      V(lambda e: e.tensor_tensor(out=scx[:, 0:nloc], in0=psum[:, sa, 0:nloc],
                                                                  in1=amask_t[:, moff:moff + nloc], op=ALU.add),
                                        [RB(sa), R("amask")], [rsc])
                                      A(lambda e: e.activation(out=scx[:, nloc:nk], in_=psum[:, sbk, 0:256], func=AF.Copy),
                                        [RB(sbk)], [rsc])
                                      V(lambda e: e.reduce_max(out=smx[:, 0:1], in_=scx[:, 0:nk], axis=AX.X), [rsc], [rsm])
                                      V(lambda e: e.tensor_scalar(out=smx[:, 1:2], in0=smx[:, 0:1], scalar1=-0.125,
                                                                  scalar2=nsink_t[:, hq:hq + 1], op0=ALU.mult, op1=ALU.min),
                                        [rsm, R("nsink")], [rsm])
                                      A(lambda e: e.activation(out=Px[:, 0:nk], in_=scx[:, 0:nk], func=AF.Exp, scale=0.125,
                                                               bias=smx[:, 1:2], accum_out=smx[:, 2:3]), [rsc, rsm], [rP, rsm])
                                      A(lambda e: e.activation(out=smx[:, 3:4], in_=sink_t[:, hq:hq + 1], func=AF.Exp, scale=1.0,
                                                               bias=smx[:, 1:2]), [rsm, R("sink")], [rsm])
                                      V(lambda e: e.tensor_tensor(out=smx[:, 4:5], in0=smx[:, 2:3], in1=smx[:, 3:4], op=ALU.add),
                                        [rsm], [rsm])
                                      V(lambda e: e.reciprocal(out=smx[:, 5:6], in_=smx[:, 4:5]), [rsm], [rsm])
                                      V(lambda e: e.tensor_scalar(out=dgx[:], in0=ident_b[:], scalar1=smx[:, 5:6], scalar2=None,
                                                                  op0=ALU.mult), [rsm, R("ident_b")], [rdg])
                                      for kb in range(nkb):
                                          bank = 4 + kb // 4
                                          off = (kb % 4) * 128
                                          PE(lambda e: e.matmul(psum[:, bank, off:off + 128], lhsT=Px[:, kb * 128:(kb + 1) * 128],
                                                                rhs=dgx[:], start=True, stop=True), [rP, rdg], [RB(bank)])
                                      A(lambda e: e.activation(out=pts[:, 0:4, c, :],
                                                               in_=psum[:, 4, :].rearrange("p (k q) -> p k q", q=128),
                                                               func=AF.Copy), [RB(4)], [rpts])
                                      if nkb == 5:
                                          V(lambda e: e.tensor_copy(out=pts[:, 4, c, :], in_=psum[:, 5, 0:128]), [RB(5)], [rpts])
                                  ob = 6 + pvc % 2
                                  pvc += 1
                                  for kb in range(nkb):
                                      blk = (2 + lo + kb) if kb < nlb else (kb - nlb)
                                      PE(lambda e: e.matmul(psum[0:64, ob, :], lhsT=vtok[:, blk, g * 64:(g + 1) * 64],
                                                            rhs=pts[:, kb, :, :].rearrange("p c q -> p (c q)"),
                                                            start=(kb == 0), stop=(kb == nkb - 1)),
                                         [R("vtok", blk), rpts], [RB(ob)])
                                  A(lambda e: e.activation(out=attnT[:, g * 4:(g + 1) * 4, n * 128:(n + 1) * 128],
                                                           in_=psum[0:64, ob, :].rearrange("p (c q) -> p c q", q=128),
                                                           func=AF.Copy), [RB(ob)], [R("attnT", n // 4)])
                              prologue_steps(3)
                          S.barrier()
                  S.barrier()
                  if DEBUG["stop"] == "C":
                      S.dead = True

                  with ExitStack() as pd:
                      cu = sb(pd, "cu", [128, L + 2], F32)
                      yc = sb(pd, "yc", [128, L], F32)
                      gbs = sb(pd, "gbs", [128, L], F32)
                      ut = [sb(pd, f"ut{i}", [128, 512], F32) for i in range(2)]
                      G(lambda e: e.memset(cu[:, 0:1], 0.0), [], [R("cu_pad")])
                      G(lambda e: e.memset(cu[:, L + 1:L + 2], 0.0), [], [R("cu_pad")])
                      cnt = 0
                      for c in range(4):
                          (wg, rg) = ws.get()
                          (wu, ru) = ws.get()
                          (wb_, rb_) = ws.get()
                          ws.prefetch(2)
                          for tg in range(4):
                              bk = (0, 1, 2) if cnt % 2 == 0 else (3, 4, 5)
                              utx = ut[cnt % 2]
                              rut = R("ut", cnt % 2)
                              cnt += 1
                              for (w_, r_, b_) in ((wg, rg, bk[0]), (wu, ru, bk[1]), (wb_, rb_, bk[2])):
                                  for kc in range(8):
                                      PE(lambda e: e.matmul(psum[:, b_, :], lhsT=w_[:, kc, :], rhs=hT[:, kc, tg * 512:(tg + 1) * 512],
                                                            start=(kc == 0), stop=(kc == 7)), [r_, R("hT", tg)], [RB(b_)])
                              A(lambda e: e.activation(out=utx[:], in_=psum[:, bk[1], :], func=AF.Copy), [RB(bk[1])], [rut])
                              V(lambda e: e.tensor_tensor(out=cu[:, 1 + tg * 512: 1 + (tg + 1) * 512], in0=psum[:, bk[0], :],
                                                          in1=utx[:], op=ALU.mult), [RB(bk[0]), rut], [R("cu", tg)])
                              A(lambda e: e.activation(out=gbs[:, tg * 512:(tg + 1) * 512], in_=psum[:, bk[2], :], func=AF.Copy),
                                [RB(bk[2])], [R("gbs", tg)])
                          cur = [R("cu", t) for t in range(4)] + [R("cu_pad")]
                          V(lambda e: e.tensor_scalar(out=yc[:], in0=cu[:, 0:L], scalar1=convw_t[:, c, 0:1], scalar2=None,
                                                      op0=ALU.mult), cur + [R("convw")], [R("yc")])
                          V(lambda e: e.scalar_tensor_tensor(out=yc[:], in0=cu[:, 1:L + 1], scalar=convw_t[:, c, 1:2], in1=yc[:],
                                                             op0=ALU.mult, op1=ALU.add), cur + [R("yc")], [R("yc")])
                          V(lambda e: e.scalar_tensor_tensor(out=yc[:], in0=cu[:, 2:L + 2], scalar=convw_t[:, c, 2:3], in1=yc[:],
                                                             op0=ALU.mult, op1=ALU.add), cur + [R("yc")], [R("yc")])
                          G(lambda e: e.tensor_tensor(out=convo[:, c, :], in0=yc[:], in1=gbs[:], op=ALU.mult),
                            [R("yc")] + [R("gbs", t) for t in range(4)], [R("convo")])
                          prologue_steps(4)
                  S.barrier()
                  if DEBUG["stop"] == "D":
                      S.dead = True

                  with ExitStack() as pe_:
                      xs = sb(pe_, "xs", [128, 8, 512], F32)
                      w_ao_b = sb(pe_, "w_ao_b", [64, 8, 1024], BF16)
                      w_co_b = sb(pe_, "w_co_b", [128, 4, 1024], BF16)
                      w_mo_b = sb(pe_, "w_mo_b", [128, 8, 1024], BF16)
                      load_big(lambda pc: w_ao_b[:, pc, :], lambda pc: w_ao[:, pc, :], 8, 64, "w_ao_b")
                      load_big(lambda pc: w_co_b[:, pc, :], lambda pc: w_co[:, pc, :], 4, 128, "w_co_b")
                      load_big(lambda pc: w_mo_b[:, pc, :], lambda pc: w_mo[:, pc, :], 8, 128, "w_mo_b")
                      mixT = sb(pe_, "mixT", [128, 8, 512], BF16)
                      sg = [sb(pe_, f"sg{i}", [128, 512], F32) for i in range(2)]
                      mm_ = [sb(pe_, f"mm{i}", [128, 512], F32) for i in range(2)]
                      for tg in range(4):
                          tsl = slice(tg * 512, (tg + 1) * 512)
                          S.dma("sp", xs[:], xT[:, :, s * L + tg * 512: s * L + (tg + 1) * 512], writes=[R("xs")])
                          for oc in range(8):
                              (wga, rga) = ws.get()
                              (wgv, rgv) = ws.get()
                              ws.prefetch(2)
                              osl = slice(oc * 128, (oc + 1) * 128)
                              for h in range(8):
                                  PE(lambda e: e.matmul(psum[:, 0, :], lhsT=w_ao_b[:, h, osl], rhs=attnT[:, h, tsl],
                                                        start=(h == 0), stop=(h == 7)), [R("w_ao_b"), R("attnT", tg)], [RB(0)])
                              for c in range(4):
                                  PE(lambda e: e.matmul(psum[:, 1, :], lhsT=w_co_b[:, c, osl], rhs=convo[:, c, tsl],
                                                        start=(c == 0), stop=(c == 3)), [R("w_co_b"), R("convo")], [RB(1)])
                              for kc in range(8):
                                  PE(lambda e: e.matmul(psum[:, 2, :], lhsT=wga[:, kc, :], rhs=hT[:, kc, tsl],
                                                        start=(kc == 0), stop=(kc == 7)), [rga, R("hT", tg)], [RB(2)])
                              for kc in range(8):
                                  PE(lambda e: e.matmul(psum[:, 3, :], lhsT=wgv[:, kc, :], rhs=hT[:, kc, tsl],
                                                        start=(kc == 0), stop=(kc == 7)), [rgv, R("hT", tg)], [RB(3)])
                              A(lambda e: e.activation(out=sg[0][:], in_=psum[:, 2, :], func=AF.Sigmoid), [RB(2)], [R("sg", 0)])
                              A(lambda e: e.activation(out=sg[1][:], in_=psum[:, 3, :], func=AF.Sigmoid), [RB(3)], [R("sg", 1)])
                              V(lambda e: e.tensor_tensor(out=mm_[0][:], in0=psum[:, 0, :], in1=sg[0][:], op=ALU.mult),
                                [RB(0), R("sg", 0)], [R("mm", 0)])
                              V(lambda e: e.tensor_tensor(out=mm_[1][:], in0=psum[:, 1, :], in1=sg[1][:], op=ALU.mult),
                                [RB(1), R("sg", 1)], [R("mm", 1)])
                              G(lambda e: e.tensor_tensor(out=mixT[:, oc, :], in0=mm_[0][:], in1=mm_[1][:], op=ALU.add),
                                [R("mm", 0), R("mm", 1)], [R("mixT")])
                          for oc in range(8):
                              ob = 4 + oc % 2
                              osl = slice(oc * 128, (oc + 1) * 128)
                              for c in range(8):
                                  PE(lambda e: e.matmul(psum[:, ob, :], lhsT=w_mo_b[:, c, osl], rhs=mixT[:, c, :],
                                                        start=(c == 0), stop=(c == 7)), [R("w_mo_b"), R("mixT")], [RB(ob)])
                              V(lambda e: e.scalar_tensor_tensor(out=xs[:, oc, :], in0=psum[:, ob, :],
                                                                 scalar=modp[:, 16 + oc, s:s + 1], in1=xs[:, oc, :],
                                                                 op0=ALU.mult, op1=ALU.add), [RB(ob), R("xs"), R("modp")], [R("xs")])
                          S.dma("sp", x1T[:, :, s * L + tg * 512: s * L + (tg + 1) * 512], xs[:], reads=[R("xs")],
                                writes=[R("x1T", s * 4 + tg)])
                          if DEBUG["x1"]:
                              S.dma("sp", dbg["x1"][:, :, s * L + tg * 512: s * L + (tg + 1) * 512], xs[:], reads=[R("xs")],
                                    writes=[R("dbg_x1")])
                          prologue_steps(6)
                  S.barrier()

              while prologue_step():
                  pass
          pro_stack.close()
          S.barrier()

          TG = 256
          NG = TPC // TG
          if DEBUG["stop_after_mixer"]:
              NG = 0
          with ExitStack() as p2:
              pwq_b = sb(p2, "pwq_b", [128, 8, 2048], BF16)
              keys_b = sb(p2, "keys_b", [128, 16, 128], BF16)
              x1g = sb(p2, "x1g", [128, 8, TG], F32)
              sq2 = sb(p2, "sq2", [128, 8, TG], BF16)
              rstd2 = sb(p2, "rstd2", [128, TG], F32)
              ntmp = [sb(p2, f"ntmp{i}", [128, TG], F32) for i in range(2)]
              h2T = sb(p2, "h2T", [128, 8, TG], BF16)
              qpT = sb(p2, "qpT", [128, 16, TG], BF16)
              s1 = sb(p2, "s1", [128, 16, 128], F32)
              m16 = sb(p2, "m16", [128, 16, 16], F32)
              ix = sb(p2, "ix", [128, 16, 16], U32)
              ixf = sb(p2, "ixf", [128, 16, 16], F32)
              cand = sb(p2, "cand", [128, 8, 256], F32)
              b16 = sb(p2, "b16", [128, 8, 16], F32)
              pos = sb(p2, "pos", [128, 8, 16], U32)
              pab = sb(p2, "pab", [128, 2, 128], U32)
              pabf = sb(p2, "pabf", [128, 2, 128], F32)
              ee = sb(p2, "ee", [128, 8, 16], F32)
              zs = sb(p2, "zs", [128, 8], F32)
              IJG = sb(p2, "IJG", [128, 3, 128], F32)
              IJGT = sb(p2, "IJGT", [128, 3, TG], F32)
              NB = 16
              Pmx = [sb(p2, f"Pmx{i}", [128, NB, 128], BF16) for i in range(2)]
              Qmx = [sb(p2, f"Qmx{i}", [128, NB, 128], BF16) for i in range(2)]
              Wsum = sb(p2, "Wsum", [128, 128, TG], BF16)
              NS = 3
              ub = [sb(p2, f"ub{i}", [128, 8, 128], BF16) for i in range(NS)]
              vb = [sb(p2, f"vb{i}", [128, 1024], BF16) for i in range(NS)]
              ge = [sb(p2, f"ge{i}", [128, TG], F32) for i in range(2)]
              Lj = [sb(p2, f"Lj{i}", [128, TG], BF16) for i in range(2)]

              with ExitStack() as pl:
                  stg = sb(pl, "stg", [128, 2048], F32)
                  for kc in range(8):
                      S.dma("sp", stg[:], pw_q[:, kc, :], writes=[R("stg")])
                      G(lambda e: e.tensor_copy(out=pwq_b[:, kc, :], in_=stg[:]), [R("stg")], [R("pwq_b")])
                  S.dma("sp", stg[:], keysT[:, :, :].rearrange("p a b -> p (a b)"), writes=[R("stg")])
                  G(lambda e: e.tensor_copy(out=keys_b[:].rearrange("p a b -> p (a b)"), in_=stg[:]), [R("stg")], [R("keys_b")])
              S.barrier()

              scnt = 0
              acnt = 0
              for gi in range(NG):
                  s = gi // (NG // 2) if NG >= 2 else 0
                  t0 = gi * TG
                  S.dma("sp", x1g[:], x1T[:, :, t0:t0 + TG], reads=[R("x1T", t0 // 512)], writes=[R("x1g")])
                  norm_group(x1g[:], R("x1g"), TG, sq2, rstd2, ntmp, 4,
                             lambda c: A2[:, c, s:s + 1], lambda c: modp[:, 24 + c, s:s + 1],
                             lambda c: h2T[:, c, :], R("h2T"), False)
                  for hp in range(16):
                      bank = 4 + (hp // 2) % 4
                      off = (hp % 2) * TG
                      for kc in range(8):
                          PE(lambda e: e.matmul(psum[:, bank, off:off + TG], lhsT=pwq_b[:, kc, hp * 128:(hp + 1) * 128],
                                                rhs=h2T[:, kc, :], start=(kc == 0), stop=(kc == 7)),
                             [R("pwq_b"), R("h2T")], [RB(bank)])
                      if hp % 2 == 1:
                          A(lambda e: e.activation(out=qpT[:, hp - 1:hp + 1, :],
                                                   in_=psum[:, bank, :].rearrange("p (a t) -> p a t", t=TG), func=AF.Copy),
                            [RB(bank)], [R("qpT")])
                  for tt in range(TG // 128):
                      tsl = slice(tt * 128, (tt + 1) * 128)
                      for hp in range(16):
                          bank = 4 + hp // 4
                          off = (hp % 4) * 128
                          PE(lambda e: e.matmul(psum[:, bank, off:off + 128], lhsT=qpT[:, hp, tsl], rhs=keys_b[:, hp, :],
                                                start=True, stop=True), [R("qpT"), R("keys_b")], [RB(bank)])
                      A(lambda e: e.activation(out=s1[:].rearrange("p a b -> p (a b)"),
                                               in_=psum[:, 4:8, :].rearrange("p a b -> p (a b)"), func=AF.Copy),
                        [RB(4), RB(5), RB(6), RB(7)], [R("s1")])
                      for hp in range(16):
                          V(lambda e: e.max(out=m16[:, hp, 0:8], in_=s1[:, hp, :]), [R("s1")], [R("m16")])
                          V(lambda e: e.max_index(out=ix[:, hp, 0:8], in_max=m16[:, hp, 0:8], in_values=s1[:, hp, :]),
                            [R("s1"), R("m16")], [R("ix")])
                          V(lambda e: e.match_replace(out=s1[:, hp, :], in_to_replace=m16[:, hp, 0:8], in_values=s1[:, hp, :],
                                                      imm_value=NEG), [R("s1"), R("m16")], [R("s1")])
                          V(lambda e: e.max(out=m16[:, hp, 8:16], in_=s1[:, hp, :]), [R("s1")], [R("m16")])
                          V(lambda e: e.max_index(out=ix[:, hp, 8:16], in_max=m16[:, hp, 8:16], in_values=s1[:, hp, :]),
                            [R("s1"), R("m16")], [R("ix")])
                      V(lambda e: e.tensor_copy(out=ixf[:], in_=ix[:]), [R("ix")], [R("ixf")])
                      m4 = m16[:].rearrange("p (h two) k -> p h two k", two=2)
                      i4 = ixf[:].rearrange("p (h two) k -> p h two k", two=2)
                      V(lambda e: e.tensor_tensor(out=cand[:].rearrange("p h (a b) -> p h a b", b=16),
                                                  in0=m4[:, :, 0, :].unsqueeze(3).to_broadcast([128, 8, 16, 16]),
                                                  in1=m4[:, :, 1, :].unsqueeze(2).to_broadcast([128, 8, 16, 16]), op=ALU.add),
                        [R("m16")], [R("cand")])
                      for h in range(8):
                          V(lambda e: e.max(out=b16[:, h, 0:8], in_=cand[:, h, :]), [R("cand")], [R("b16")])
                          V(lambda e: e.max_index(out=pos[:, h, 0:8], in_max=b16[:, h, 0:8], in_values=cand[:, h, :]),
                            [R("cand"), R("b16")], [R("pos")])
                          V(lambda e: e.match_replace(out=cand[:, h, :], in_to_replace=b16[:, h, 0:8], in_values=cand[:, h, :],
                                                      imm_value=NEG), [R("cand"), R("b16")], [R("cand")])
                          V(lambda e: e.max(out=b16[:, h, 8:16], in_=cand[:, h, :]), [R("cand")], [R("b16")])
                          V(lambda e: e.max_index(out=pos[:, h, 8:16], in_max=b16[:, h, 8:16], in_values=cand[:, h, :]),
                            [R("cand"), R("b16")], [R("pos")])
                      V(lambda e: e.tensor_tensor(out=ee[:], in0=b16[:], in1=b16[:, :, 0:1].to_broadcast([128, 8, 16]),
                                                  op=ALU.subtract), [R("b16")], [R("ee")])
                      A(lambda e: e.activation(out=ee[:], in_=ee[:], func=AF.Exp), [R("ee")], [R("ee")])
                      V(lambda e: e.reduce_sum(out=zs[:], in_=ee[:], axis=AX.X), [R("ee")], [R("zs")])
                      V(lambda e: e.reciprocal(out=zs[:], in_=zs[:]), [R("zs")], [R("zs")])
                      V(lambda e: e.tensor_tensor(out=IJG[:, 2, :].rearrange("p (h k) -> p h k", k=16), in0=ee[:],
                                                  in1=zs[:, :].unsqueeze(2).to_broadcast([128, 8, 16]), op=ALU.mult),
                        [R("ee"), R("zs")], [R("IJG")])
                      posf = pos[:].rearrange("p h k -> p (h k)")
                      V(lambda e: e.tensor_scalar(out=pab[:, 0, :], in0=posf, scalar1=cu32_t[:, 0:1], scalar2=None,
                                                  op0=ALU.logical_shift_right), [R("pos"), R("cu32")], [R("pab")])
                      V(lambda e: e.tensor_scalar(out=pab[:, 1, :], in0=posf, scalar1=cu32_t[:, 1:2], scalar2=None,
                                                  op0=ALU.bitwise_and), [R("pos"), R("cu32")], [R("pab")])
                      V(lambda e: e.tensor_copy(out=pabf[:], in_=pab[:]), [R("pab")], [R("pabf")])
                      eq = cand[:].rearrange("p h (a b) -> p h a b", b=16)
                      io = iota_t[:, 0:16].unsqueeze(1).unsqueeze(1).to_broadcast([128, 8, 16, 16])
                      for w in range(2):
                          sel = pabf[:, w, :].rearrange("p (h k) -> p h k", k=16).unsqueeze(3).to_broadcast([128, 8, 16, 16])
                          V(lambda e: e.tensor_tensor(out=eq, in0=sel, in1=io, op=ALU.is_equal),
                            [R("pabf"), R("iota"), R("cand")], [R("cand")])
                          V(lambda e: e.tensor_tensor(out=eq, in0=eq, in1=i4[:, :, w, :].unsqueeze(2).to_broadcast([128, 8, 16, 16]),
                                                      op=ALU.mult), [R("cand"), R("ixf")], [R("cand")])
                          V(lambda e: e.tensor_reduce(out=IJG[:, w, :].rearrange("p (h k) -> p h k", k=16), in_=eq,
                                                      axis=AX.X, op=ALU.add), [R("cand")], [R("IJG")])
                      if DEBUG["sel"] and gi == 0 and tt == 0:
                          S.dma("sp", dbg["sel"][:, :, :], IJG[:], reads=[R("IJG")], writes=[R("dbg_sel")])
                      for w in range(3):
                          PE(lambda e: e.transpose(out=psum[:, 4, w * 128:(w + 1) * 128], in_=IJG[:, w, :], identity=ident_f[:]),
                             [R("IJG"), R("ident_f")], [RB(4)])
                      A(lambda e: e.activation(out=IJGT[:, :, tsl], in_=psum[:, 4, 0:384].rearrange("p (w t) -> p w t", t=128),
                                               func=AF.Copy), [RB(4)], [R("IJGT")])
                      for bt in range(128 // NB):
                          i2 = scnt % 2
                          scnt += 1
                          Px_, Qx_ = Pmx[i2], Qmx[i2]
                          rPx, rQx = R("Pmx", i2), R("Qmx", i2)
                          wb0 = 4 * (i2)
                          tb0 = tt * 128 + bt * NB
                          iob = iota_t[:, :].unsqueeze(1).to_broadcast([128, NB, 128])
                          V(lambda e: e.tensor_tensor(out=Px_[:], in0=IJGT[:, 0, tb0:tb0 + NB].unsqueeze(2).to_broadcast([128, NB, 128]),
                                                      in1=iob, op=ALU.is_equal), [R("IJGT"), R("iota")], [rPx])
                          V(lambda e: e.tensor_tensor(out=Qx_[:], in0=IJGT[:, 1, tb0:tb0 + NB].unsqueeze(2).to_broadcast([128, NB, 128]),
                                                      in1=iob, op=ALU.is_equal), [R("IJGT"), R("iota")], [rQx])
                          V(lambda e: e.tensor_tensor(out=Qx_[:], in0=Qx_[:],
                                                      in1=IJGT[:, 2, tb0:tb0 + NB].unsqueeze(2).to_broadcast([128, NB, 128]),
                                                      op=ALU.mult), [R("IJGT"), rQx], [rQx])
                          for tl in range(NB):
                              bank = 4 + tl // 4
                              off = (tl % 4) * 128
                              PE(lambda e: e.matmul(psum[:, bank, off:off + 128], lhsT=Px_[:, tl, :], rhs=Qx_[:, tl, :],
                                                    start=True, stop=True), [rPx, rQx], [RB(bank)])
                          tb = tt * 128 + bt * NB
                          A(lambda e: e.activation(out=Wsum[:, :, tb:tb + NB],
                                                   in_=psum[:, 4:8, :].rearrange("p a (t j) -> p j (a t)", j=128),
                                                   func=AF.Copy), [RB(4), RB(5), RB(6), RB(7)], [R("Wsum")])
                  for b in range(4):
                      PE(lambda e: e.matmul(psum[:, b, :], lhsT=zeros_b[:, 0:128], rhs=zeros_b[:], start=True, stop=False,
                                            skip_group_check=True), [R("zeros_b")], [RB(b)])
                  for j in range(128):
                      sl = acnt % NS
                      i2 = acnt % 2
                      acnt += 1
                      S.dma("sp", ub[sl][:].rearrange("p a b -> p (a b)"), uTb[j], reads=[R("scr_u", j)], writes=[R("ub", sl)])
                      S.dma("sp", vb[sl][:], vtb[j], reads=[R("scr_v", j)], writes=[R("vb", sl)])
                      ab = 4 + i2
                      for dc in range(8):
                          PE(lambda e: e.matmul(psum[:, ab, 0:TG], lhsT=ub[sl][:, dc, :], rhs=h2T[:, dc, :],
                                                start=(dc == 0), stop=(dc == 7)), [R("ub", sl), R("h2T")], [RB(ab)])
                      A(lambda e: e.activation(out=ge[i2][:], in_=psum[:, ab, 0:TG], func=AF.Gelu), [RB(ab)], [R("ge", i2)])
                      V(lambda e: e.tensor_tensor(out=Lj[i2][:], in0=ge[i2][:], in1=Wsum[:, j, :], op=ALU.mult),
                        [R("ge", i2), R("Wsum")], [R("Lj", i2)])
                      for dc in range(8):
                          PE(lambda e: e.matmul(psum[:, dc // 2, (dc % 2) * TG:(dc % 2 + 1) * TG],
                                                lhsT=vb[sl][:, dc * 128:(dc + 1) * 128], rhs=Lj[i2][:],
                                                start=False, stop=(j == 127), skip_group_check=True),
                             [R("vb", sl), R("Lj", i2)], [RB(dc // 2)])
                  for dc in range(8):
                      V(lambda e: e.scalar_tensor_tensor(out=x1g[:, dc, :], in0=psum[:, dc // 2, (dc % 2) * TG:(dc % 2 + 1) * TG],
                                                         scalar=modp[:, 40 + dc, s:s + 1], in1=x1g[:, dc, :],
                                                         op0=ALU.mult, op1=ALU.add), [RB(dc // 2), R("x1g"), R("modp")], [R("x1g")])
                  norm_group(x1g[:], R("x1g"), TG, sq2, rstd2, None, 4,
                             lambda c: fing_t[:, c:c + 1], None,
                             lambda c: x1g[:, c, :], R("x1g"), True)
                  S.dma("sp", outT[:, :, t0:t0 + TG], x1g[:], reads=[R("x1g")], writes=[R("outT", gi)])
        except _Stop:
            pass
        build.stats = dict(S.cnt); build.stats["dma"] = dict(S.dq); build.stats["sig"] = dict(S.sig)
        build.used = S.used
        S.finish([R("outT", gi) for gi in range(NG)] + ([R("dbg_x1")] if DEBUG["x1"] else [])
                 + ([R("dbg_sel")] if DEBUG["sel"] else []) + [R("x1T", i) for i in range(8)])
    return nc


def _fm(a):
    rows, D = a.shape
    return np.ascontiguousarray(a.T.reshape(D // 128, 128, rows).transpose(1, 0, 2))


def _partner(d):
    return d + 16 if (d % 32) < 16 else d - 16


def prep_shared(inp):
    f = np.float32
    sh = {}
    w_mod = np.asarray(inp["w_mod"], f)[0]
    sh["w_mod"] = np.ascontiguousarray(w_mod.reshape(8, 128, 6144).transpose(1, 0, 2))
    sh["b_mod"] = np.ascontiguousarray(np.asarray(inp["b_mod"], f)[0].reshape(48, 128).T)
    sh["n1g"] = np.ascontiguousarray(np.asarray(inp["norm1_g"], f)[0].reshape(8, 128).T)
    sh["n2g"] = np.ascontiguousarray(np.asarray(inp["norm2_g"], f)[0].reshape(8, 128).T)
    sh["fing"] = np.ascontiguousarray(np.asarray(inp["final_g"], f).reshape(8, 128).T)
    w_in = np.asarray(inp["w_in"], f)[0]
    pr = np.array([_partner(d) for d in range(64)])
    chunks = []
    for c in range(4):
        chunks.append(np.concatenate([c * 64 + np.arange(64), (4 + c) * 64 + np.arange(64)]))
    for c in range(4):
        chunks.append(np.concatenate([c * 64 + pr, (4 + c) * 64 + pr]))
    chunks.append(512 + np.arange(128))
    chunks.append(512 + np.concatenate([pr, 64 + pr]))
    chunks.append(640 + np.arange(128))
    GB, GC, UU, GA, GV = 768, 1280, 1792, 2304, 3328
    for c in range(4):
        chunks.append(GC + c * 128 + np.arange(128))
        chunks.append(UU + c * 128 + np.arange(128))
        chunks.append(GB + c * 128 + np.arange(128))
    for c in range(8):
        chunks.append(GA + c * 128 + np.arange(128))
    for c in range(8):
        chunks.append(GV + c * 128 + np.arange(128))
    assert len(chunks) == 39
    wl = np.empty((39, 128, 8, 128), f)
    for i, cols in enumerate(chunks):
        wl[i] = w_in[:, cols].reshape(8, 128, 128).transpose(1, 0, 2)
    sh["w_in"] = wl
    sh["w_ao"] = np.ascontiguousarray(np.asarray(inp["w_attn_out"], f)[0].reshape(8, 64, 1024).transpose(1, 0, 2))
    sh["w_co"] = np.ascontiguousarray(np.asarray(inp["w_conv_out"], f)[0].reshape(4, 128, 1024).transpose(1, 0, 2))
    sh["w_mo"] = np.ascontiguousarray(np.asarray(inp["w_mix_out"], f)[0].reshape(8, 128, 1024).transpose(1, 0, 2))
    sh["conv_w"] = np.ascontiguousarray(np.asarray(inp["conv_w"], f)[0].reshape(3, 4, 128).transpose(2, 1, 0))
    sh["sinkv"] = np.ascontiguousarray(np.broadcast_to(np.asarray(inp["attn_sink"], f)[0][None, :], (128, 8)))
    sh["pw_q"] = np.ascontiguousarray(np.asarray(inp["peer_w_q"], f)[0].reshape(8, 128, 2048).transpose(1, 0, 2))
    sk = np.asarray(inp["peer_sub_keys"], f)[0]
    sh["keysT"] = np.ascontiguousarray(sk.reshape(16, 128, 128).transpose(2, 0, 1))
    pu = np.asarray(inp["peer_u"], f)[0]
    sh["uT"] = np.ascontiguousarray(pu.reshape(128, 128, 8, 128).transpose(1, 3, 2, 0)).reshape(128, 128, 1024)
    pv = np.asarray(inp["peer_v"], f)[0]
    sh["vt"] = np.ascontiguousarray(pv.reshape(128, 128, 1024).transpose(1, 0, 2))
    inv_freq = (np.float32(10000.0) ** (-np.arange(16, dtype=f) / np.float32(16))).astype(f)
    l = np.arange(L)
    cos_t = np.empty((128, L), f)
    sin_t = np.empty((128, L), f)
    for p in range(128):
        d = p % 64
        posn = (l % 64) if d >= 32 else (l // 64)
        ang = posn.astype(f) * inv_freq[d % 16]
        cos_t[p] = np.cos(ang)
        sin_t[p] = np.sin(ang) * (-1.0 if (d % 32) < 16 else 1.0)
    sh["rope_cos"] = cos_t
    sh["rope_sin"] = sin_t
    qi = np.arange(128)[:, None]
    kk = np.arange(384)[None, :]
    sh["amask"] = np.where((kk >= qi) & (kk <= qi + 256), 0.0, NEG).astype(f)
    sh["ident"] = np.eye(128, dtype=f)
    sh["iota128"] = np.ascontiguousarray(np.broadcast_to(np.arange(128, dtype=f)[None, :], (128, 128)))
    sh["cu32"] = np.ascontiguousarray(np.broadcast_to(np.array([4, 15], np.uint32)[None, :], (128, 2)))
    return sh


def prep_core(inp, core, sh):
    f = np.float32
    x = np.asarray(inp["x"], f)
    ctx = np.asarray(inp["ctx"], f)
    c = np.asarray(inp["c"], f)
    c_ctx = np.asarray(inp["c_ctx"], f)
    m = dict(sh)
    b0 = 2 * core
    m["xT"] = _fm(x[b0:b0 + 2].reshape(2 * L, 1024))
    m["ctxT"] = _fm(ctx[b0:b0 + 2].reshape(512, 1024))
    m["cT"] = _fm(np.stack([c[b0], c[b0 + 1], c_ctx], 0))
    return m


_NC_CACHE = {}


def kernel(**inputs):
    sh = prep_shared(inputs)
    in_maps = [prep_core(inputs, core, sh) for core in range(NCORES)]
    if "nc" not in _NC_CACHE:
        build()
        _NC_CACHE["nc"] = build(used=set(build.used))
    nc = _NC_CACHE["nc"]
    res = run_bass_kernel_spmd(nc, in_maps, core_ids=list(range(NCORES)))
    out = np.empty((16, L, 1024), np.float32)
    for core in range(NCORES):
        o = np.asarray(res.results[core]["outT"])
        o = o.transpose(2, 1, 0).reshape(2, L, 1024)
        out[2 * core:2 * core + 2] = o
    return out
```

```python
import numpy as np
from contextlib import ExitStack
import concourse.bass as bass
import concourse.mybir as mybir
from concourse.bass_utils import run_bass_kernel_spmd

F32 = mybir.dt.float32
BF16 = mybir.dt.bfloat16
U32 = mybir.dt.uint32
AF = mybir.ActivationFunctionType
ALU = mybir.AluOpType
AX = mybir.AxisListType

NCORES = 8
L = 2048
TPC = 2 * L
EPS = 1e-6
NEG = -1e30
DEBUG = {"x1": False, "sel": False, "stop_after_mixer": False, "stop": None, "no_prologue": False}


class _Stop(Exception):
    pass


class Res:
    __slots__ = ("name", "w", "r")

    def __init__(self, name):
        self.name = name
        self.w = None
        self.r = {}


class Sched:
    ENG = ("pe", "act", "dve", "pool", "sp")

    def __init__(self, nc, stack, ndma=12, used=None):
        self.nc = nc
        self.used_in = used
        self.used = set()
        self.sig = {}
        self.vmap = {}
        self.e = {"pe": nc.tensor, "act": nc.scalar, "dve": nc.vector, "pool": nc.gpsimd, "sp": nc.sync}
        self.sem = {}
        self.cnt = {}
        for k in self.ENG:
            self.sem[k] = stack.enter_context(nc.semaphore("sem_" + k))
            self.cnt[k] = 0
        self.ndma = ndma
        self.dq = {}
        for q in ("sp", "pool"):
            for i in range(ndma):
                self.sem[(q, i)] = stack.enter_context(nc.semaphore(f"dsem_{q}_{i}"))
            self.dq[q] = 0
        self.waited = {k: {} for k in self.ENG}
        self.dead = False

    def _hw(self, tok):
        key, val = tok
        self.used.add(tok)
        if self.used_in is not None and isinstance(key, str):
            return self.vmap[tok]
        return val

    def _wait(self, eng, tok):
        key, val = tok
        if self.waited[eng].get(key, 0) >= val:
            return
        self.e[eng].wait_ge(self.sem[key], self._hw(tok))
        self.waited[eng][key] = val

    def _deps(self, eng, reads, writes, attach=False):
        toks = {}

        def add(t):
            if t is None:
                return
            k, v = t
            if toks.get(k, 0) < v:
                toks[k] = v
        for r in reads:
            add(r.w)
        for w in writes:
            add(w.w)
            for k, v in w.r.items():
                add((k, v))
        need = [(k, v) for k, v in toks.items() if self.waited[eng].get(k, 0) < v and not (eng == "pe" and k == "pe")]
        if attach and need:
            last = need.pop()
        else:
            last = None
        for k, v in need:
            self._wait(eng, (k, v))
        return last

    def _attach(self, eng, ins, last):
        if last is not None:
            ins._wait_ge(self.sem[last[0]], self._hw(last))
            self.waited[eng][last[0]] = last[1]

    def _commit(self, tok, reads, writes):
        k, v = tok
        for r in reads:
            if r.r.get(k, 0) < v:
                r.r[k] = v
        for w in writes:
            w.w = tok
            w.r = {}

    def op(self, eng, fn, reads=(), writes=()):
        if self.dead:
            return None
        last = self._deps(eng, reads, writes, attach=True)
        ins = fn(self.e[eng])
        self._attach(eng, ins, last)
        self.cnt[eng] += 1
        tok = (eng, self.cnt[eng])
        if self.used_in is None:
            ins.then_inc(self.sem[eng], 1)
        elif tok in self.used_in:
            self.sig[eng] = self.sig.get(eng, 0) + 1
            self.vmap[tok] = self.sig[eng]
            ins.then_inc(self.sem[eng], 1)
        self._commit(tok, reads, writes)
        return tok

    def dma(self, q, out, in_, reads=(), writes=()):
        if self.dead:
            return None
        n = self.dq[q]
        self.dq[q] += 1
        slot = n % self.ndma
        rnd = n // self.ndma
        key = (q, slot)
        if rnd > 0:
            self._wait(q, (key, 16 * rnd))
        last = self._deps(q, reads, writes, attach=True)
        ins = self.e[q].dma_start(out=out, in_=in_)
        self._attach(q, ins, last)
        ins.then_inc(self.sem[key], 16)
        tok = (key, 16 * (rnd + 1))
        self._commit(tok, reads, writes)
        return tok

    def barrier(self):
        if self.dead:
            return
        toks = [(k, self.cnt[k]) for k in self.ENG if self.cnt[k] > 0]
        for q, n in self.dq.items():
            for slot in range(min(n, self.ndma)):
                rnd = (n - 1 - slot) // self.ndma
                toks.append(((q, slot), 16 * (rnd + 1)))
        for e in self.ENG:
            for t in toks:
                self._wait(e, t)

    def finish(self, ress):
        for r in ress:
            if r.w is not None:
                self._wait("sp", r.w)


def build(used=None):
    nc = bass.Bass("TRN2", target_bir_lowering=False)

    def din(name, shape, dt=F32):
        return nc.dram_tensor(name, list(shape), dt, kind="ExternalInput").ap()

    xT = din("xT", [128, 8, TPC])
    ctxT = din("ctxT", [128, 8, 512])
    cT = din("cT", [128, 8, 3])
    w_mod = din("w_mod", [128, 8, 6144])
    b_mod = din("b_mod", [128, 48])
    n1g = din("n1g", [128, 8])
    n2g = din("n2g", [128, 8])
    fing = din("fing", [128, 8])
    w_in = din("w_in", [39, 128, 8, 128])
    w_ao = din("w_ao", [64, 8, 1024])
    w_co = din("w_co", [128, 4, 1024])
    w_mo = din("w_mo", [128, 8, 1024])
    conv_w = din("conv_w", [128, 4, 3])
    sinkv = din("sinkv", [128, 8])
    pw_q = din("pw_q", [128, 8, 2048])
    keysT = din("keysT", [128, 16, 128])
    uT = din("uT", [128, 128, 1024])
    vt = din("vt", [128, 128, 1024])
    rope_cos = din("rope_cos", [128, L])
    rope_sin = din("rope_sin", [128, L])
    amask = din("amask", [128, 384])
    ident = din("ident", [128, 128])
    iota128 = din("iota128", [128, 128])
    cu32 = din("cu32", [128, 2], U32)
    outT = nc.dram_tensor("outT", [128, 8, TPC], F32, kind="ExternalOutput").ap()
    x1T = nc.dram_tensor("x1T_scr", [128, 8, TPC], F32, kind="Internal").ap()
    uvb_d = nc.dram_tensor("uvb_scr", [128, 128, 2048], BF16, kind="Internal").ap()
    dbg = {}
    if DEBUG["x1"]:
        dbg["x1"] = nc.dram_tensor("dbg_x1", [128, 8, TPC], F32, kind="ExternalOutput").ap()
    if DEBUG["sel"]:
        dbg["sel"] = nc.dram_tensor("dbg_sel", [128, 3, 128], F32, kind="ExternalOutput").ap()

    with ExitStack() as st:
        S = Sched(nc, st, used=used)
        NG = 0
        RES = {}

        def R(*key):
            r = RES.get(key)
            if r is None:
                r = RES[key] = Res(str(key))
            return r

        _uid = [0]

        def sb(stack, name, shape, dt):
            _uid[0] += 1
            return stack.enter_context(nc.sbuf_tensor(f"{name}_{_uid[0]}", list(shape), dt))

        def V(fn, reads, writes):
            return S.op("dve", fn, reads, writes)

        def A(fn, reads, writes):
            return S.op("act", fn, reads, writes)

        def G(fn, reads, writes):
            return S.op("pool", fn, reads, writes)

        def PE(fn, reads, writes):
            return S.op("pe", fn, reads, writes)

        psum = st.enter_context(nc.psum_tensor("psum", [128, 8, 512], F32))

        def RB(b):
            return R("psb", b)

        ident_f = sb(st, "ident_f", [128, 128], F32)
        ident_b = sb(st, "ident_b", [128, 128], BF16)
        ones_b = sb(st, "ones_b", [128, 128], BF16)
        zeros_b = sb(st, "zeros_b", [128, 512], BF16)
        iota_t = sb(st, "iota_t", [128, 128], F32)
        cu32_t = sb(st, "cu32_t", [128, 2], U32)
        eps_t = sb(st, "eps_t", [128, 1], F32)
        modp = sb(st, "modp", [128, 48, 3], F32)
        A1 = sb(st, "A1", [128, 8, 3], F32)
        A2 = sb(st, "A2", [128, 8, 3], F32)
        bmod_t = sb(st, "bmod_t", [128, 48], F32)
        n1g_t = sb(st, "n1g_t", [128, 8], F32)
        n2g_t = sb(st, "n2g_t", [128, 8], F32)
        fing_t = sb(st, "fing_t", [128, 8], F32)
        sink_t = sb(st, "sink_t", [128, 8], F32)
        nsink_t = sb(st, "nsink_t", [128, 8], F32)
        convw_t = sb(st, "convw_t", [128, 4, 3], F32)

        S.dma("sp", ident_f[:], ident[:, :], writes=[R("ident_f")])
        S.dma("sp", iota_t[:], iota128[:, :], writes=[R("iota")])
        S.dma("sp", cu32_t[:], cu32[:, :], writes=[R("cu32")])
        S.dma("sp", bmod_t[:], b_mod[:, :], writes=[R("bmod")])
        S.dma("sp", n1g_t[:], n1g[:, :], writes=[R("n1g")])
        S.dma("sp", n2g_t[:], n2g[:, :], writes=[R("n2g")])
        S.dma("sp", fing_t[:], fing[:, :], writes=[R("fing")])
        S.dma("sp", sink_t[:], sinkv[:, :], writes=[R("sink")])
        S.dma("sp", convw_t[:], conv_w[:, :, :], writes=[R("convw")])
        V(lambda e: e.tensor_copy(out=ident_b[:], in_=ident_f[:]), [R("ident_f")], [R("ident_b")])
        V(lambda e: e.memset(ones_b[:], 1.0), [], [R("ones_b")])
        V(lambda e: e.memset(zeros_b[:], 0.0), [], [R("zeros_b")])
        V(lambda e: e.memset(eps_t[:], EPS), [], [R("eps")])
        V(lambda e: e.tensor_scalar(out=nsink_t[:], in0=sink_t[:], scalar1=-1.0, scalar2=None, op0=ALU.mult),
          [R("sink")], [R("nsink")])

        NPB = 2
        pro_stack = st.enter_context(ExitStack())
        pst = [sb(pro_stack, f"pst{i}", [128, 1024], F32) for i in range(NPB)]
        pbf = [sb(pro_stack, f"pbf{i}", [128, 1024], BF16) for i in range(NPB)]
        pro_state = {"k": 0}
        NPRO = 0 if DEBUG["no_prologue"] else 256

        def pro_src_dst(k):
            tab, j = divmod(k, 128)
            if tab == 0:
                return uT[j], uvb_d[j][:, 0:1024], R("scr_u", j)
            return vt[j], uvb_d[j][:, 1024:2048], R("scr_v", j)

        def prologue_step():
            k = pro_state["k"]
            if k >= NPRO + 1:
                return False
            if k < NPRO:
                src, _, _ = pro_src_dst(k)
                S.dma("sp", pst[k % NPB][:], src, writes=[R("pst", k % NPB)])
            k2 = k - 1
            if 0 <= k2 < NPRO:
                _, dst, rr = pro_src_dst(k2)
                sl = k2 % NPB
                G(lambda e: e.tensor_copy(out=pbf[sl][:], in_=pst[sl][:]), [R("pst", sl)], [R("pbf", sl)])
                S.dma("sp", dst, pbf[sl][:], reads=[R("pbf", sl)], writes=[rr])
            pro_state["k"] = k + 1
            return True

        def prologue_steps(n):
            for _ in range(n):
                if not prologue_step():
                    break

        def norm_group(xs, rxs, N, sq, rstd, tmp2, bank, Acol, Bcol, outc, rout, in_place):
            A(lambda e: e.activation(out=sq[:, :, 0:N], in_=xs, func=AF.Square), [rxs], [R("sq")])
            for c in range(8):
                PE(lambda e: e.matmul(psum[:, bank, 0:N], lhsT=ones_b[:], rhs=sq[:, c, 0:N],
                                      start=(c == 0), stop=(c == 7)), [R("sq"), R("ones_b")], [RB(bank)])
            A(lambda e: e.activation(out=rstd[:, 0:N], in_=psum[:, bank, 0:N], func=AF.Sqrt,
                                     scale=1.0 / 1024.0, bias=eps_t[:, 0:1]), [RB(bank), R("eps")], [R("rstd")])
            V(lambda e: e.reciprocal(out=rstd[:, 0:N], in_=rstd[:, 0:N]), [R("rstd")], [R("rstd")])
            for c in range(8):
                if in_place:
                    t = xs[:, c, :]
                    rt = rxs
                else:
                    t = tmp2[c % 2][:, 0:N]
                    rt = R("ntmp", c % 2)
                V(lambda e: e.tensor_tensor(out=t, in0=xs[:, c, :], in1=rstd[:, 0:N], op=ALU.mult),
                  [rxs, R("rstd")], [rt])
                b = Bcol(c) if Bcol is not None else None
                if b is not None:
                    V(lambda e: e.tensor_scalar(out=outc(c), in0=t, scalar1=Acol(c), scalar2=b,
                                                op0=ALU.mult, op1=ALU.add), [rt], [rout])
                else:
                    V(lambda e: e.tensor_scalar(out=outc(c), in0=t, scalar1=Acol(c), scalar2=None,
                                                op0=ALU.mult), [rt], [rout])

        with ExitStack() as p0:
            cTt = sb(p0, "cTt", [128, 8, 3], F32)
            scT = sb(p0, "scT", [128, 8, 3], F32)
            wm = [sb(p0, f"wm{i}", [128, 8, 512], F32) for i in range(2)]
            S.dma("sp", cTt[:], cT[:, :, :], writes=[R("cTt")])
            A(lambda e: e.activation(out=scT[:], in_=cTt[:], func=AF.Silu), [R("cTt")], [R("scT")])
            for pc in range(12):
                b = pc % 2
                S.dma("sp", wm[b][:], w_mod[:, :, pc * 512:(pc + 1) * 512], writes=[R("wm", b)])
                for cc in range(4):
                    j = pc * 4 + cc
                    for kc in range(8):
                        PE(lambda e: e.matmul(psum[:, 0, j * 3:(j + 1) * 3], lhsT=wm[b][:, kc, cc * 128:(cc + 1) * 128],
                                              rhs=scT[:, kc, :], start=(kc == 0), stop=(kc == 7)),
                           [R("wm", b), R("scT")], [RB(0)])
            V(lambda e: e.tensor_tensor(out=modp[:], in0=psum[:, 0, 0:144].rearrange("p (a b) -> p a b", b=3),
                                        in1=bmod_t[:, :].unsqueeze(2).to_broadcast([128, 48, 3]), op=ALU.add),
              [RB(0), R("bmod")], [R("modp")])
            for (Ax, off, gt, rg) in ((A1, 8, n1g_t, "n1g"), (A2, 32, n2g_t, "n2g")):
                V(lambda e: e.tensor_scalar(out=Ax[:], in0=modp[:, off:off + 8, :], scalar1=1.0, scalar2=None,
                                            op0=ALU.add), [R("modp")], [R("A12")])
                V(lambda e: e.tensor_tensor(out=Ax[:], in0=Ax[:], in1=gt[:, :].unsqueeze(2).to_broadcast([128, 8, 3]),
                                            op=ALU.mult), [R("A12"), R(rg)], [R("A12")])
        S.barrier()

        try:
          with ExitStack() as p1:
              NW = 5
              wst = [sb(p1, f"wst{i}", [128, 8, 128], F32) for i in range(NW)]
              wbf = [sb(p1, f"wbf{i}", [128, 8, 128], BF16) for i in range(NW)]
              wcount = [0]

              def load_w(idx):
                  i = wcount[0] % NW
                  wcount[0] += 1
                  S.dma("sp", wst[i][:], w_in[idx], writes=[R("wst", i)])
                  G(lambda e: e.tensor_copy(out=wbf[i][:], in_=wst[i][:]), [R("wst", i)], [R("wbf", i)])
                  return wbf[i], R("wbf", i)

              class WStream:
                  def __init__(self, order):
                      self.order = order
                      self.pos = 0
                      self.q = []

                  def prefetch(self, n):
                      while len(self.q) < n and self.pos < len(self.order):
                          self.q.append(load_w(self.order[self.pos]))
                          self.pos += 1

                  def get(self):
                      self.prefetch(1)
                      return self.q.pop(0)

              hT = sb(p1, "hT", [128, 8, L], BF16)
              hcT = sb(p1, "hcT", [128, 8, 256], BF16)
              attnT = sb(p1, "attnT", [64, 8, L], BF16)
              convo = sb(p1, "convo", [128, 4, L], BF16)
              rstd = sb(p1, "rstd", [128, 512], F32)

              def load_big(dst_fn, src_fn, npieces, parts, rname):
                  for pc in range(npieces):
                      i = wcount[0] % NW
                      wcount[0] += 1
                      stg = wst[i][:].rearrange("p a b -> p (a b)")
                      S.dma("sp", stg[0:parts, :], src_fn(pc), writes=[R("wst", i)])
                      G(lambda e: e.tensor_copy(out=dst_fn(pc), in_=stg[0:parts, :]), [R("wst", i)], [R(rname)])


              for s in range(2):
                  order = [0, 4, 1, 5, 2, 6, 3, 7, 8, 9, 10] + list(range(11, 23))
                  for tg in range(4):
                      for oc in range(8):
                          order += [23 + oc, 31 + oc]
                  ws = WStream(order)
                  ws.prefetch(2)

                  with ExitStack() as psa:
                      xs = sb(psa, "xs", [128, 8, 512], F32)
                      sq = sb(psa, "sq", [128, 8, 512], BF16)
                      for tg in range(4):
                          S.dma("sp", xs[:], xT[:, :, s * L + tg * 512: s * L + (tg + 1) * 512], writes=[R("xs")])
                          norm_group(xs[:], R("xs"), 512, sq, rstd, None, tg % 2,
                                     lambda c: A1[:, c, s:s + 1], lambda c: modp[:, c, s:s + 1],
                                     lambda c: hT[:, c, tg * 512:(tg + 1) * 512], R("hT", tg), True)
                      S.dma("sp", xs[:, :, 0:256], ctxT[:, :, s * 256:(s + 1) * 256], writes=[R("xs")])
                      norm_group(xs[:, :, 0:256], R("xs"), 256, sq, rstd, None, 0,
                                 lambda c: A1[:, c, 2:3], lambda c: modp[:, c, 2:3],
                                 lambda c: hcT[:, c, :], R("hcT"), True)
                      prologue_steps(8)
                  S.barrier()
                  if DEBUG["stop"] == "A":
                      S.dead = True

                  with ExitStack() as pa:
                      qT = sb(pa, "qT", [128, 4, L], BF16)
                      kT = sb(pa, "kT", [128, 256 + L], BF16)
                      vtok = sb(pa, "vtok", [128, 18, 128], BF16)
                      amask_t = sb(pa, "amask_t", [128, 384], F32)
                      pb_ = ExitStack()
                      cos_t = sb(pb_, "cos_t", [128, L], F32)
                      sin_t = sb(pb_, "sin_t", [128, L], F32)
                      t1 = [sb(pb_, f"t1_{i}", [128, 512], F32) for i in range(2)]
                      t2 = [sb(pb_, f"t2_{i}", [128, 512], F32) for i in range(2)]
                      S.dma("sp", cos_t[:], rope_cos[:, :], writes=[R("cos")])
                      S.dma("sp", sin_t[:], rope_sin[:, :], writes=[R("sin")])
                      S.dma("sp", amask_t[:], amask[:, :], writes=[R("amask")])

                      it = 0

                      def proj_rope(wa, ra, wb_, rb_, dst_fn, rdst_fn):
                          nonlocal it
                          for tg in range(4):
                              ba, bb = (2, 3) if it % 2 == 0 else (4, 5)
                              tt1, tt2 = t1[it % 2], t2[it % 2]
                              r1, r2 = R("t1", it % 2), R("t2", it % 2)
                              it += 1
                              for kc in range(8):
                                  PE(lambda e: e.matmul(psum[:, ba, :], lhsT=wa[:, kc, :], rhs=hT[:, kc, tg * 512:(tg + 1) * 512],
                                                        start=(kc == 0), stop=(kc == 7)), [ra, R("hT", tg)], [RB(ba)])
                              for kc in range(8):
                                  PE(lambda e: e.matmul(psum[:, bb, :], lhsT=wb_[:, kc, :], rhs=hT[:, kc, tg * 512:(tg + 1) * 512],
                                                        start=(kc == 0), stop=(kc == 7)), [rb_, R("hT", tg)], [RB(bb)])
                              V(lambda e: e.tensor_tensor(out=tt1[:], in0=psum[:, ba, :], in1=cos_t[:, tg * 512:(tg + 1) * 512],
                                                          op=ALU.mult), [RB(ba), R("cos")], [r1])
                              V(lambda e: e.tensor_tensor(out=tt2[:], in0=psum[:, bb, :], in1=sin_t[:, tg * 512:(tg + 1) * 512],
                                                          op=ALU.mult), [RB(bb), R("sin")], [r2])
                              G(lambda e: e.tensor_tensor(out=dst_fn(tg), in0=tt1[:], in1=tt2[:], op=ALU.add),
                                [r1, r2], [rdst_fn(tg)])

                      for ch in range(4):
                          (wq, rq) = ws.get()
                          (wqs, rqs) = ws.get()
                          ws.prefetch(2)
                          proj_rope(wq, rq, wqs, rqs, lambda tg: qT[:, ch, tg * 512:(tg + 1) * 512],
                                    lambda tg: R("qT", ch, tg))
                          prologue_steps(4)
                      (wk, rk) = ws.get()
                      (wks, rks) = ws.get()
                      ws.prefetch(2)
                      for kc in range(8):
                          PE(lambda e: e.matmul(psum[:, 6, 0:256], lhsT=wk[:, kc, :], rhs=hcT[:, kc, :],
                                                start=(kc == 0), stop=(kc == 7)), [rk, R("hcT")], [RB(6)])
                      A(lambda e: e.activation(out=kT[:, 0:256], in_=psum[:, 6, 0:256], func=AF.Copy), [RB(6)], [R("kT", "ctx")])
                      proj_rope(wk, rk, wks, rks, lambda tg: kT[:, 256 + tg * 512: 256 + (tg + 1) * 512],
                                lambda tg: R("kT", tg))
                      (wv, rv) = ws.get()
                      ws.prefetch(3)
                      for blk in range(18):
                          bank = 6 + blk % 2
                          for kc in range(8):
                              if blk < 2:
                                  lh = hcT[:, kc, blk * 128:(blk + 1) * 128]
                                  rl = R("hcT")
                              else:
                                  lh = hT[:, kc, (blk - 2) * 128:(blk - 1) * 128]
                                  rl = R("hT", (blk - 2) // 4)
                              PE(lambda e: e.matmul(psum[:, bank, 0:128], lhsT=lh, rhs=wv[:, kc, :],
                                                    start=(kc == 0), stop=(kc == 7)), [rl, rv], [RB(bank)])
                          A(lambda e: e.activation(out=vtok[:, blk, :], in_=psum[:, bank, 0:128], func=AF.Copy),
                            [RB(bank)], [R("vtok", blk)])
                      prologue_steps(8)
                      pb_.close()
                      S.barrier()
                      if DEBUG["stop"] == "B":
                          S.dead = True

                      with ExitStack() as pc_:
                          sc = [sb(pc_, f"sc{i}", [128, 640], F32) for i in range(2)]
                          Pm = [sb(pc_, f"Pm{i}", [128, 640], BF16) for i in range(2)]
                          sm = [sb(pc_, f"sm{i}", [128, 8], F32) for i in range(2)]
                          dg = [sb(pc_, f"dg{i}", [128, 128], BF16) for i in range(2)]
                          PTs = [sb(pc_, f"PTs{i}", [128, 5, 4, 128], BF16) for i in range(2)]
                          hcnt = 0
                          pvc = 0
                          for n in range(16):
                              lo = max(n - 1, 0)
                              hi = min(n + 1, 15)
                              nlb = hi - lo + 1
                              nloc = nlb * 128
                              nk = nloc + 256
                              nkb = nlb + 2
                              moff = (lo - (n - 1)) * 128
                              krs = [R("kT", "ctx")] + [R("kT", t) for t in sorted(set([(lo * 128) // 512, (hi * 128 + 127) // 512]))]
                              for g in range(2):
                                  psl = slice(g * 64, (g + 1) * 64)
                                  pts = PTs[pvc % 2]
                                  rpts = R("PTs", pvc % 2)
                                  for c in range(4):
                                      hq = g * 4 + c
                                      i2 = hcnt % 2
                                      sa, sbk = (0, 1) if i2 == 0 else (2, 3)
                                      hcnt += 1
                                      scx, Px, smx, dgx = sc[i2], Pm[i2], sm[i2], dg[i2]
                                      rsc, rP, rsm, rdg = R("sc", i2), R("Pm", i2), R("sm", i2), R("dg", i2)
                                      lq = qT[psl, c, n * 128:(n + 1) * 128]
                                      PE(lambda e: e.matmul(psum[:, sa, 0:nloc], lhsT=lq,
                                                            rhs=kT[psl, 256 + lo * 128: 256 + (hi + 1) * 128],
                                                            start=True, stop=True), [R("qT", c, n // 4)] + krs, [RB(sa)])
                                      PE(lambda e: e.matmul(psum[:, sbk, 0:256], lhsT=lq, rhs=kT[psl, 0:256],
                                                            start=True, stop=True), [R("qT", c, n // 4)] + krs, [RB(sbk)])
                                      V(lambda e: e.tensor_tensor(out=scx[:, 0:nloc], in0=psum[:, sa, 0:nloc],
                                                                  in1=amask_t[:, moff:moff + nloc], op=ALU.add),
                                        [RB(sa), R("amask")], [rsc])
                                      A(lambda e: e.activation(out=scx[:, nloc:nk], in_=psum[:, sbk, 0:256], func=AF.Copy),
                                        [RB(sbk)], [rsc])
                                      V(lambda e: e.reduce_max(out=smx[:, 0:1], in_=scx[:, 0:nk], axis=AX.X), [rsc], [rsm])
                                      V(lambda e: e.tensor_scalar(out=smx[:, 1:2], in0=smx[:, 0:1], scalar1=-0.125,
                                                                  scalar2=nsink_t[:, hq:hq + 1], op0=ALU.mult, op1=ALU.min),
                                        [rsm, R("nsink")], [rsm])
                                      A(lambda e: e.activation(out=Px[:, 0:nk], in_=scx[:, 0:nk], func=AF.Exp, scale=0.125,
                                                               bias=smx[:, 1:2], accum_out=smx[:, 2:3]), [rsc, rsm], [rP, rsm])
                                      A(lambda e: e.activation(out=smx[:, 3:4], in_=sink_t[:, hq:hq + 1], func=AF.Exp, scale=1.0,
                                                               bias=smx[:, 1:2]), [rsm, R("sink")], [rsm])
                                      V(lambda e: e.tensor_tensor(out=smx[:, 4:5], in0=smx[:, 2:3], in1=smx[:, 3:4], op=ALU.add),
                                        [rsm], [rsm])
                                      V(lambda e: e.reciprocal(out=smx[:, 5:6], in_=smx[:, 4:5]), [rsm], [rsm])
                                      V(lambda e: e.tensor_scalar(out=dgx[:], in0=ident_b[:], scalar1=smx[:, 5:6], scalar2=None,
                                                                  op0=ALU.mult), [rsm, R("ident_b")], [rdg])
                                      for kb in range(nkb):
                                          bank = 4 + kb // 4
                                          off = (kb % 4) * 128
                                          PE(lambda e: e.matmul(psum[:, bank, off:off + 128], lhsT=Px[:, kb * 128:(kb + 1) * 128],
                                                                rhs=dgx[:], start=True, stop=True), [rP, rdg], [RB(bank)])
                                      A(lambda e: e.activation(out=pts[:, 0:4, c, :],
                                                               in_=psum[:, 4, :].rearrange("p (k q) -> p k q", q=128),
                                                               func=AF.Copy), [RB(4)], [rpts])
                                      if nkb == 5:
                                          V(lambda e: e.tensor_copy(out=pts[:, 4, c, :], in_=psum[:, 5, 0:128]), [RB(5)], [rpts])
                                  ob = 6 + pvc % 2
                                  pvc += 1
                                  for kb in range(nkb):
                                      blk = (2 + lo + kb) if kb < nlb else (kb - nlb)
                                      PE(lambda e: e.matmul(psum[0:64, ob, :], lhsT=vtok[:, blk, g * 64:(g + 1) * 64],
                                                            rhs=pts[:, kb, :, :].rearrange("p c q -> p (c q)"),
                                                            start=(kb == 0), stop=(kb == nkb - 1)),
                                         [R("vtok", blk), rpts], [RB(ob)])
                                  A(lambda e: e.activation(out=attnT[:, g * 4:(g + 1) * 4, n * 128:(n + 1) * 128],
                                                           in_=psum[0:64, ob, :].rearrange("p (c q) -> p c q", q=128),
                                                           func=AF.Copy), [RB(ob)], [R("attnT", n // 4)])
                              prologue_steps(3)
                          S.barrier()
                  S.barrier()
                  if DEBUG["stop"] == "C":
                      S.dead = True

                  with ExitStack() as pd:
                      cu = sb(pd, "cu", [128, L + 2], F32)
                      yc = sb(pd, "yc", [128, L], F32)
                      gbs = sb(pd, "gbs", [128, L], F32)
                      ut = [sb(pd, f"ut{i}", [128, 512], F32) for i in range(2)]
                      G(lambda e: e.memset(cu[:, 0:1], 0.0), [], [R("cu_pad")])
                      G(lambda e: e.memset(cu[:, L + 1:L + 2], 0.0), [], [R("cu_pad")])
                      cnt = 0
                      for c in range(4):
                          (wg, rg) = ws.get()
                          (wu, ru) = ws.get()
                          (wb_, rb_) = ws.get()
                          ws.prefetch(2)
                          for tg in range(4):
                              bk = (0, 1, 2) if cnt % 2 == 0 else (3, 4, 5)
                              utx = ut[cnt % 2]
                              rut = R("ut", cnt % 2)
                              cnt += 1
                              for (w_, r_, b_) in ((wg, rg, bk[0]), (wu, ru, bk[1]), (wb_, rb_, bk[2])):
                                  for kc in range(8):
                                      PE(lambda e: e.matmul(psum[:, b_, :], lhsT=w_[:, kc, :], rhs=hT[:, kc, tg * 512:(tg + 1) * 512],
                                                            start=(kc == 0), stop=(kc == 7)), [r_, R("hT", tg)], [RB(b_)])
                              A(lambda e: e.activation(out=utx[:], in_=psum[:, bk[1], :], func=AF.Copy), [RB(bk[1])], [rut])
                              V(lambda e: e.tensor_tensor(out=cu[:, 1 + tg * 512: 1 + (tg + 1) * 512], in0=psum[:, bk[0], :],
                                                          in1=utx[:], op=ALU.mult), [RB(bk[0]), rut], [R("cu", tg)])
                              A(lambda e: e.activation(out=gbs[:, tg * 512:(tg + 1) * 512], in_=psum[:, bk[2], :], func=AF.Copy),
                                [RB(bk[2])], [R("gbs", tg)])
                          cur = [R("cu", t) for t in range(4)] + [R("cu_pad")]
                          V(lambda e: e.tensor_scalar(out=yc[:], in0=cu[:, 0:L], scalar1=convw_t[:, c, 0:1], scalar2=None,
                                                      op0=ALU.mult), cur + [R("convw")], [R("yc")])
                          V(lambda e: e.scalar_tensor_tensor(out=yc[:], in0=cu[:, 1:L + 1], scalar=convw_t[:, c, 1:2], in1=yc[:],
                                                             op0=ALU.mult, op1=ALU.add), cur + [R("yc")], [R("yc")])
                          V(lambda e: e.scalar_tensor_tensor(out=yc[:], in0=cu[:, 2:L + 2], scalar=convw_t[:, c, 2:3], in1=yc[:],
                                                             op0=ALU.mult, op1=ALU.add), cur + [R("yc")], [R("yc")])
                          G(lambda e: e.tensor_tensor(out=convo[:, c, :], in0=yc[:], in1=gbs[:], op=ALU.mult),
                            [R("yc")] + [R("gbs", t) for t in range(4)], [R("convo")])
                          prologue_steps(4)
                  S.barrier()
                  if DEBUG["stop"] == "D":
                      S.dead = True

                  with ExitStack() as pe_:
                      xs = sb(pe_, "xs", [128, 8, 512], F32)
                      w_ao_b = sb(pe_, "w_ao_b", [64, 8, 1024], BF16)
                      w_co_b = sb(pe_, "w_co_b", [128, 4, 1024], BF16)
                      w_mo_b = sb(pe_, "w_mo_b", [128, 8, 1024], BF16)
                      load_big(lambda pc: w_ao_b[:, pc, :], lambda pc: w_ao[:, pc, :], 8, 64, "w_ao_b")
                      load_big(lambda pc: w_co_b[:, pc, :], lambda pc: w_co[:, pc, :], 4, 128, "w_co_b")
                      load_big(lambda pc: w_mo_b[:, pc, :], lambda pc: w_mo[:, pc, :], 8, 128, "w_mo_b")
                      mixT = sb(pe_, "mixT", [128, 8, 512], BF16)
                      sg = [sb(pe_, f"sg{i}", [128, 512], F32) for i in range(2)]
                      mm_ = [sb(pe_, f"mm{i}", [128, 512], F32) for i in range(2)]
                      for tg in range(4):
                          tsl = slice(tg * 512, (tg + 1) * 512)
                          S.dma("sp", xs[:], xT[:, :, s * L + tg * 512: s * L + (tg + 1) * 512], writes=[R("xs")])
                          for oc in range(8):
                              (wga, rga) = ws.get()
                              (wgv, rgv) = ws.get()
                              ws.prefetch(2)
                              osl = slice(oc * 128, (oc + 1) * 128)
                              for h in range(8):
                                  PE(lambda e: e.matmul(psum[:, 0, :], lhsT=w_ao_b[:, h, osl], rhs=attnT[:, h, tsl],
                                                        start=(h == 0), stop=(h == 7)), [R("w_ao_b"), R("attnT", tg)], [RB(0)])
                              for c in range(4):
                                  PE(lambda e: e.matmul(psum[:, 1, :], lhsT=w_co_b[:, c, osl], rhs=convo[:, c, tsl],
                                                        start=(c == 0), stop=(c == 3)), [R("w_co_b"), R("convo")], [RB(1)])
                              for kc in range(8):
                                  PE(lambda e: e.matmul(psum[:, 2, :], lhsT=wga[:, kc, :], rhs=hT[:, kc, tsl],
                                                        start=(kc == 0), stop=(kc == 7)), [rga, R("hT", tg)], [RB(2)])
                              for kc in range(8):
                                  PE(lambda e: e.matmul(psum[:, 3, :], lhsT=wgv[:, kc, :], rhs=hT[:, kc, tsl],
                                                        start=(kc == 0), stop=(kc == 7)), [rgv, R("hT", tg)], [RB(3)])
                              A(lambda e: e.activation(out=sg[0][:], in_=psum[:, 2, :], func=AF.Sigmoid), [RB(2)], [R("sg", 0)])
                              A(lambda e: e.activation(out=sg[1][:], in_=psum[:, 3, :], func=AF.Sigmoid), [RB(3)], [R("sg", 1)])
                              V(lambda e: e.tensor_tensor(out=mm_[0][:], in0=psum[:, 0, :], in1=sg[0][:], op=ALU.mult),
                                [RB(0), R("sg", 0)], [R("mm", 0)])
                              V(lambda e: e.tensor_tensor(out=mm_[1][:], in0=psum[:, 1, :], in1=sg[1][:], op=ALU.mult),
                                [RB(1), R("sg", 1)], [R("mm", 1)])
                              G(lambda e: e.tensor_tensor(out=mixT[:, oc, :], in0=mm_[0][:], in1=mm_[1][:], op=ALU.add),
                                [R("mm", 0), R("mm", 1)], [R("mixT")])
                          for oc in range(8):
                              ob = 4 + oc % 2
                              osl = slice(oc * 128, (oc + 1) * 128)
                              for c in range(8):
                                  PE(lambda e: e.matmul(psum[:, ob, :], lhsT=w_mo_b[:, c, osl], rhs=mixT[:, c, :],
                                                        start=(c == 0), stop=(c == 7)), [R("w_mo_b"), R("mixT")], [RB(ob)])
                              V(lambda e: e.scalar_tensor_tensor(out=xs[:, oc, :], in0=psum[:, ob, :],
                                                                 scalar=modp[:, 16 + oc, s:s + 1], in1=xs[:, oc, :],
                                                                 op0=ALU.mult, op1=ALU.add), [RB(ob), R("xs"), R("modp")], [R("xs")])
                          S.dma("sp", x1T[:, :, s * L + tg * 512: s * L + (tg + 1) * 512], xs[:], reads=[R("xs")],
                                writes=[R("x1T", s * 4 + tg)])
                          if DEBUG["x1"]:
                              S.dma("sp", dbg["x1"][:, :, s * L + tg * 512: s * L + (tg + 1) * 512], xs[:], reads=[R("xs")],
                                    writes=[R("dbg_x1")])
                          prologue_steps(6)
                  S.barrier()

              while prologue_step():
                  pass
          pro_stack.close()
          S.barrier()

          TG = 256
          NG = TPC // TG
          if DEBUG["stop_after_mixer"]:
              NG = 0
          with ExitStack() as p2:
              pwq_b = sb(p2, "pwq_b", [128, 8, 2048], BF16)
              keys_b = sb(p2, "keys_b", [128, 16, 128], BF16)
              x1gs = [sb(p2, f"x1g{i}", [128, 8, TG], F32) for i in range(2)]
              sq2 = sb(p2, "sq2", [128, 8, TG], BF16)
              rstd2 = sb(p2, "rstd2", [128, TG], F32)
              ntmp = [sb(p2, f"ntmp{i}", [128, TG], F32) for i in range(2)]
              h2Ts = [sb(p2, f"h2T{i}", [128, 8, TG], BF16) for i in range(2)]
              qpT = sb(p2, "qpT", [128, 16, TG], BF16)
              s1 = sb(p2, "s1", [128, 16, 128], F32)
              m16 = sb(p2, "m16", [128, 16, 16], F32)
              ix = sb(p2, "ix", [128, 16, 16], U32)
              ixf = sb(p2, "ixf", [128, 16, 16], F32)
              cand = sb(p2, "cand", [128, 8, 256], F32)
              b16 = sb(p2, "b16", [128, 8, 16], F32)
              pos = sb(p2, "pos", [128, 8, 16], U32)
              pab = sb(p2, "pab", [128, 2, 128], U32)
              pabf = sb(p2, "pabf", [128, 2, 128], F32)
              ee = sb(p2, "ee", [128, 8, 16], F32)
              zs = sb(p2, "zs", [128, 8], F32)
              IJG = sb(p2, "IJG", [128, 3, 128], F32)
              IJGT = sb(p2, "IJGT", [128, 3, TG], F32)
              NB = 16
              Pmx = [sb(p2, f"Pmx{i}", [128, NB, 128], BF16) for i in range(2)]
              Qmx = [sb(p2, f"Qmx{i}", [128, NB, 128], BF16) for i in range(2)]
              Wsum = sb(p2, "Wsum", [128, 128, TG], BF16)
              NS = 3
              uv = [sb(p2, f"uv{i}", [128, 2048], BF16) for i in range(NS)]
              ge = [sb(p2, f"ge{i}", [128, TG], F32) for i in range(2)]
              Lj = [sb(p2, f"Lj{i}", [128, TG], BF16) for i in range(2)]

              with ExitStack() as pl:
                  stg = sb(pl, "stg", [128, 1024], F32)
                  for kc in range(8):
                      for hf in range(2):
                          S.dma("sp", stg[:], pw_q[:, kc, hf * 1024:(hf + 1) * 1024], writes=[R("stg")])
                          G(lambda e: e.tensor_copy(out=pwq_b[:, kc, hf * 1024:(hf + 1) * 1024], in_=stg[:]), [R("stg")], [R("pwq_b")])
                  for hf in range(2):
                      S.dma("sp", stg[:], keysT[:, hf * 8:(hf + 1) * 8, :].rearrange("p a b -> p (a b)"), writes=[R("stg")])
                      G(lambda e: e.tensor_copy(out=keys_b[:, hf * 8:(hf + 1) * 8, :].rearrange("p a b -> p (a b)"), in_=stg[:]),
                        [R("stg")], [R("keys_b")])
              S.barrier()

              scnt = [0]
              acnt = [0]

              def sel1(gi):
                  s = gi // (NG // 2) if NG >= 2 else 0
                  t0 = gi * TG
                  pb = gi % 2
                  x1g, h2T = x1gs[pb], h2Ts[pb]
                  rx, rh = R("x1g", pb), R("h2T", pb)
                  S.dma("sp", x1g[:], x1T[:, :, t0:t0 + TG], reads=[R("x1T", t0 // 512)], writes=[rx])
                  norm_group(x1g[:], rx, TG, sq2, rstd2, ntmp, 7,
                             lambda c: A2[:, c, s:s + 1], lambda c: modp[:, 24 + c, s:s + 1],
                             lambda c: h2T[:, c, :], rh, False)
                  yield
                  for hp in range(16):
                      bank = 6 + (hp // 2) % 2
                      off = (hp % 2) * TG
                      for kc in range(8):
                          PE(lambda e: e.matmul(psum[:, bank, off:off + TG], lhsT=pwq_b[:, kc, hp * 128:(hp + 1) * 128],
                                                rhs=h2T[:, kc, :], start=(kc == 0), stop=(kc == 7)),
                             [R("pwq_b"), rh], [RB(bank)])
                      if hp % 2 == 1:
                          A(lambda e: e.activation(out=qpT[:, hp - 1:hp + 1, :],
                                                   in_=psum[:, bank, :].rearrange("p (a t) -> p a t", t=TG), func=AF.Copy),
                            [RB(bank)], [R("qpT")])
                      yield
                  for tt in range(TG // 128):
                      tsl = slice(tt * 128, (tt + 1) * 128)
                      for hp in range(16):
                          bank = 4 + hp // 4
                          off = (hp % 4) * 128
                          PE(lambda e: e.matmul(psum[:, bank, off:off + 128], lhsT=qpT[:, hp, tsl], rhs=keys_b[:, hp, :],
                                                start=True, stop=True), [R("qpT"), R("keys_b")], [RB(bank)])
                      A(lambda e: e.activation(out=s1[:].rearrange("p a b -> p (a b)"),
                                               in_=psum[:, 4:8, :].rearrange("p a b -> p (a b)"), func=AF.Copy),
                        [RB(4), RB(5), RB(6), RB(7)], [R("s1")])
                      yield
                      for hp in range(16):
                          V(lambda e: e.max(out=m16[:, hp, 0:8], in_=s1[:, hp, :]), [R("s1")], [R("m16")])
                          V(lambda e: e.max_index(out=ix[:, hp, 0:8], in_max=m16[:, hp, 0:8], in_values=s1[:, hp, :]),
                            [R("s1"), R("m16")], [R("ix")])
                          V(lambda e: e.match_replace(out=s1[:, hp, :], in_to_replace=m16[:, hp, 0:8], in_values=s1[:, hp, :],
                                                      imm_value=NEG), [R("s1"), R("m16")], [R("s1")])
                          V(lambda e: e.max(out=m16[:, hp, 8:16], in_=s1[:, hp, :]), [R("s1")], [R("m16")])
                          V(lambda e: e.max_index(out=ix[:, hp, 8:16], in_max=m16[:, hp, 8:16], in_values=s1[:, hp, :]),
                            [R("s1"), R("m16")], [R("ix")])
                          yield
                      V(lambda e: e.tensor_copy(out=ixf[:], in_=ix[:]), [R("ix")], [R("ixf")])
                      m4 = m16[:].rearrange("p (h two) k -> p h two k", two=2)
                      i4 = ixf[:].rearrange("p (h two) k -> p h two k", two=2)
                      V(lambda e: e.tensor_tensor(out=cand[:].rearrange("p h (a b) -> p h a b", b=16),
                                                  in0=m4[:, :, 0, :].unsqueeze(3).to_broadcast([128, 8, 16, 16]),
                                                  in1=m4[:, :, 1, :].unsqueeze(2).to_broadcast([128, 8, 16, 16]), op=ALU.add),
                        [R("m16")], [R("cand")])
                      yield
                      for h in range(8):
                          V(lambda e: e.max(out=b16[:, h, 0:8], in_=cand[:, h, :]), [R("cand")], [R("b16")])
                          V(lambda e: e.max_index(out=pos[:, h, 0:8], in_max=b16[:, h, 0:8], in_values=cand[:, h, :]),
                            [R("cand"), R("b16")], [R("pos")])
                          V(lambda e: e.match_replace(out=cand[:, h, :], in_to_replace=b16[:, h, 0:8], in_values=cand[:, h, :],
                                                      imm_value=NEG), [R("cand"), R("b16")], [R("cand")])
                          V(lambda e: e.max(out=b16[:, h, 8:16], in_=cand[:, h, :]), [R("cand")], [R("b16")])
                          V(lambda e: e.max_index(out=pos[:, h, 8:16], in_max=b16[:, h, 8:16], in_values=cand[:, h, :]),
                            [R("cand"), R("b16")], [R("pos")])
                          yield
                      V(lambda e: e.tensor_tensor(out=ee[:], in0=b16[:], in1=b16[:, :, 0:1].to_broadcast([128, 8, 16]),
                                                  op=ALU.subtract), [R("b16")], [R("ee")])
                      A(lambda e: e.activation(out=ee[:], in_=ee[:], func=AF.Exp), [R("ee")], [R("ee")])
                      V(lambda e: e.reduce_sum(out=zs[:], in_=ee[:], axis=AX.X), [R("ee")], [R("zs")])
                      V(lambda e: e.reciprocal(out=zs[:], in_=zs[:]), [R("zs")], [R("zs")])
                      V(lambda e: e.tensor_tensor(out=IJG[:, 2, :].rearrange("p (h k) -> p h k", k=16), in0=ee[:],
                                                  in1=zs[:, :].unsqueeze(2).to_broadcast([128, 8, 16]), op=ALU.mult),
                        [R("ee"), R("zs")], [R("IJG")])
                      yield
                      posf = pos[:].rearrange("p h k -> p (h k)")
                      V(lambda e: e.tensor_scalar(out=pab[:, 0, :], in0=posf, scalar1=cu32_t[:, 0:1], scalar2=None,
                                                  op0=ALU.logical_shift_right), [R("pos"), R("cu32")], [R("pab")])
                      V(lambda e: e.tensor_scalar(out=pab[:, 1, :], in0=posf, scalar1=cu32_t[:, 1:2], scalar2=None,
                                                  op0=ALU.bitwise_and), [R("pos"), R("cu32")], [R("pab")])
                      V(lambda e: e.tensor_copy(out=pabf[:], in_=pab[:]), [R("pab")], [R("pabf")])
                      eq = cand[:].rearrange("p h (a b) -> p h a b", b=16)
                      io = iota_t[:, 0:16].unsqueeze(1).unsqueeze(1).to_broadcast([128, 8, 16, 16])
                      for w in range(2):
                          sel = pabf[:, w, :].rearrange("p (h k) -> p h k", k=16).unsqueeze(3).to_broadcast([128, 8, 16, 16])
                          V(lambda e: e.tensor_tensor(out=eq, in0=sel, in1=io, op=ALU.is_equal),
                            [R("pabf"), R("iota"), R("cand")], [R("cand")])
                          V(lambda e: e.tensor_tensor(out=eq, in0=eq, in1=i4[:, :, w, :].unsqueeze(2).to_broadcast([128, 8, 16, 16]),
                                                      op=ALU.mult), [R("cand"), R("ixf")], [R("cand")])
                          V(lambda e: e.tensor_reduce(out=IJG[:, w, :].rearrange("p (h k) -> p h k", k=16), in_=eq,
                                                      axis=AX.X, op=ALU.add), [R("cand")], [R("IJG")])
                          yield
                      if DEBUG["sel"] and gi == 0 and tt == 0:
                          S.dma("sp", dbg["sel"][:, :, :], IJG[:], reads=[R("IJG")], writes=[R("dbg_sel")])
                      for w in range(3):
                          PE(lambda e: e.transpose(out=psum[:, 6, w * 128:(w + 1) * 128], in_=IJG[:, w, :], identity=ident_f[:]),
                             [R("IJG"), R("ident_f")], [RB(6)])
                      A(lambda e: e.activation(out=IJGT[:, :, tsl], in_=psum[:, 6, 0:384].rearrange("p (w t) -> p w t", t=128),
                                               func=AF.Copy), [RB(6)], [R("IJGT")])
                      yield

              def sel2(gi):
                  for tt in range(TG // 128):
                      for bt in range(128 // NB):
                          i2 = scnt[0] % 2
                          scnt[0] += 1
                          Px_, Qx_ = Pmx[i2], Qmx[i2]
                          rPx, rQx = R("Pmx", i2), R("Qmx", i2)
                          tb0 = tt * 128 + bt * NB
                          iob = iota_t[:, :].unsqueeze(1).to_broadcast([128, NB, 128])
                          V(lambda e: e.tensor_tensor(out=Px_[:], in0=IJGT[:, 0, tb0:tb0 + NB].unsqueeze(2).to_broadcast([128, NB, 128]),
                                                      in1=iob, op=ALU.is_equal), [R("IJGT"), R("iota")], [rPx])
                          V(lambda e: e.tensor_tensor(out=Qx_[:], in0=IJGT[:, 1, tb0:tb0 + NB].unsqueeze(2).to_broadcast([128, NB, 128]),
                                                      in1=iob, op=ALU.is_equal), [R("IJGT"), R("iota")], [rQx])
                          V(lambda e: e.tensor_tensor(out=Qx_[:], in0=Qx_[:],
                                                      in1=IJGT[:, 2, tb0:tb0 + NB].unsqueeze(2).to_broadcast([128, NB, 128]),
                                                      op=ALU.mult), [R("IJGT"), rQx], [rQx])
                          for tl in range(NB):
                              bank = 4 + tl // 4
                              off = (tl % 4) * 128
                              PE(lambda e: e.matmul(psum[:, bank, off:off + 128], lhsT=Px_[:, tl, :], rhs=Qx_[:, tl, :],
                                                    start=True, stop=True), [rPx, rQx], [RB(bank)])
                          A(lambda e: e.activation(out=Wsum[:, :, tb0:tb0 + NB],
                                                   in_=psum[:, 4:8, :].rearrange("p a (t j) -> p j (a t)", j=128),
                                                   func=AF.Copy), [RB(4), RB(5), RB(6), RB(7)], [R("Wsum")])

              def drain(gen):
                  if gen is not None:
                      for _ in gen:
                          pass

              drain(sel1(0) if NG > 0 else None)
              for gi in range(NG):
                  s = gi // (NG // 2) if NG >= 2 else 0
                  t0 = gi * TG
                  pb = gi % 2
                  x1g, h2T = x1gs[pb], h2Ts[pb]
                  rx, rh = R("x1g", pb), R("h2T", pb)
                  sel2(gi)
                  gen = sel1(gi + 1) if gi + 1 < NG else None
                  for b in range(4):
                      PE(lambda e: e.matmul(psum[:, b, :], lhsT=zeros_b[:, 0:128], rhs=zeros_b[:], start=True, stop=False,
                                            skip_group_check=True), [R("zeros_b")], [RB(b)])
                  for j in range(128):
                      sl = acnt[0] % NS
                      i2 = acnt[0] % 2
                      acnt[0] += 1
                      S.dma("sp", uv[sl][:], uvb_d[j], reads=[R("scr_u", j), R("scr_v", j)], writes=[R("uv", sl)])
                      ab = 4 + i2
                      for dc in range(8):
                          PE(lambda e: e.matmul(psum[:, ab, 0:TG], lhsT=uv[sl][:, dc * 128:(dc + 1) * 128], rhs=h2T[:, dc, :],
                                                start=(dc == 0), stop=(dc == 7)), [R("uv", sl), rh], [RB(ab)])
                      A(lambda e: e.activation(out=ge[i2][:], in_=psum[:, ab, 0:TG], func=AF.Gelu), [RB(ab)], [R("ge", i2)])
                      V(lambda e: e.tensor_tensor(out=Lj[i2][:], in0=ge[i2][:], in1=Wsum[:, j, :], op=ALU.mult),
                        [R("ge", i2), R("Wsum")], [R("Lj", i2)])
                      for dc in range(8):
                          PE(lambda e: e.matmul(psum[:, dc // 2, (dc % 2) * TG:(dc % 2 + 1) * TG],
                                                lhsT=uv[sl][:, 1024 + dc * 128:1024 + (dc + 1) * 128], rhs=Lj[i2][:],
                                                start=False, stop=(j == 127), skip_group_check=True),
                             [R("uv", sl), R("Lj", i2)], [RB(dc // 2)])
                      if gen is not None and j >= 2:
                          next(gen, None)
                  drain(gen)
                  for dc in range(8):
                      V(lambda e: e.scalar_tensor_tensor(out=x1g[:, dc, :], in0=psum[:, dc // 2, (dc % 2) * TG:(dc % 2 + 1) * TG],
                                                         scalar=modp[:, 40 + dc, s:s + 1], in1=x1g[:, dc, :],
                                                         op0=ALU.mult, op1=ALU.add), [RB(dc // 2), rx, R("modp")], [rx])
                  norm_group(x1g[:], rx, TG, sq2, rstd2, None, 6,
                             lambda c: fing_t[:, c:c + 1], None,
                             lambda c: x1g[:, c, :], rx, True)
                  S.dma("sp", outT[:, :, t0:t0 + TG], x1g[:], reads=[rx], writes=[R("outT", gi)])

        except _Stop:
            pass
        build.stats = dict(S.cnt); build.stats["dma"] = dict(S.dq); build.stats["sig"] = dict(S.sig)
        build.used = S.used
        S.finish([R("outT", gi) for gi in range(NG)] + ([R("dbg_x1")] if DEBUG["x1"] else [])
                 + ([R("dbg_sel")] if DEBUG["sel"] else []) + [R("x1T", i) for i in range(8)])
    return nc


def _fm(a):
    rows, D = a.shape
    return np.ascontiguousarray(a.T.reshape(D // 128, 128, rows).transpose(1, 0, 2))


def _partner(d):
    return d + 16 if (d % 32) < 16 else d - 16


def prep_shared(inp):
    f = np.float32
    sh = {}
    w_mod = np.asarray(inp["w_mod"], f)[0]
    sh["w_mod"] = np.ascontiguousarray(w_mod.reshape(8, 128, 6144).transpose(1, 0, 2))
    sh["b_mod"] = np.ascontiguousarray(np.asarray(inp["b_mod"], f)[0].reshape(48, 128).T)
    sh["n1g"] = np.ascontiguousarray(np.asarray(inp["norm1_g"], f)[0].reshape(8, 128).T)
    sh["n2g"] = np.ascontiguousarray(np.asarray(inp["norm2_g"], f)[0].reshape(8, 128).T)
    sh["fing"] = np.ascontiguousarray(np.asarray(inp["final_g"], f).reshape(8, 128).T)
    w_in = np.asarray(inp["w_in"], f)[0]
    pr = np.array([_partner(d) for d in range(64)])
    chunks = []
    for c in range(4):
        chunks.append(np.concatenate([c * 64 + np.arange(64), (4 + c) * 64 + np.arange(64)]))
    for c in range(4):
        chunks.append(np.concatenate([c * 64 + pr, (4 + c) * 64 + pr]))
    chunks.append(512 + np.arange(128))
    chunks.append(512 + np.concatenate([pr, 64 + pr]))
    chunks.append(640 + np.arange(128))
    GB, GC, UU, GA, GV = 768, 1280, 1792, 2304, 3328
    for c in range(4):
        chunks.append(GC + c * 128 + np.arange(128))
        chunks.append(UU + c * 128 + np.arange(128))
        chunks.append(GB + c * 128 + np.arange(128))
    for c in range(8):
        chunks.append(GA + c * 128 + np.arange(128))
    for c in range(8):
        chunks.append(GV + c * 128 + np.arange(128))
    assert len(chunks) == 39
    wl = np.empty((39, 128, 8, 128), f)
    for i, cols in enumerate(chunks):
        wl[i] = w_in[:, cols].reshape(8, 128, 128).transpose(1, 0, 2)
    sh["w_in"] = wl
    sh["w_ao"] = np.ascontiguousarray(np.asarray(inp["w_attn_out"], f)[0].reshape(8, 64, 1024).transpose(1, 0, 2))
    sh["w_co"] = np.ascontiguousarray(np.asarray(inp["w_conv_out"], f)[0].reshape(4, 128, 1024).transpose(1, 0, 2))
    sh["w_mo"] = np.ascontiguousarray(np.asarray(inp["w_mix_out"], f)[0].reshape(8, 128, 1024).transpose(1, 0, 2))
    sh["conv_w"] = np.ascontiguousarray(np.asarray(inp["conv_w"], f)[0].reshape(3, 4, 128).transpose(2, 1, 0))
    sh["sinkv"] = np.ascontiguousarray(np.broadcast_to(np.asarray(inp["attn_sink"], f)[0][None, :], (128, 8)))
    sh["pw_q"] = np.ascontiguousarray(np.asarray(inp["peer_w_q"], f)[0].reshape(8, 128, 2048).transpose(1, 0, 2))
    sk = np.asarray(inp["peer_sub_keys"], f)[0]
    sh["keysT"] = np.ascontiguousarray(sk.reshape(16, 128, 128).transpose(2, 0, 1))
    pu = np.asarray(inp["peer_u"], f)[0]
    sh["uT"] = np.ascontiguousarray(pu.reshape(128, 128, 8, 128).transpose(1, 3, 2, 0)).reshape(128, 128, 1024)
    pv = np.asarray(inp["peer_v"], f)[0]
    sh["vt"] = np.ascontiguousarray(pv.reshape(128, 128, 1024).transpose(1, 0, 2))
    inv_freq = (np.float32(10000.0) ** (-np.arange(16, dtype=f) / np.float32(16))).astype(f)
    l = np.arange(L)
    cos_t = np.empty((128, L), f)
    sin_t = np.empty((128, L), f)
    for p in range(128):
        d = p % 64
        posn = (l % 64) if d >= 32 else (l // 64)
        ang = posn.astype(f) * inv_freq[d % 16]
        cos_t[p] = np.cos(ang)
        sin_t[p] = np.sin(ang) * (-1.0 if (d % 32) < 16 else 1.0)
    sh["rope_cos"] = cos_t
    sh["rope_sin"] = sin_t
    qi = np.arange(128)[:, None]
    kk = np.arange(384)[None, :]
    sh["amask"] = np.where((kk >= qi) & (kk <= qi + 256), 0.0, NEG).astype(f)
    sh["ident"] = np.eye(128, dtype=f)
    sh["iota128"] = np.ascontiguousarray(np.broadcast_to(np.arange(128, dtype=f)[None, :], (128, 128)))
    sh["cu32"] = np.ascontiguousarray(np.broadcast_to(np.array([4, 15], np.uint32)[None, :], (128, 2)))
    return sh


def prep_core(inp, core, sh):
    f = np.float32
    x = np.asarray(inp["x"], f)
    ctx = np.asarray(inp["ctx"], f)
    c = np.asarray(inp["c"], f)
    c_ctx = np.asarray(inp["c_ctx"], f)
    m = dict(sh)
    b0 = 2 * core
    m["xT"] = _fm(x[b0:b0 + 2].reshape(2 * L, 1024))
    m["ctxT"] = _fm(ctx[b0:b0 + 2].reshape(512, 1024))
    m["cT"] = _fm(np.stack([c[b0], c[b0 + 1], c_ctx], 0))
    return m


_NC_CACHE = {}


def kernel(**inputs):
    sh = prep_shared(inputs)
    in_maps = [prep_core(inputs, core, sh) for core in range(NCORES)]
    if "nc" not in _NC_CACHE:
        build()
        _NC_CACHE["nc"] = build(used=set(build.used))
    nc = _NC_CACHE["nc"]
    res = run_bass_kernel_spmd(nc, in_maps, core_ids=list(range(NCORES)))
    out = np.empty((16, L, 1024), np.float32)
    for core in range(NCORES):
        o = np.asarray(res.results[core]["outT"])
        o = o.transpose(2, 1, 0).reshape(2, L, 1024)
        out[2 * core:2 * core + 2] = o
    return out
```
